# Optimizing a Trainium2 kernel written in Bass

```python
import math
import jax, jax.numpy as jnp
from jax import lax
import numpy as np

D_MODEL = 1024
BATCH = 8
SEQ = 4096
DEPTH = 4

GRID_W = 64
HEAD_DIM = 64
NA_HEADS = 4
NA_KH_MAX = 8
NA_KW = 16
WIN_HEADS = 4
WIN_KV_HEADS = 2
WINDOW = 128
WIN_BLOCK = 128
MLA_HEADS = 4
MLA_Q_RANK = 192
MLA_KV_RANK = 256
MLA_NOPE = 64
MLA_ROPE = 32
MLA_V = 64
AX_HEADS = 4
AX_KV_HEADS = 2
Q_BLOCK = 128
ROPE_THETA = 10000.0
N_BRANCH = 4
BRANCH_W = 256
N_GROUPS = 4
EXPERTS_PER_GROUP = 8
N_EXPERTS = N_GROUPS * EXPERTS_PER_GROUP
TOP_K = 2
D_EXPERT = 256
N_MOD = 6
EPS = 1e-6
NEG_INF = -1e30

IN_SIZES = (
    NA_HEADS * HEAD_DIM, NA_HEADS * HEAD_DIM, NA_HEADS * HEAD_DIM,
    WIN_HEADS * HEAD_DIM, WIN_KV_HEADS * HEAD_DIM, WIN_KV_HEADS * HEAD_DIM,
    MLA_Q_RANK, MLA_KV_RANK, MLA_ROPE,
    AX_HEADS * HEAD_DIM, AX_KV_HEADS * HEAD_DIM, AX_KV_HEADS * HEAD_DIM,
    N_BRANCH * D_MODEL,
)
D_IN = sum(IN_SIZES)

kernel_name = 'hybrid_gated_multimixer_hmoe_encoder'


def rmsnorm(x, g):
    xf = x.astype(jnp.float32)
    y = xf * lax.rsqrt(jnp.mean(xf * xf, axis=-1, keepdims=True) + EPS)
    return (y * g.astype(jnp.float32)).astype(x.dtype)


def to_heads(t, n):
    B, S, _ = t.shape
    return t.reshape(B, S, n, -1).transpose(0, 2, 1, 3)


def from_heads(t):
    B, H, S, d = t.shape
    return t.transpose(0, 2, 1, 3).reshape(B, S, H * d)


def rope_angles(pos, dim):
    inv = ROPE_THETA ** (-jnp.arange(0, dim, 2, dtype=jnp.float32) / dim)
    ang = pos.astype(jnp.float32)[:, None] * inv[None, :]
    return jnp.cos(ang), jnp.sin(ang)


def apply_rope(x, cos, sin):
    x1, x2 = jnp.split(x, 2, axis=-1)
    c = cos.astype(x.dtype)
    s = sin.astype(x.dtype)
    return jnp.concatenate([x1 * c - x2 * s, x1 * s + x2 * c], axis=-1)


def axial_rope(x, cos_r, sin_r, cos_c, sin_c):
    xr, xc = jnp.split(x, 2, axis=-1)
    return jnp.concatenate([apply_rope(xr, cos_r, sin_r), apply_rope(xc, cos_c, sin_c)], axis=-1)


def neighbourhood_attention(q, k, v, rel_bias, scale):
    B, H, S, dh = q.shape
    rows = S // GRID_W
    kh = min(NA_KH_MAX, rows)
    qg = q.reshape(B, H, rows, GRID_W, dh)
    kg = k.reshape(B, H, rows, GRID_W, dh)
    vg = v.reshape(B, H, rows, GRID_W, dh)
    r = jnp.arange(rows)
    row_start = jnp.clip(r - kh // 2, 0, rows - kh)
    key_rows = row_start[:, None] + jnp.arange(kh)[None, :]
    k_blk = kg[:, :, key_rows]
    v_blk = vg[:, :, key_rows]
    col = jnp.arange(GRID_W)
    col_start = jnp.clip(col - NA_KW // 2, 0, GRID_W - NA_KW)
    col_off = col[None, :] - col_start[:, None]
    col_in = (col_off >= 0) & (col_off < NA_KW)
    dr = key_rows - r[:, None]
    dc_idx = jnp.clip(col[None, :] - col[:, None], -(NA_KW - 1), NA_KW - 1) + NA_KW - 1
    bias = rel_bias.astype(jnp.float32)[:, dr + NA_KH_MAX - 1]
    bias = bias[..., dc_idx].transpose(0, 1, 3, 2, 4)
    s = jnp.einsum('bhrqd,bhrikd->bhrqik', qg, k_blk, preferred_element_type=jnp.float32) * scale
    s = jnp.where(col_in[:, None, :], s + bias[None], NEG_INF)
    p = jax.nn.softmax(s.reshape(B, H, rows, GRID_W, kh * GRID_W), axis=-1)
    p = p.reshape(B, H, rows, GRID_W, kh, GRID_W).astype(v.dtype)
    out = jnp.einsum('bhrqik,bhrikd->bhrqd', p, v_blk)
    return out.reshape(B, H, S, dh)


def window_attention(q, k, v, sink, scale):
    B, Hq, S, dh = q.shape
    Hkv = k.shape[1]
    G = Hq // Hkv
    nb = S // WIN_BLOCK
    qb = q.reshape(B, Hkv, G, nb, WIN_BLOCK, dh)
    pad = ((0, 0), (0, 0), (WIN_BLOCK, WIN_BLOCK), (0, 0))
    kp = jnp.pad(k, pad).reshape(B, Hkv, nb + 2, WIN_BLOCK, dh)
    vp = jnp.pad(v, pad).reshape(B, Hkv, nb + 2, WIN_BLOCK, dh)
    kw = jnp.concatenate([kp[:, :, :-2], kp[:, :, 1:-1], kp[:, :, 2:]], axis=3)
    vw = jnp.concatenate([vp[:, :, :-2], vp[:, :, 1:-1], vp[:, :, 2:]], axis=3)
    i = jnp.arange(WIN_BLOCK)
    m = jnp.arange(3 * WIN_BLOCK)
    rel = m[None, :] - WIN_BLOCK - i[:, None]
    s_abs = (jnp.arange(nb)[:, None] - 1) * WIN_BLOCK + m[None, :]
    valid = (jnp.abs(rel) <= WINDOW)[None] & ((s_abs >= 0) & (s_abs < S))[:, None, :]
    slopes = 2.0 ** (-8.0 * (jnp.arange(Hq, dtype=jnp.float32) + 1.0) / Hq)
    alibi = -slopes.reshape(Hkv, G)[:, :, None, None, None] * jnp.abs(rel).astype(jnp.float32)
    s = jnp.einsum('bkgnid,bknmd->bkgnim', qb, kw, preferred_element_type=jnp.float32) * scale
    s = jnp.where(valid, s + alibi, NEG_INF)
    sink_col = jnp.broadcast_to(sink.astype(jnp.float32).reshape(1, Hkv, G, 1, 1, 1), s.shape[:-1] + (1,))
    p = jax.nn.softmax(jnp.concatenate([s, sink_col], axis=-1), axis=-1)[..., :-1]
    out = jnp.einsum('bkgnim,bknmd->bkgnid', p.astype(v.dtype), vw)
    return out.reshape(B, Hq, S, dh)


def dense_attention(q, k, v, scale):
    B, Hq, S, dq = q.shape
    Hkv = k.shape[1]
    G = Hq // Hkv
    dv = v.shape[-1]
    nb = S // Q_BLOCK
    qb = q.reshape(B, Hkv, G, nb, Q_BLOCK, dq).transpose(3, 0, 1, 2, 4, 5)

    def block(qi):
        s = jnp.einsum('bkgid,bksd->bkgis', qi, k, preferred_element_type=jnp.float32) * scale
        p = jax.nn.softmax(s, axis=-1)
        return jnp.einsum('bkgis,bksd->bkgid', p.astype(v.dtype), v)

    out = lax.map(block, qb)
    return out.transpose(1, 2, 3, 0, 4, 5).reshape(B, Hq, S, dv)


def hybrid_mixer(h, rope_tabs, w_in, na_rel_bias, win_sink, mla_q_norm_g, mla_kv_norm_g, w_uq, w_ukv,
                 ax_q_norm_g, ax_k_norm_g, w_branch, w_out):
    B, S, _ = h.shape
    cos_t, sin_t, cos_r, sin_r, cos_c, sin_c = rope_tabs
    proj = h @ w_in
    (na_q, na_k, na_v, win_q, win_k, win_v, c_q, c_kv, k_rope,
     ax_q, ax_k, ax_v, gate_logits) = jnp.split(proj, np.cumsum(IN_SIZES)[:-1].tolist(), axis=-1)
    scale = HEAD_DIM ** -0.5
    y_a = neighbourhood_attention(to_heads(na_q, NA_HEADS), to_heads(na_k, NA_HEADS),
                                  to_heads(na_v, NA_HEADS), na_rel_bias, scale)
    y_b = window_attention(to_heads(win_q, WIN_HEADS), to_heads(win_k, WIN_KV_HEADS),
                           to_heads(win_v, WIN_KV_HEADS), win_sink, scale)
    q_c = to_heads(rmsnorm(c_q, mla_q_norm_g) @ w_uq, MLA_HEADS)
    q_nope, q_pe = jnp.split(q_c, [MLA_NOPE], axis=-1)
    kv_c = to_heads(rmsnorm(c_kv, mla_kv_norm_g) @ w_ukv, MLA_HEADS)
    k_nope, v_c = jnp.split(kv_c, [MLA_NOPE], axis=-1)
    k_pe = apply_rope(k_rope[:, None], cos_t, sin_t)
    q_full = jnp.concatenate([q_nope, apply_rope(q_pe, cos_t, sin_t)], axis=-1)
    k_full = jnp.concatenate([k_nope, jnp.broadcast_to(k_pe, (B, MLA_HEADS, S, MLA_ROPE))], axis=-1)
    y_c = dense_attention(q_full, k_full, v_c, (MLA_NOPE + MLA_ROPE) ** -0.5)
    q_d = axial_rope(rmsnorm(to_heads(ax_q, AX_HEADS), ax_q_norm_g), cos_r, sin_r, cos_c, sin_c)
    k_d = axial_rope(rmsnorm(to_heads(ax_k, AX_KV_HEADS), ax_k_norm_g), cos_r, sin_r, cos_c, sin_c)
    y_d = dense_attention(q_d, k_d, to_heads(ax_v, AX_KV_HEADS), scale)
    gates = jax.nn.sigmoid(gate_logits.reshape(B, S, N_BRANCH, D_MODEL))
    branches = (y_a, y_b, y_c, y_d)
    merged = gates[:, :, 0] * (from_heads(branches[0]) @ w_branch[0])
    for n in range(1, N_BRANCH):
        merged = merged + gates[:, :, n] * (from_heads(branches[n]) @ w_branch[n])
    return merged @ w_out


def hierarchical_moe(h, w_group, b_group, w_router, b_router, w_exp1, w_exp3, w_exp2):
    B, S, D = h.shape
    t = h.reshape(-1, D)
    N = t.shape[0]
    g_logits = (t @ w_group).astype(jnp.float32) + b_group.astype(jnp.float32)
    g_prob = jax.nn.softmax(g_logits, axis=-1)
    g_sel = jnp.argmax(g_logits, axis=-1)
    g_w = jnp.take_along_axis(g_prob, g_sel[:, None], axis=-1)
    e_logits = ((t @ w_router).astype(jnp.float32) + b_router.astype(jnp.float32)).reshape(N, N_GROUPS, EXPERTS_PER_GROUP)
    e_logits = jnp.take_along_axis(e_logits, g_sel[:, None, None], axis=1)[:, 0]
    e_prob = jax.nn.softmax(e_logits, axis=-1)
    top_p, top_i = lax.top_k(e_prob, TOP_K)
    weights = g_w * top_p / jnp.sum(top_p, axis=-1, keepdims=True)
    expert_id = g_sel[:, None] * EXPERTS_PER_GROUP + top_i
    combine = jnp.sum(jax.nn.one_hot(expert_id, N_EXPERTS, dtype=jnp.float32) * weights[..., None], axis=1)

    def expert_step(acc, params):
        w1e, w3e, w2e, ce = params
        hid = jax.nn.silu(t @ w1e) * (t @ w3e)
        return acc + ce[:, None].astype(t.dtype) * (hid @ w2e), None

    out, _ = lax.scan(expert_step, jnp.zeros_like(t), (w_exp1, w_exp3, w_exp2, combine.T))
    return out.reshape(B, S, D)


def setup_inputs(seed: int = 0) -> dict:
    key = jax.random.key(seed)
    ks = jax.random.split(key, 25)
    L, D = DEPTH, D_MODEL

    def nrm(k, shape, scale):
        return jax.random.normal(k, shape, jnp.float32) * scale

    return {
        'x': nrm(ks[0], (BATCH, SEQ, D), 1.0),
        'c': nrm(ks[1], (BATCH, D), 1.0),
        'w_ada': nrm(ks[2], (L, D, N_MOD * D), 0.5 * D ** -0.5),
        'b_ada': nrm(ks[3], (L, N_MOD * D), 0.01),
        'norm1_g': 1.0 + nrm(ks[4], (L, D), 0.01),
        'norm2_g': 1.0 + nrm(ks[5], (L, D), 0.01),
        'w_in': nrm(ks[6], (L, D, D_IN), D ** -0.5),
        'na_rel_bias': nrm(ks[7], (L, NA_HEADS, 2 * NA_KH_MAX - 1, 2 * NA_KW - 1), 0.1),
        'win_sink': nrm(ks[8], (L, WIN_HEADS), 0.5),
        'mla_q_norm_g': 1.0 + nrm(ks[9], (L, MLA_Q_RANK), 0.01),
        'mla_kv_norm_g': 1.0 + nrm(ks[10], (L, MLA_KV_RANK), 0.01),
        'w_uq': nrm(ks[11], (L, MLA_Q_RANK, MLA_HEADS * (MLA_NOPE + MLA_ROPE)), MLA_Q_RANK ** -0.5),
        'w_ukv': nrm(ks[12], (L, MLA_KV_RANK, MLA_HEADS * (MLA_NOPE + MLA_V)), MLA_KV_RANK ** -0.5),
        'ax_q_norm_g': 1.0 + nrm(ks[13], (L, HEAD_DIM), 0.01),
        'ax_k_norm_g': 1.0 + nrm(ks[14], (L, HEAD_DIM), 0.01),
        'w_branch': nrm(ks[15], (L, N_BRANCH, BRANCH_W, D), BRANCH_W ** -0.5),
        'w_out': nrm(ks[16], (L, D, D), D ** -0.5),
        'w_group': nrm(ks[17], (L, D, N_GROUPS), D ** -0.5),
        'b_group': nrm(ks[18], (L, N_GROUPS), 0.01),
        'w_router': nrm(ks[19], (L, D, N_EXPERTS), D ** -0.5),
        'b_router': nrm(ks[20], (L, N_EXPERTS), 0.01),
        'w_exp1': nrm(ks[21], (L, N_EXPERTS, D, D_EXPERT), D ** -0.5),
        'w_exp3': nrm(ks[22], (L, N_EXPERTS, D, D_EXPERT), D ** -0.5),
        'w_exp2': nrm(ks[23], (L, N_EXPERTS, D_EXPERT, D), D_EXPERT ** -0.5),
        'final_norm_g': 1.0 + nrm(ks[24], (D,), 0.01),
    }


def reference(x, c, w_ada, b_ada, norm1_g, norm2_g, w_in, na_rel_bias, win_sink, mla_q_norm_g,
              mla_kv_norm_g, w_uq, w_ukv, ax_q_norm_g, ax_k_norm_g, w_branch, w_out, w_group, b_group,
              w_router, b_router, w_exp1, w_exp3, w_exp2, final_norm_g):
    S = x.shape[1]
    pos = jnp.arange(S)
    cos_t, sin_t = rope_angles(pos, MLA_ROPE)
    cos_r, sin_r = rope_angles(pos // GRID_W, HEAD_DIM // 2)
    cos_c, sin_c = rope_angles(pos % GRID_W, HEAD_DIM // 2)
    rope_tabs = (cos_t, sin_t, cos_r, sin_r, cos_c, sin_c)
    c_act = jax.nn.silu(c)
    for l in range(DEPTH):
        mod = c_act @ w_ada[l] + b_ada[l]
        sh1, sc1, g1, sh2, sc2, g2 = jnp.split(mod[:, None, :], N_MOD, axis=-1)
        h = rmsnorm(x, norm1_g[l]) * (1.0 + sc1) + sh1
        x = x + g1 * hybrid_mixer(h, rope_tabs, w_in[l], na_rel_bias[l], win_sink[l], mla_q_norm_g[l],
                                  mla_kv_norm_g[l], w_uq[l], w_ukv[l], ax_q_norm_g[l], ax_k_norm_g[l],
                                  w_branch[l], w_out[l])
        h = rmsnorm(x, norm2_g[l]) * (1.0 + sc2) + sh2
        x = x + g2 * hierarchical_moe(h, w_group[l], b_group[l], w_router[l], b_router[l],
                                      w_exp1[l], w_exp3[l], w_exp2[l])
    return rmsnorm(x, final_norm_g)
```

```python
import math
from contextlib import ExitStack
import numpy as np
import concourse.bass as bass
import concourse.mybir as mybir
from concourse.bass_utils import run_bass_kernel_spmd

F32 = mybir.dt.float32
BF16 = mybir.dt.bfloat16
AF = mybir.ActivationFunctionType
ALU = mybir.AluOpType
AX = mybir.AxisListType
ENGS = ("sync", "scalar", "vector", "gpsimd", "tensor")

S = 4096
D = 1024
NT = 32
NCH = 8
CH = 512
DEPTH = 4
D_IN = 6368
EPS = 1e-6
NEG = -30000.0


class Op:
    __slots__ = ("eng", "fn", "deps", "signal", "val", "dsem", "dval")

    def __init__(self, eng, fn):
        self.eng = eng
        self.fn = fn
        self.deps = []
        self.signal = False
        self.val = None
        self.dsem = None
        self.dval = None


class Prog:
    def __init__(self, nc, n_dma_sems=56):
        self.nc = nc
        self.ops = {e: [] for e in ENGS}
        self.last_writer = {}
        self.readers = {}
        self.n_dma_sems = n_dma_sems
        self.dma_counts = [0] * n_dma_sems
        self.dma_last = [None] * n_dma_sems
        self.next_slot = 0
        self.pending = {e: [] for e in ENGS}

    def slot(self):
        s = self.next_slot
        self.next_slot += 1
        self.max_slot = max(getattr(self, "max_slot", 0), self.next_slot)
        assert s < self.n_dma_sems, "out of dma sems"
        return s

    def _dep(self, op, d):
        if d is None or d is op:
            return
        if d.dsem is None and d.eng == op.eng and op.eng == "tensor":
            return
        for x in op.deps:
            if x is d:
                return
        op.deps.append(d)
        if d.dsem is None:
            d.signal = True

    def _track(self, op, reads, writes):
        for b in reads:
            w = self.last_writer.get(b)
            if w is not None:
                self._dep(op, w)
        for b in writes:
            w = self.last_writer.get(b)
            if w is not None:
                self._dep(op, w)
            rd = self.readers.get(b)
            if rd:
                for r in rd.values():
                    self._dep(op, r)
        for b in reads:
            key = op.eng if op.dsem is None else ("d", op.dsem)
            self.readers.setdefault(b, {})[key] = op
        for b in writes:
            self.last_writer[b] = op
            self.readers[b] = {}
        pend = self.pending[op.eng]
        if pend:
            for d in pend:
                self._dep(op, d)
            self.pending[op.eng] = []

    def add(self, eng, fn, reads=(), writes=()):
        op = Op(eng, fn)
        self._track(op, reads, writes)
        self.ops[eng].append(op)
        return op

    def dma(self, eng, out, in_, sem, reads=(), writes=(), **kw):
        def fn(e, out=out, in_=in_, kw=kw):
            return e.dma_start(out=out, in_=in_, **kw)
        op = Op(eng, fn)
        op.dsem = sem
        self.dma_counts[sem] += 16
        op.dval = self.dma_counts[sem]
        self.dma_last[sem] = op
        self._track(op, reads, writes)
        self.ops[eng].append(op)
        return op

    def barrier(self):
        lasts = []
        for e in ENGS:
            for op in reversed(self.ops[e]):
                if op.dsem is None:
                    lasts.append(op)
                    break
        for o in self.dma_last:
            if o is not None:
                lasts.append(o)
        for e in ENGS:
            self.pending[e] = list(lasts)

    def emit(self, final_waits=()):
        nc = self.nc
        for e in ENGS:
            c = 0
            for op in self.ops[e]:
                if op.dsem is None and op.signal:
                    c += 1
                    op.val = c
        with ExitStack() as st:
            esem = {e: st.enter_context(nc.semaphore("s_" + e)) for e in ENGS}
            dsem = [st.enter_context(nc.semaphore("d_%d" % i)) for i in range(max(1, self.max_slot))]
            block = st.enter_context(nc.Block())

            def run(e_name):
                def body(eng):
                    waited = {}
                    for op in self.ops[e_name]:
                        for d in op.deps:
                            if d.dsem is not None:
                                key, v, s = ("d", d.dsem), d.dval, dsem[d.dsem]
                            else:
                                key, v, s = ("e", d.eng), d.val, esem[d.eng]
                            if waited.get(key, 0) >= v:
                                continue
                            waited[key] = v
                            eng.wait_ge(s, v)
                        inst = op.fn(eng)
                        if op.dsem is not None:
                            inst.then_inc(dsem[op.dsem], 16)
                        elif op.signal:
                            inst.then_inc(esem[e_name], 1)
                    if e_name == "sync":
                        for d in final_waits:
                            eng.wait_ge(dsem[d.dsem], d.dval)
                return body

            block.sync(run("sync"))
            block.scalar(run("scalar"))
            block.vector(run("vector"))
            block.gpsimd(run("gpsimd"))
            block.tensor(run("tensor"))


class Arena:
    def __init__(self, base, nwords):
        self.base = base
        self.n = nwords
        self.off = 0
        self.cnt = 0

    def mark(self):
        return self.off

    def release(self, m):
        self.off = m

    def alloc(self, free, dt=F32, parts=128):
        n = 1
        for f in free:
            n *= f
        words = n if dt == F32 else (n + 1) // 2
        words = (words + 7) // 8 * 8
        assert self.off + words <= self.n, "arena overflow %d + %d > %d" % (self.off, words, self.n)
        ap = self.base[:, self.off:self.off + (n if dt == F32 else (n + 1) // 2)]
        if dt != F32:
            ap = ap.bitcast(dt)
        if len(free) == 2:
            ap = ap.rearrange("p (a b) -> p a b", a=free[0])
        elif len(free) == 3:
            ap = ap.rearrange("p (a b c) -> p a b c", a=free[0], b=free[1])
        elif len(free) == 4:
            ap = ap.rearrange("p (a b c d) -> p a b c d", a=free[0], b=free[1], c=free[2])
        self.off += words
        self.cnt += 1
        return ap, ("sb", self.cnt)


class Rot:
    def __init__(self, P, arena, n, free, dt=F32):
        self.bufs = []
        for _ in range(n):
            ap, tok = arena.alloc(free, dt)
            self.bufs.append((ap, tok, P.slot()))
        self.i = 0

    def next(self):
        b = self.bufs[self.i % len(self.bufs)]
        self.i += 1
        return b


def _rope_np(pos, dim):
    inv = (np.float32(10000.0) ** (-(np.arange(0, dim, 2, dtype=np.float32)) / np.float32(dim))).astype(np.float32)
    ang = pos.astype(np.float32)[:, None] * inv[None, :]
    return np.cos(ang).astype(np.float32), np.sin(ang).astype(np.float32)


def host_consts():
    pos = np.arange(S)
    ct, st_ = _rope_np(pos, 32)
    cr, sr = _rope_np(pos // 64, 32)
    cc, sc = _rope_np(pos % 64, 32)
    ropeC = np.zeros((2, 96, S), np.float32)
    ropeC[0, 0:64] = 1.0
    ropeC[0, 64:80] = ct.T
    ropeC[0, 80:96] = ct.T
    ropeC[1, 64:80] = st_.T
    ropeC[1, 80:96] = st_.T
    cosD = np.concatenate([cr.T, cr.T, cc.T, cc.T], 0)
    sinD = np.concatenate([sr.T, sr.T, sc.T, sc.T], 0)
    ropeD = np.stack([np.concatenate([cosD, cosD], 0), np.concatenate([sinD, sinD], 0)], 0).astype(np.float32)
    al = np.zeros((128, 3, 4, 128), np.float32)
    slopes = 2.0 ** (-8.0 * (np.arange(4, dtype=np.float32) + 1.0) / 4.0)
    i = np.arange(128)[None, :]
    m = np.arange(128)[:, None]
    for o in range(3):
        rel = (o - 1) * 128 + m - i
        for h in range(4):
            al[:, o, h, :] = np.where(np.abs(rel) <= 128, -slopes[h] * np.abs(rel).astype(np.float32), NEG)
    e2 = np.zeros((32, 64, 128), np.float32)
    for q in range(64):
        cs = min(max(q - 8, 0), 48)
        for k in range(64):
            valid = (k >= cs) and (k < cs + 16)
            if valid:
                idx = min(max(k - q, -15), 15) + 15
                e2[idx, q, k] = 1.0
                e2[idx, q, 64 + k] = 1.0
            else:
                e2[31, q, k] = NEG
                e2[31, q, 64 + k] = NEG
    return {
        "k_ident": np.eye(128, dtype=np.float32),
        "k_ropeC": ropeC,
        "k_ropeD": ropeD,
        "k_al": al,
        "k_e2": e2,
    }


WEIGHT_SPECS = [
    ("w_ada", [DEPTH, D, 6 * D]), ("b_ada", [DEPTH, 6 * D]), ("norm1_g", [DEPTH, D]), ("norm2_g", [DEPTH, D]),
    ("w_in", [DEPTH, D, D_IN]), ("na_rel_bias", [DEPTH, 4, 15, 31]), ("win_sink", [DEPTH, 4]),
    ("mla_q_norm_g", [DEPTH, 192]), ("mla_kv_norm_g", [DEPTH, 256]), ("w_uq", [DEPTH, 192, 384]),
    ("w_ukv", [DEPTH, 256, 512]), ("ax_q_norm_g", [DEPTH, 64]), ("ax_k_norm_g", [DEPTH, 64]),
    ("w_branch", [DEPTH, 4, 256, D]), ("w_out", [DEPTH, D, D]), ("w_group", [DEPTH, D, 4]), ("b_group", [DEPTH, 4]),
    ("w_router", [DEPTH, D, 32]), ("b_router", [DEPTH, 32]), ("w_exp1", [DEPTH, 32, D, 256]),
    ("w_exp3", [DEPTH, 32, D, 256]), ("w_exp2", [DEPTH, 32, 256, D]), ("final_norm_g", [D]),
]
CONST_SPECS = [("k_ident", [128, 128]), ("k_ropeC", [2, 96, S]), ("k_ropeD", [2, 128, S]),
               ("k_al", [128, 3, 4, 128]), ("k_e2", [32, 64, 128])]


def build_program(depth=DEPTH, debug=False, stop_after=None):
    nc = bass.Bass("TRN2", target_bir_lowering=False)
    I = {}
    I["x"] = nc.dram_tensor("x", [S, D], F32, kind="ExternalInput").ap()
    I["c"] = nc.dram_tensor("c", [D], F32, kind="ExternalInput").ap()
    for name, shp in WEIGHT_SPECS + CONST_SPECS:
        if len(shp) > 1 and shp[0] == DEPTH and name not in ("k_e2",):
            shp = [depth] + list(shp[1:])
        I[name] = nc.dram_tensor(name, shp, F32, kind="ExternalInput").ap()
    y_out = nc.dram_tensor("y", [S, D], F32, kind="ExternalOutput").ap()
    skind = "ExternalOutput" if debug else "Internal"

    def scratch(name, shp, dt):
        return nc.dram_tensor(name, shp, dt, kind=skind).ap()

    XR = scratch("xres", [S, D], F32)
    QT_A = scratch("qt_a", [2, 128, S], BF16)
    KT_A = scratch("kt_a", [2, 128, S], BF16)
    QT_B = scratch("qt_b", [2, 128, S], BF16)
    KT_B = scratch("kt_b", [1, 128, S], BF16)
    QT_D = scratch("qt_d", [2, 128, S], BF16)
    KT_D = scratch("kt_d", [1, 128, S], BF16)
    QT_C = scratch("qt_c", [4, 96, S], BF16)
    KT_C = scratch("kt_c", [4, 96, S], BF16)
    V_ABD = scratch("v_abd", [S, 8, 128], BF16)
    V_C = scratch("v_c", [S, 4, 128], BF16)
    YT = scratch("yt", [4, 128, 2, S], BF16)

    P = Prog(nc)
    st = ExitStack()
    with st:
        arena_t = st.enter_context(nc.sbuf_tensor("arena", [128, 52000], F32))
        AR = Arena(arena_t, 52000)
        PS = [st.enter_context(nc.psum_tensor("ps%d" % i, [128, 512], F32)) for i in range(8)]
        PST = [("ps", i) for i in range(8)]

        def A(eng, fn, reads=(), writes=()):
            return P.add(eng, fn, reads, writes)

        def mm(out, lhsT, rhs, start, stop, reads, writes, **kw):
            return P.add("tensor", lambda e: e.matmul(out, lhsT=lhsT, rhs=rhs, start=start, stop=stop, **kw), reads, writes)

        identb, t_identb = AR.alloc([128], BF16)
        ones_f, t_ones_f = AR.alloc([128], F32)
        bd_f, t_bd_f = AR.alloc([128], F32)
        eps_c, t_eps = AR.alloc([1], F32)
        cact, t_cact = AR.alloc([8], F32)
        crep, t_crep = AR.alloc([8, 128], F32)
        mod_sb, t_mod = AR.alloc([6 * D], F32)
        G1, t_G1 = AR.alloc([D], F32)
        G2, t_G2 = AR.alloc([D], F32)
        s_c = P.slot()
        P.dma("gpsimd", identb, I["k_ident"], s_c, writes=[t_identb])
        A("vector", lambda e: e.memset(ones_f, 1.0), writes=[t_ones_f])
        A("vector", lambda e: e.memset(bd_f, 0.0), writes=[t_bd_f])
        A("vector", lambda e: e.memset(bd_f[0:64, 0:64], 1.0), writes=[t_bd_f])
        A("vector", lambda e: e.memset(bd_f[64:128, 64:128], 1.0), writes=[t_bd_f])
        A("vector", lambda e: e.memset(eps_c, EPS), writes=[t_eps])
        s_c2 = P.slot()
        P.dma("sync", cact, I["c"].rearrange("(k p) -> p k", p=128), s_c2, writes=[t_cact], allow_slow_non_contiguous=True)
        A("scalar", lambda e: e.activation(out=cact, in_=cact, func=AF.Silu), reads=[t_cact], writes=[t_cact])
        for kc in range(8):
            A("vector", lambda e, kc=kc: e.tensor_scalar(out=crep[:, kc, :], in0=ones_f, scalar1=cact[:, kc:kc + 1], scalar2=None, op0=ALU.mult),
              reads=[t_cact, t_ones_f], writes=[t_crep])
        base_mark = AR.mark()
        base_slot = P.next_slot

        out_dmas = []

        def rstd_from(ss_ap, out_ap, n, reads, writes, tmp):
            p = ss_ap.shape[0]
            A("scalar", lambda e: e.activation(out=tmp, in_=ss_ap, func=AF.Sqrt, bias=eps_c[0:p, 0:1], scale=1.0 / n), reads=list(reads) + [t_eps], writes=writes)
            A("vector", lambda e: e.reciprocal(out=out_ap, in_=tmp), reads=writes, writes=writes)

        def norm_chunk(xsrc, xtoks, ch, G, SH, gs_tok, hT, t_hT, xrot, nb):
            xts = []
            for t in range(4):
                tile = ch * 4 + t
                xt, t_xt, sl = xrot.next()
                P.dma("sync", xt, xsrc[tile * 128:(tile + 1) * 128, :], sl, reads=[(xtoks, tile)], writes=[t_xt])
                junk, t_junk = nb["junk"]
                ssq, t_ssq = nb["ss"]
                A("scalar", lambda e, xt=xt: e.activation(out=junk, in_=xt, func=AF.Square, accum_out=ssq[:, 0:1]),
                  reads=[t_xt], writes=[t_junk, t_ssq])
                rstd_from(ssq[:, 0:1], ssq[:, 1:2], float(D), [t_ssq], [t_ssq], ssq[:, 2:3])
                hf, t_hf = nb["hf"]
                A("vector", lambda e, xt=xt: e.scalar_tensor_tensor(out=hf, in0=xt, scalar=ssq[:, 1:2], in1=G, op0=ALU.mult, op1=ALU.mult),
                  reads=[t_xt, t_ssq] + gs_tok, writes=[t_hf])
                hb, t_hb = nb["hb"].next()[:2]
                A("gpsimd", lambda e, hb=hb: e.tensor_tensor(out=hb, in0=hf, in1=SH, op=ALU.add), reads=[t_hf] + gs_tok, writes=[t_hb])
                pT = PS[0][:, :].bitcast(BF16)
                for kc in range(8):
                    A("tensor", lambda e, kc=kc, hb=hb: e.transpose(out=pT[:, kc * 128:(kc + 1) * 128], in_=hb[:, kc * 128:(kc + 1) * 128], identity=identb),
                      reads=[t_hb, t_identb], writes=[PST[0]])
                A("scalar", lambda e, t=t: e.copy(out=hT[:, :, t * 128:(t + 1) * 128], in_=pT.rearrange("p (k t) -> p k t", k=8)),
                  reads=[PST[0]], writes=[t_hT])
                xts.append((xt, t_xt, sl))
            return xts

        psrot = [0]

        def ps_next(lo=1, hi=8):
            i = lo + psrot[0] % (hi - lo)
            psrot[0] += 1
            return PS[i], PST[i]

        for l in range(depth):
            xsrc = I["x"] if l == 0 else XR
            xtk = ("xin" if l == 0 else "xr")
            AR.release(base_mark)
            P.next_slot = base_slot
            P.barrier()
            m0 = AR.mark(); sl_m0 = P.next_slot
            wst = Rot(P, AR, 2, [3072], F32)
            brow, t_brow = AR.alloc([6 * D], F32)
            n1g, t_n1g = AR.alloc([D], F32)
            n2g, t_n2g = AR.alloc([D], F32)
            sb_ = P.slot()
            P.dma("sync", brow[0:1, :], I["b_ada"][l:l + 1, :], sb_, writes=[t_brow])
            sn1 = P.slot()
            P.dma("sync", n1g, I["norm1_g"][l:l + 1, :].partition_broadcast(128), sn1, writes=[t_n1g])
            sn2 = P.slot()
            P.dma("sync", n2g, I["norm2_g"][l:l + 1, :].partition_broadcast(128), sn2, writes=[t_n2g])
            for half in range(2):
                for kc in range(8):
                    w, t_w, sl = wst.next()
                    P.dma("sync", w, I["w_ada"][l, kc * 128:(kc + 1) * 128, half * 3072:(half + 1) * 3072], sl, writes=[t_w])
                    for n in range(6):
                        mm(PS[n][:, :], crep[:, kc, :], w[:, n * 512:(n + 1) * 512], kc == 0, False, [t_crep, t_w], [PST[n]])
                for n in range(6):
                    col = half * 3072 + n * 512
                    mm(PS[n][:, :], ones_f[0:1, :], brow[0:1, col:col + 512], False, True, [t_ones_f, t_brow], [PST[n]])
                    A("scalar", lambda e, n=n, col=col: e.copy(out=mod_sb[:, col:col + 512], in_=PS[n][:, :]), reads=[PST[n]], writes=[t_mod])
            A("vector", lambda e: e.scalar_tensor_tensor(out=G1, in0=mod_sb[:, D:2 * D], scalar=1.0, in1=n1g, op0=ALU.add, op1=ALU.mult),
              reads=[t_mod, t_n1g], writes=[t_G1])
            A("vector", lambda e: e.scalar_tensor_tensor(out=G2, in0=mod_sb[:, 4 * D:5 * D], scalar=1.0, in1=n2g, op0=ALU.add, op1=ALU.mult),
              reads=[t_mod, t_n2g], writes=[t_G2])
            SH1 = mod_sb[:, 0:D]
            GT1 = mod_sb[:, 2 * D:3 * D]
            SH2 = mod_sb[:, 3 * D:4 * D]
            GT2 = mod_sb[:, 5 * D:6 * D]
            AR.release(m0); P.next_slot = sl_m0
            P.barrier()

            mA = AR.mark(); sl_mA = P.next_slot
            WC = 2304
            Wfm, t_Wfm = AR.alloc([8, WC], BF16)
            Wv, t_Wv = AR.alloc([8, 512], BF16)
            win_v = I["w_in"][l].rearrange("(k p) n -> p k n", p=128)
            sw = P.slot()

            def wload(dst0, src0, w):
                P.dma("gpsimd", Wfm[:, :, dst0:dst0 + w], win_v[:, :, src0:src0 + w], sw, writes=[t_Wfm])

            O_QA, O_KA, O_QB, O_KB, O_QD, O_KD, O_CQ, O_CKV, O_KR = 0, 256, 512, 768, 896, 1408, 1664, 1856, 2112
            wload(O_QA, 0, 256)
            wload(O_KA, 256, 256)
            for g in range(2):
                for j, h in enumerate((g, g + 2)):
                    wload(O_QB + g * 128 + j * 64, 768 + h * 64, 64)
            wload(O_KB, 1024, 128)
            for g in range(2):
                for j, h in enumerate((g, g + 2)):
                    wload(O_QD + g * 256 + j * 64, 1760 + h * 64, 64)
            wload(O_KD, 2016, 128)
            wload(O_CQ, 1280, 192)
            wload(O_CKV, 1472, 256)
            A("vector", lambda e: e.memset(Wfm[:, :, O_KR:O_KR + 64], 0.0), writes=[t_Wfm])
            A("vector", lambda e: e.memset(Wfm[:, :, O_KR + 96:O_KR + 160], 0.0), writes=[t_Wfm])
            wload(O_KR + 64, 1728, 32)
            P.dma("gpsimd", Wv[:, :, 0:256], win_v[:, :, 512:768], sw, writes=[t_Wv])
            P.dma("gpsimd", Wv[:, :, 256:384], win_v[:, :, 1152:1280], sw, writes=[t_Wv])
            P.dma("gpsimd", Wv[:, :, 384:512], win_v[:, :, 2144:2272], sw, writes=[t_Wv])

            def make_rot(src_o, dst_o, nheads):
                sv = Wfm[:, :, src_o:src_o + 64 * nheads].rearrange("p k (q two s) -> p k q two s", two=2, s=16)
                dv = Wfm[:, :, dst_o:dst_o + 64 * nheads].rearrange("p k (q two s) -> p k q two s", two=2, s=16)
                for kc in range(8):
                    A("vector", lambda e, kc=kc: e.tensor_scalar(out=dv[:, kc, :, 0, :], in0=sv[:, kc, :, 1, :], scalar1=-1.0, scalar2=None, op0=ALU.mult),
                      reads=[t_Wfm], writes=[t_Wfm])
                    A("gpsimd", lambda e, kc=kc: e.tensor_copy(out=dv[:, kc, :, 1, :], in_=sv[:, kc, :, 0, :]), reads=[t_Wfm], writes=[t_Wfm])

            make_rot(O_QD, O_QD + 128, 2)
            make_rot(O_QD + 256, O_QD + 384, 2)
            make_rot(O_KD, O_KD + 128, 2)
            for kc in range(8):
                A("vector", lambda e, kc=kc: e.tensor_scalar(out=Wfm[:, kc, O_KR + 160:O_KR + 176], in0=Wfm[:, kc, O_KR + 80:O_KR + 96], scalar1=-1.0, scalar2=None, op0=ALU.mult),
                  reads=[t_Wfm], writes=[t_Wfm])
                A("gpsimd", lambda e, kc=kc: e.tensor_copy(out=Wfm[:, kc, O_KR + 176:O_KR + 192], in_=Wfm[:, kc, O_KR + 64:O_KR + 80]), reads=[t_Wfm], writes=[t_Wfm])

            wuq_f, t_wuqf = AR.alloc([2, 384], F32)
            wukv_f, t_wukvf = AR.alloc([2, 512], F32)
            gq, t_gq = AR.alloc([2], F32)
            gkv, t_gkv = AR.alloc([2], F32)
            Wuq, t_Wuq = AR.alloc([2, 4, 96], BF16)
            Wuqr, t_Wuqr = AR.alloc([2, 4, 96], BF16)
            Wukk, t_Wukk = AR.alloc([2, 4, 64], BF16)
            Wukv_v, t_Wukv = AR.alloc([2, 4, 64], BF16)
            s1 = P.slot()
            A("vector", lambda e: e.memset(wuq_f, 0.0), writes=[t_wuqf])
            A("vector", lambda e: e.memset(gq, 0.0), writes=[t_gq])
            P.dma("sync", wuq_f[:, 0, :], I["w_uq"][l, 0:128, :], s1, writes=[t_wuqf])
            P.dma("sync", wuq_f[0:64, 1, :], I["w_uq"][l, 128:192, :], s1, writes=[t_wuqf])
            P.dma("sync", wukv_f, I["w_ukv"][l].rearrange("(k p) n -> p k n", p=128), s1, writes=[t_wukvf])
            P.dma("sync", gq[:, 0:1], I["mla_q_norm_g"][l, 0:128].rearrange("(p o) -> p o", o=1), s1, writes=[t_gq])
            P.dma("sync", gq[0:64, 1:2], I["mla_q_norm_g"][l, 128:192].rearrange("(p o) -> p o", o=1), s1, writes=[t_gq])
            P.dma("sync", gkv, I["mla_kv_norm_g"][l].rearrange("(k p) -> p k", p=128), s1, writes=[t_gkv], allow_slow_non_contiguous=True)
            wuq4 = wuq_f.rearrange("p k (h c) -> p k h c", h=4)
            wukv4 = wukv_f.rearrange("p k (h c) -> p k h c", h=4)
            A("vector", lambda e: e.memset(Wuqr, 0.0), writes=[t_Wuqr])
            for k in range(2):
                A("vector", lambda e, k=k: e.tensor_scalar(out=Wuq[:, k], in0=wuq4[:, k], scalar1=gq[:, k:k + 1], scalar2=None, op0=ALU.mult),
                  reads=[t_wuqf, t_gq], writes=[t_Wuq])
                A("vector", lambda e, k=k: e.tensor_scalar(out=Wuqr[:, k, :, 64:80], in0=wuq4[:, k, :, 80:96], scalar1=gq[:, k:k + 1], scalar2=-1.0, op0=ALU.mult, op1=ALU.mult),
                  reads=[t_wuqf, t_gq], writes=[t_Wuqr])
                A("vector", lambda e, k=k: e.tensor_scalar(out=Wuqr[:, k, :, 80:96], in0=wuq4[:, k, :, 64:80], scalar1=gq[:, k:k + 1], scalar2=None, op0=ALU.mult),
                  reads=[t_wuqf, t_gq], writes=[t_Wuqr])
                A("vector", lambda e, k=k: e.tensor_scalar(out=Wukk[:, k], in0=wukv4[:, k, :, 0:64], scalar1=gkv[:, k:k + 1], scalar2=None, op0=ALU.mult),
                  reads=[t_wukvf, t_gkv], writes=[t_Wukk])
                A("vector", lambda e, k=k: e.tensor_scalar(out=Wukv_v[:, k], in0=wukv4[:, k, :, 64:128], scalar1=gkv[:, k:k + 1], scalar2=None, op0=ALU.mult),
                  reads=[t_wukvf, t_gkv], writes=[t_Wukv])
            gD, t_gD = AR.alloc([4], F32)
            for ci, nm in ((0, "ax_q_norm_g"), (2, "ax_k_norm_g")):
                for half in range(2):
                    P.dma("sync", gD[half * 64:(half + 1) * 64, ci:ci + 1], I[nm][l, :].rearrange("(p o) -> p o", o=1), s1, writes=[t_gD])
                    for blk, src in enumerate((1, 0, 3, 2)):
                        P.dma("sync", gD[half * 64 + blk * 16:half * 64 + (blk + 1) * 16, ci + 1:ci + 2],
                              I[nm][l, src * 16:(src + 1) * 16].rearrange("(p o) -> p o", o=1), s1, writes=[t_gD])
            A("vector", lambda e: e.tensor_scalar(out=gD[:, 0:2], in0=gD[:, 0:2], scalar1=0.125, scalar2=None, op0=ALU.mult), reads=[t_gD], writes=[t_gD])

            hTr = Rot(P, AR, 2, [8, CH], BF16)
            xrot = Rot(P, AR, 2, [D], F32)
            nb = {"junk": AR.alloc([D], BF16), "ss": AR.alloc([4], F32), "hf": AR.alloc([D], F32), "hb": Rot(P, AR, 2, [D], BF16)}
            stg = Rot(P, AR, 4, [CH], BF16)
            vst = Rot(P, AR, 2, [8, 128], BF16)
            vcst = Rot(P, AR, 2, [4, 128], BF16)
            for b in vst.bufs + vcst.bufs:
                A("vector", lambda e, b=b: e.memset(b[0], 1.0), writes=[b[1]])
            tabr = Rot(P, AR, 2, [2, CH], F32)
            tabc = Rot(P, AR, 2, [2, CH], F32)
            sq_a, t_sqa = AR.alloc([2, CH], F32)
            rb, t_rb = AR.alloc([CH], F32)
            rtmp, t_rtmp = AR.alloc([CH], F32)
            f1, t_f1 = AR.alloc([CH], F32)
            f2, t_f2 = AR.alloc([CH], F32)
            cqn, t_cqn = AR.alloc([2, CH], BF16)
            ckvn, t_ckvn = AR.alloc([2, CH], BF16)
            kpe, t_kpe = AR.alloc([CH], BF16)

            def fm_group(ps, col0, M, hT, t_hT, tps):
                for kc in range(8):
                    mm(ps[0:M, :], Wfm[:, kc, col0:col0 + M], hT[:, kc, :], kc == 0, kc == 7, [t_Wfm, t_hT], [tps])

            for ch in range(NCH):
                cs = slice(ch * CH, (ch + 1) * CH)
                hT, t_hT, _ = hTr.next()
                norm_chunk(xsrc, xtk, ch, G1, SH1, [t_G1, t_mod], hT, t_hT, xrot, nb)
                simple = [(O_QA, QT_A, 0, 0.125, "qa"), (O_QA + 128, QT_A, 1, 0.125, "qa"), (O_KA, KT_A, 0, 1.0, "ka"), (O_KA + 128, KT_A, 1, 1.0, "ka"),
                          (O_QB, QT_B, 0, 0.125, "qb"), (O_QB + 128, QT_B, 1, 0.125, "qb"), (O_KB, KT_B, 0, 1.0, "kb")]
                for col0, dst, gi, scl, nm in simple:
                    ps, tps = ps_next(1, 5)
                    fm_group(ps, col0, 128, hT, t_hT, tps)
                    sg, t_sg, sl = stg.next()
                    A("scalar", lambda e, ps=ps, sg=sg, scl=scl: e.mul(out=sg, in_=ps[:, :], mul=scl), reads=[tps], writes=[t_sg])
                    P.dma("gpsimd", dst[gi, :, cs], sg, sl, reads=[t_sg], writes=[(nm, gi, ch)])
                tb, t_tb, sl = tabr.next()
                P.dma("sync", tb, I["k_ropeD"][:, :, cs].rearrange("a p t -> p a t"), sl, writes=[t_tb])
                for col0, dst, gi, gc, nm in ((O_QD, QT_D, 0, 0, "qd"), (O_QD + 256, QT_D, 1, 0, "qd"), (O_KD, KT_D, 0, 2, "kd")):
                    psa, tpa = ps_next(1, 5)
                    fm_group(psa, col0, 128, hT, t_hT, tpa)
                    psb, tpb = ps_next(1, 5)
                    fm_group(psb, col0 + 128, 128, hT, t_hT, tpb)
                    A("scalar", lambda e, psa=psa: e.activation(out=sq_a[:, 0, :], in_=psa[:, :], func=AF.Square), reads=[tpa], writes=[t_sqa])
                    mm(PS[5][:, :], bd_f, sq_a[:, 0, :], True, True, [t_bd_f, t_sqa], [PST[5]])
                    rstd_from(PS[5][:, :], rb, 64.0, [PST[5]], [t_rb], rtmp)
                    A("vector", lambda e, psa=psa, gc=gc, tb=tb: e.scalar_tensor_tensor(out=f1, in0=psa[:, :], scalar=gD[:, gc:gc + 1], in1=tb[:, 0, :], op0=ALU.mult, op1=ALU.mult),
                      reads=[tpa, t_gD, t_tb], writes=[t_f1])
                    A("vector", lambda e, psb=psb, gc=gc, tb=tb: e.scalar_tensor_tensor(out=f2, in0=psb[:, :], scalar=gD[:, gc + 1:gc + 2], in1=tb[:, 1, :], op0=ALU.mult, op1=ALU.mult),
                      reads=[tpb, t_gD, t_tb], writes=[t_f2])
                    A("gpsimd", lambda e: e.tensor_tensor(out=f1, in0=f1, in1=f2, op=ALU.add), reads=[t_f1, t_f2], writes=[t_f1])
                    sg, t_sg, sl = stg.next()
                    A("vector", lambda e, sg=sg: e.tensor_tensor(out=sg, in0=f1, in1=rb, op=ALU.mult), reads=[t_f1, t_rb], writes=[t_sg])
                    P.dma("gpsimd", dst[gi, :, cs], sg, sl, reads=[t_sg], writes=[(nm, gi, ch)])
                tc, t_tc, sl = tabc.next()
                P.dma("sync", tc[0:96], I["k_ropeC"][:, :, cs].rearrange("a p t -> p a t"), sl, writes=[t_tc])
                for (col0, widths, dstn, t_dn, nfeat) in ((O_CQ, (128, 64), cqn, t_cqn, 192.0), (O_CKV, (128, 128), ckvn, t_ckvn, 256.0)):
                    pss = []
                    for k, w in enumerate(widths):
                        ps, tps = ps_next(1, 5)
                        fm_group(ps, col0 + k * 128, w, hT, t_hT, tps)
                        A("scalar", lambda e, ps=ps, k=k, w=w: e.activation(out=sq_a[0:w, k, :], in_=ps[0:w, :], func=AF.Square), reads=[tps], writes=[t_sqa])
                        pss.append((ps, tps, w))
                    for k, w in enumerate(widths):
                        mm(PS[5][:, :], ones_f[0:w, :], sq_a[0:w, k, :], k == 0, k == 1, [t_ones_f, t_sqa], [PST[5]])
                    rstd_from(PS[5][:, :], rb, nfeat, [PST[5]], [t_rb], rtmp)
                    for k, (ps, tps, w) in enumerate(pss):
                        A("vector", lambda e, ps=ps, k=k, w=w, dstn=dstn: e.tensor_tensor(out=dstn[0:w, k, :], in0=ps[0:w, :], in1=rb[0:w, :], op=ALU.mult),
                          reads=[tps, t_rb], writes=[t_dn])
                psa, tpa = ps_next(1, 5)
                fm_group(psa, O_KR, 96, hT, t_hT, tpa)
                psb, tpb = ps_next(1, 5)
                fm_group(psb, O_KR + 96, 96, hT, t_hT, tpb)
                A("vector", lambda e, psa=psa, tc=tc: e.tensor_tensor(out=f1[64:96, :], in0=psa[64:96, :], in1=tc[64:96, 0, :], op=ALU.mult), reads=[tpa, t_tc], writes=[t_f1])
                A("vector", lambda e, psb=psb, tc=tc: e.tensor_tensor(out=f2[64:96, :], in0=psb[64:96, :], in1=tc[64:96, 1, :], op=ALU.mult), reads=[tpb, t_tc], writes=[t_f2])
                A("gpsimd", lambda e: e.tensor_tensor(out=kpe[64:96, :], in0=f1[64:96, :], in1=f2[64:96, :], op=ALU.add), reads=[t_f1, t_f2], writes=[t_kpe])
                for h in range(4):
                    psa, tpa = ps_next(1, 5)
                    psb, tpb = ps_next(1, 5)
                    for k, w in enumerate((128, 64)):
                        mm(psa[0:96, :], Wuq[0:w, k, h, :], cqn[0:w, k, :], k == 0, k == 1, [t_Wuq, t_cqn], [tpa])
                    for k, w in enumerate((128, 64)):
                        mm(psb[0:96, :], Wuqr[0:w, k, h, :], cqn[0:w, k, :], k == 0, k == 1, [t_Wuqr, t_cqn], [tpb])
                    A("vector", lambda e, psa=psa, tc=tc: e.tensor_tensor(out=f1[0:96, :], in0=psa[0:96, :], in1=tc[0:96, 0, :], op=ALU.mult), reads=[tpa, t_tc], writes=[t_f1])
                    A("vector", lambda e, psb=psb, tc=tc: e.tensor_tensor(out=f2[0:96, :], in0=psb[0:96, :], in1=tc[0:96, 1, :], op=ALU.mult), reads=[tpb, t_tc], writes=[t_f2])
                    sg, t_sg, sl = stg.next()
                    A("gpsimd", lambda e, sg=sg: e.tensor_tensor(out=sg[0:96, :], in0=f1[0:96, :], in1=f2[0:96, :], op=ALU.add), reads=[t_f1, t_f2], writes=[t_sg])
                    P.dma("gpsimd", QT_C[h, :, cs], sg[0:96, :], sl, reads=[t_sg], writes=[("qc", h, ch)])
                    psk, tpk = ps_next(1, 5)
                    for k in range(2):
                        mm(psk[0:64, :], Wukk[:, k, h, :], ckvn[:, k, :], k == 0, k == 1, [t_Wukk, t_ckvn], [tpk])
                    sg, t_sg, sl = stg.next()
                    A("scalar", lambda e, sg=sg, psk=psk: e.copy(out=sg[0:64, :], in_=psk[0:64, :]), reads=[tpk], writes=[t_sg])
                    A("gpsimd", lambda e, sg=sg: e.tensor_copy(out=sg[64:96, :], in_=kpe[64:96, :]), reads=[t_kpe], writes=[t_sg])
                    P.dma("gpsimd", KT_C[h, :, cs], sg[0:96, :], sl, reads=[t_sg], writes=[("kc", h, ch)])
                for t in range(4):
                    tile = ch * 4 + t
                    ts_ = slice(t * 128, (t + 1) * 128)
                    ps, tps = ps_next(6, 8)
                    for kc in range(8):
                        mm(ps[:, :], hT[:, kc, ts_], Wv[:, kc, :], kc == 0, kc == 7, [t_hT, t_Wv], [tps])
                    vb, t_vb, sl = vst.next()
                    A("scalar", lambda e, vb=vb, ps=ps: e.copy(out=vb[:, :, 0:64], in_=ps[:, :].rearrange("p (h c) -> p h c", h=8)), reads=[tps], writes=[t_vb])
                    P.dma("gpsimd", V_ABD[tile * 128:(tile + 1) * 128], vb, sl, reads=[t_vb], writes=[("vabd", tile)])
                    ps, tps = ps_next(6, 8)
                    for k in range(2):
                        mm(ps[:, 0:256], ckvn[:, k, ts_], Wukv_v[:, k].rearrange("p h c -> p (h c)"), k == 0, k == 1, [t_ckvn, t_Wukv], [tps])
                    vb, t_vb, sl = vcst.next()
                    A("scalar", lambda e, vb=vb, ps=ps: e.copy(out=vb[:, :, 0:64], in_=ps[:, 0:256].rearrange("p (h c) -> p h c", h=4)), reads=[tps], writes=[t_vb])
                    P.dma("gpsimd", V_C[tile * 128:(tile + 1) * 128], vb, sl, reads=[t_vb], writes=[("vc", tile)])
            AR.release(mA); P.next_slot = sl_mA
            P.barrier()
            if stop_after == "A":
                break

            def finish_heads(O, tO, heads_blocks, br, tcol, rc, t_rc, yst, add_sink=None):
                n = sum(w for _, _, w in heads_blocks)
                if add_sink is not None:
                    es, t_es = add_sink
                    A("vector", lambda e: e.tensor_tensor(out=rc[64:128, 0:n].rearrange("p (h q) -> p h q", h=4),
                                                          in0=O[64:128, 0:n].rearrange("p (h q) -> p h q", h=4),
                                                          in1=es[64:128, :].unsqueeze(2).to_broadcast([64, 4, n // 4]), op=ALU.add),
                      reads=[tO, t_es], writes=[t_rc])
                    A("vector", lambda e: e.reciprocal(out=rc[64:128, 0:n], in_=rc[64:128, 0:n]), reads=[t_rc], writes=[t_rc])
                else:
                    A("vector", lambda e: e.reciprocal(out=rc[64:128, 0:n], in_=O[64:128, 0:n]), reads=[tO], writes=[t_rc])
                ys, t_ys, sl = yst.next()
                for (h, c0, w) in heads_blocks:
                    po = (h % 2) * 64
                    if len(heads_blocks) == 1:
                        oap = ys[po:po + 64, 0:w]
                    else:
                        oap = ys[po:po + 64, h // 2, 0:w]
                    A("vector", lambda e, oap=oap, c0=c0, w=w: e.tensor_tensor(out=oap, in0=O[0:64, c0:c0 + w], in1=rc[64:128, c0:c0 + w], op=ALU.mult),
                      reads=[tO, t_rc], writes=[t_ys])
                return ys, t_ys, sl

            for br in (0, 1):
                mB = AR.mark(); sl_mB = P.next_slot
                ngq = 2
                KT, t_KT = AR.alloc([2 if br == 0 else 1, S], BF16)
                QT, t_QT = AR.alloc([2, S], BF16)
                Vt, t_Vt = AR.alloc([NT, 4 if br == 0 else 2, 128], BF16)
                sl0 = P.slot()
                if br == 0:
                    for g in range(2):
                        P.dma("sync", KT[:, g, :], KT_A[g], sl0, reads=[("ka", g, c_) for c_ in range(NCH)], writes=[t_KT])
                        P.dma("sync", QT[:, g, :], QT_A[g], sl0, reads=[("qa", g, c_) for c_ in range(NCH)], writes=[t_QT])
                    for tq in range(4):
                        P.dma("sync", Vt[:, tq * 8:(tq + 1) * 8], V_ABD.rearrange("(t p) h c -> p t h c", p=128)[:, tq * 8:(tq + 1) * 8, 0:4, :], sl0, reads=[("vabd", t_) for t_ in range(NT)], writes=[t_Vt])
                else:
                    P.dma("sync", KT[:, 0, :], KT_B[0], sl0, reads=[("kb", 0, c_) for c_ in range(NCH)], writes=[t_KT])
                    for g in range(2):
                        P.dma("sync", QT[:, g, :], QT_B[g], sl0, reads=[("qb", g, c_) for c_ in range(NCH)], writes=[t_QT])
                    for tq in range(4):
                        P.dma("sync", Vt[:, tq * 8:(tq + 1) * 8], V_ABD.rearrange("(t p) h c -> p t h c", p=128)[:, tq * 8:(tq + 1) * 8, 4:6, :], sl0, reads=[("vabd", t_) for t_ in range(NT)], writes=[t_Vt])
                Sb, t_Sb = AR.alloc([4, 128], F32)
                Ptr = Rot(P, AR, 2, [4, 128], BF16)
                rc, t_rc = AR.alloc([512], F32)
                yst = Rot(P, AR, 2, [2, 128], BF16)
                if br == 0:
                    TT, t_TT = AR.alloc([60, 64], F32)
                    NEGT, t_NEGT = AR.alloc([4, 64], F32)
                    E2, t_E2 = AR.alloc([64, 128], F32)
                    rbT, t_rbT = AR.alloc([64], F32)
                    A("vector", lambda e: e.memset(NEGT, NEG), writes=[t_NEGT])
                    A("vector", lambda e: e.memset(rbT[0:32, :], 1.0), writes=[t_rbT])
                    P.dma("sync", E2[0:32], I["k_e2"], sl0, writes=[t_E2])
                    P.dma("sync", rbT[0:31, 0:60], I["na_rel_bias"][l].rearrange("h r i -> i (h r)"), sl0, reads=[t_rbT], writes=[t_rbT], allow_slow_non_contiguous=True)
                    for q0 in range(0, 64, 8):
                        ps, tps = ps_next(1, 8)
                        for q in range(8):
                            mm(ps[:, q * 64:q * 64 + 60], E2[0:32, q0 + q, :], rbT[0:32, 0:60], True, True, [t_E2, t_rbT], [tps])
                        A("vector", lambda e, ps=ps, q0=q0: e.tensor_copy(out=TT[:, :, q0:q0 + 8].rearrange("p c q -> p q c"),
                                                                         in_=ps[:, :].rearrange("p (q c) -> p q c", q=8)[:, :, 0:60]),
                          reads=[tps], writes=[t_TT])
                    TTv = TT.rearrange("p (g hf r) q -> p hf g r q", g=2, hf=2)
                else:
                    ALt, t_AL = AR.alloc([3, 4, 128], F32)
                    es, t_es = AR.alloc([4], F32)
                    P.dma("sync", ALt, I["k_al"], sl0, writes=[t_AL])
                    P.dma("sync", es, I["win_sink"][l:l + 1, :].partition_broadcast(128), sl0, writes=[t_es])
                    A("scalar", lambda e: e.activation(out=es, in_=es, func=AF.Exp), reads=[t_es], writes=[t_es])

                def rs(r):
                    return min(max(r - 4, 0), 56)

                for j in range(NT):
                    qs = slice(j * 128, (j + 1) * 128)
                    if br == 0:
                        kts = list(range(rs(2 * j) // 2, (rs(2 * j + 1) + 7) // 2 + 1))
                    else:
                        kts = [k for k in (j - 1, j, j + 1) if 0 <= k < NT]
                    O, tO = PS[6 + (j % 2)], PST[6 + (j % 2)]
                    for ki, kt in enumerate(kts):
                        ks = slice(kt * 128, (kt + 1) * 128)
                        banks = (ps_next(1, 6), ps_next(1, 6))
                        for h in range(4):
                            if br == 0:
                                half, slot = h % 2, h // 2
                                kidx = slot
                            else:
                                half, slot = h // 2, h % 2
                                kidx = 0
                            po = half * 64
                            ps_, tps_ = banks[half]
                            mm(ps_[:, slot * 128:(slot + 1) * 128], KT[po:po + 64, kidx, ks], QT[po:po + 64, slot, qs], True, True, [t_KT, t_QT], [tps_])
                        Sb5 = Sb.rearrange("p (hf g) q -> p hf g q", hf=2)
                        for half in range(2):
                            ps_, tps_ = banks[half]
                            pv = ps_[:, 0:256].rearrange("p (g q) -> p g q", g=2)
                            if br == 0:
                                for krl in range(2):
                                    for rl in range(2):
                                        r, kr = 2 * j + rl, 2 * kt + krl
                                        pp = slice(krl * 64, (krl + 1) * 64)
                                        if rs(r) <= kr < rs(r) + 8:
                                            dr = kr - r
                                            in1 = TTv[pp, half, :, dr + 7, :]
                                            rd = [t_TT]
                                        else:
                                            in1 = NEGT[pp, 0:2, :]
                                            rd = [t_NEGT]
                                        A("vector", lambda e, pp=pp, rl=rl, in1=in1, pv=pv, half=half, Sb5=Sb5: e.tensor_tensor(out=Sb5[pp, half, :, rl * 64:(rl + 1) * 64], in0=pv[pp, :, rl * 64:(rl + 1) * 64], in1=in1, op=ALU.add),
                                          reads=[tps_] + rd, writes=[t_Sb])
                            else:
                                o = kt - j + 1
                                A("vector", lambda e, pv=pv, o=o, half=half, Sb5=Sb5, ALt=ALt: e.tensor_tensor(out=Sb5[:, half, :, :], in0=pv, in1=ALt[:, o, half * 2:half * 2 + 2, :], op=ALU.add),
                                  reads=[tps_, t_AL], writes=[t_Sb])
                        Pt, t_Pt, _ = Ptr.next()
                        A("scalar", lambda e, Pt=Pt, Sb=Sb: e.activation(out=Pt, in_=Sb, func=AF.Exp), reads=[t_Sb], writes=[t_Pt])
                        for h in range(4):
                            vh = h if br == 0 else h // 2
                            hh = (h % 2) * 2 + h // 2 if br == 0 else h
                            mm(O[:, h * 128:(h + 1) * 128], Vt[:, kt, vh, :], Pt[:, hh, :], ki == 0 and h == 0, ki == len(kts) - 1, [t_Vt, t_Pt], [tO],
                               skip_group_check=True)
                    ys, t_ys, sl = finish_heads(O, tO, [(h, h * 128, 128) for h in range(4)], br, None, rc, t_rc, yst,
                                                add_sink=(es, t_es) if br == 1 else None)
                    P.dma("gpsimd", YT[br, :, :, qs], ys, sl, reads=[t_ys], writes=[("yt", br, j // 4, j % 4)])
                AR.release(mB); P.next_slot = sl_mB
                P.barrier()
                if stop_after == "attn%d" % br:
                    break
            if stop_after in ("attn0", "attn1"):
                break

            for br in (2, 3):
                mC = AR.mark(); sl_mC = P.next_slot
                sl0 = P.slot()
                if br == 2:
                    KT, t_KT = AR.alloc([4, S], BF16)
                    Vt, t_Vt = AR.alloc([NT, 4, 128], BF16)
                    for h in range(4):
                        P.dma("sync", KT[0:96, h, :], KT_C[h], sl0, reads=[("kc", h, c_) for c_ in range(NCH)], writes=[t_KT])
                    for tq in range(4):
                        P.dma("sync", Vt[:, tq * 8:(tq + 1) * 8], V_C.rearrange("(t p) h c -> p t h c", p=128)[:, tq * 8:(tq + 1) * 8], sl0, reads=[("vc", t_) for t_ in range(NT)], writes=[t_Vt])
                    scl = 96.0 ** -0.5
                else:
                    KT, t_KT = AR.alloc([1, S], BF16)
                    Vt, t_Vt = AR.alloc([NT, 2, 128], BF16)
                    P.dma("sync", KT[:, 0, :], KT_D[0], sl0, reads=[("kd", 0, c_) for c_ in range(NCH)], writes=[t_KT])
                    for tq in range(4):
                        P.dma("sync", Vt[:, tq * 8:(tq + 1) * 8], V_ABD.rearrange("(t p) h c -> p t h c", p=128)[:, tq * 8:(tq + 1) * 8, 6:8, :], sl0, reads=[("vabd", t_) for t_ in range(NT)], writes=[t_Vt])
                    scl = 1.0
                Qr = Rot(P, AR, 2, [CH], BF16)
                Ptr = Rot(P, AR, 3, [CH], BF16)
                rc, t_rc = AR.alloc([512], F32)
                yst = Rot(P, AR, 2, [CH], BF16)
                it = 0
                for h in range(4):
                    for ch in range(NCH):
                        cs = slice(ch * CH, (ch + 1) * CH)
                        Qc, t_Qc, sl = Qr.next()
                        if br == 2:
                            P.dma("sync", Qc[0:96, :], QT_C[h, :, cs], sl, reads=[("qc", h, ch)], writes=[t_Qc])
                            kd, po = 96, 0
                        else:
                            po = (h // 2) * 64
                            P.dma("sync", Qc[po:po + 64, :], QT_D[h % 2, po:po + 64, cs], sl, reads=[("qd", h % 2, ch)], writes=[t_Qc])
                            kd = 64
                        O, tO = PS[6 + (it % 2)], PST[6 + (it % 2)]
                        it += 1
                        for kt in range(NT):
                            ks = slice(kt * 128, (kt + 1) * 128)
                            ps, tps = ps_next(0, 6)
                            if br == 2:
                                mm(ps[:, :], KT[0:96, h, ks], Qc[0:96, :], True, True, [t_KT, t_Qc], [tps])
                            else:
                                mm(ps[:, :], KT[po:po + 64, 0, ks], Qc[po:po + 64, :], True, True, [t_KT, t_Qc], [tps])
                            Pt, t_Pt, _ = Ptr.next()
                            A("scalar", lambda e, Pt=Pt, ps=ps, scl=scl: e.activation(out=Pt, in_=ps[:, :], func=AF.Exp, scale=scl), reads=[tps], writes=[t_Pt])
                            vh = h if br == 2 else h // 2
                            mm(O[:, :], Vt[:, kt, vh, :], Pt, kt == 0, kt == NT - 1, [t_Vt, t_Pt], [tO])
                        ys, t_ys, sl = finish_heads(O, tO, [(h, 0, CH)], br, None, rc, t_rc, yst)
                        po2 = (h % 2) * 64
                        P.dma("gpsimd", YT[br, po2:po2 + 64, h // 2, cs], ys[po2:po2 + 64, :], sl, reads=[t_ys], writes=[("yt", br, ch, h)])
                AR.release(mC); P.next_slot = sl_mC
                P.barrier()
                if stop_after == "attn%d" % br:
                    break
            if stop_after in ("attn", "attn2", "attn3"):
                break

            mM = AR.mark(); sl_mM = P.next_slot
            Wg, t_Wg = AR.alloc([8, 4096], BF16)
            Wb, t_Wb = AR.alloc([4, 2, D], BF16)
            Wo, t_Wo = AR.alloc([8, D], BF16)
            sw = P.slot()
            for n in range(8):
                P.dma("gpsimd", Wg[:, :, n * 512:(n + 1) * 512], win_v[:, :, 2272 + n * 512:2272 + (n + 1) * 512], sw, writes=[t_Wg])
            P.dma("gpsimd", Wb, I["w_branch"][l].rearrange("n (k p) d -> p n k d", p=128), sw, writes=[t_Wb])
            P.dma("gpsimd", Wo, I["w_out"][l].rearrange("(k p) n -> p k n", p=128), sw, writes=[t_Wo])
            hTr = Rot(P, AR, 1, [8, CH], BF16)
            xrot = Rot(P, AR, 4, [D], F32)
            nb = {"junk": AR.alloc([D], BF16), "ss": AR.alloc([4], F32), "hf": AR.alloc([D], F32), "hb": Rot(P, AR, 2, [D], BF16)}
            Yr = Rot(P, AR, 1, [4, 2, CH], BF16)
            gtr = Rot(P, AR, 2, [CH], BF16)
            macc, t_macc = AR.alloc([CH], F32)
            mtmp = Rot(P, AR, 2, [CH], F32)
            mT, t_mT = AR.alloc([8, CH], BF16)
            slxo = P.slot()
            for ch in range(NCH):
                cs = slice(ch * CH, (ch + 1) * CH)
                hT, t_hT, _ = hTr.next()
                xts = norm_chunk(xsrc, xtk, ch, G1, SH1, [t_G1, t_mod], hT, t_hT, xrot, nb)
                Y, t_Y, sly = Yr.next()
                for b4 in range(4):
                    P.dma("sync", Y[:, b4], YT[b4, :, :, cs], sly, reads=[("yt", b4, ch, k_) for k_ in range(4)], writes=[t_Y])
                for dc in range(8):
                    for n in range(4):
                        pg, tpg = ps_next(0, 4)
                        for kc in range(8):
                            mm(pg[:, :], Wg[:, kc, n * D + dc * 128:n * D + (dc + 1) * 128], hT[:, kc, :], kc == 0, kc == 7, [t_Wg, t_hT], [tpg])
                        gt, t_gt, _ = gtr.next()
                        A("scalar", lambda e, gt=gt, pg=pg: e.activation(out=gt, in_=pg[:, :], func=AF.Sigmoid), reads=[tpg], writes=[t_gt])
                        pb, tpb = ps_next(0, 4)
                        for k in range(2):
                            mm(pb[:, :], Wb[:, n, k, dc * 128:(dc + 1) * 128], Y[:, n, k, :], k == 0, k == 1, [t_Wb, t_Y], [tpb])
                        if n == 0:
                            A("vector", lambda e, pb=pb, gt=gt: e.tensor_tensor(out=macc, in0=pb[:, :], in1=gt, op=ALU.mult), reads=[tpb, t_gt], writes=[t_macc])
                        else:
                            mt, t_mt, _ = mtmp.next()
                            A("vector", lambda e, pb=pb, gt=gt, mt=mt: e.tensor_tensor(out=mt, in0=pb[:, :], in1=gt, op=ALU.mult), reads=[tpb, t_gt], writes=[t_mt])
                            if n < 3:
                                A("gpsimd", lambda e, mt=mt: e.tensor_tensor(out=macc, in0=macc, in1=mt, op=ALU.add), reads=[t_macc, t_mt], writes=[t_macc])
                            else:
                                A("gpsimd", lambda e, mt=mt, dc=dc: e.tensor_tensor(out=mT[:, dc, :], in0=macc, in1=mt, op=ALU.add), reads=[t_macc, t_mt], writes=[t_mT])
                for t in range(4):
                    tile = ch * 4 + t
                    xt, t_xt, slx = xts[t]
                    xn, t_xn = xt, t_xt
                    for nh in range(2):
                        po_, tpo = ps_next(4, 8)
                        for dc in range(8):
                            mm(po_[:, :], mT[:, dc, t * 128:(t + 1) * 128], Wo[:, dc, nh * 512:(nh + 1) * 512], dc == 0, dc == 7, [t_mT, t_Wo], [tpo])
                        mt, t_mt, _ = mtmp.next()
                        A("vector", lambda e, po_=po_, mt=mt, nh=nh: e.tensor_tensor(out=mt, in0=po_[:, :], in1=GT1[:, nh * 512:(nh + 1) * 512], op=ALU.mult),
                          reads=[tpo, t_mod], writes=[t_mt])
                        A("gpsimd", lambda e, xn=xn, xt=xt, mt=mt, nh=nh: e.tensor_tensor(out=xn[:, nh * 512:(nh + 1) * 512], in0=xt[:, nh * 512:(nh + 1) * 512], in1=mt, op=ALU.add),
                          reads=[t_xt, t_mt], writes=[t_xn])
                    P.dma("gpsimd", XR[tile * 128:(tile + 1) * 128, :], xn, slx, reads=[t_xn, ("xr", tile)], writes=[("xr", tile), ("xm", tile)])
            AR.release(mM); P.next_slot = sl_mM
            P.barrier()
            if stop_after == "merge":
                break

            mF = AR.mark(); sl_mF = P.next_slot
            SC = 1024
            h2, t_h2 = AR.alloc([8, SC], BF16)
            acc, t_acc = AR.alloc([8, D], F32)
            comb, t_comb = AR.alloc([8, 32], F32)
            Wr, t_Wr = AR.alloc([8, 36], BF16)
            brt, t_brt = AR.alloc([36], F32)
            sw = P.slot()
            P.dma("gpsimd", Wr[:, :, 0:4], I["w_group"][l].rearrange("(k p) n -> p k n", p=128), sw, writes=[t_Wr])
            P.dma("gpsimd", Wr[:, :, 4:36], I["w_router"][l].rearrange("(k p) n -> p k n", p=128), sw, writes=[t_Wr])
            swb = P.slot()
            P.dma("sync", brt[:, 0:4], I["b_group"][l:l + 1, :].partition_broadcast(128), swb, writes=[t_brt])
            P.dma("sync", brt[:, 4:36], I["b_router"][l:l + 1, :].partition_broadcast(128), swb, writes=[t_brt])
            hTr = Rot(P, AR, 1, [8, CH], BF16)
            xrot = Rot(P, AR, 2, [D], F32)
            nb = {"junk": AR.alloc([D], BF16), "ss": AR.alloc([4], F32), "hf": AR.alloc([D], F32), "hb": Rot(P, AR, 2, [D], BF16)}
            W1r = Rot(P, AR, 2, [8, 256], BF16)
            W3r = Rot(P, AR, 2, [8, 256], BF16)
            W2r = Rot(P, AR, 2, [2, D], BF16)
            slr = Rot(P, AR, 2, [CH], F32)
            hidr = Rot(P, AR, 2, [2, CH], BF16)
            rt, t_rt = AR.alloc([160], F32)
            xo = Rot(P, AR, 2, [D], F32)
            gfin, t_gfin = AR.alloc([D], F32)
            if l == depth - 1:
                P.dma("sync", gfin, I["final_norm_g"].rearrange("(o d) -> o d", o=1).partition_broadcast(128), swb, writes=[t_gfin])
            for sc in range(4):
                for c4 in range(2):
                    ch = sc * 2 + c4
                    hT, t_hT, _ = hTr.next()
                    norm_chunk(XR, "xm", ch, G2, SH2, [t_G2, t_mod], hT, t_hT, xrot, nb)
                    A("gpsimd", lambda e, c4=c4, hT=hT: e.tensor_copy(out=h2[:, :, c4 * CH:(c4 + 1) * CH], in_=hT), reads=[t_hT], writes=[t_h2])
                    for t in range(4):
                        lt = c4 * 4 + t
                        ps, tps = ps_next(0, 4)
                        for kc in range(8):
                            mm(ps[:, 0:36], hT[:, kc, t * 128:(t + 1) * 128], Wr[:, kc, :], kc == 0, kc == 7, [t_hT, t_Wr], [tps])
                        lg = rt[:, 0:36]
                        V_ = "vector"
                        R = [t_rt]
                        A(V_, lambda e, ps=ps: e.tensor_tensor(out=lg, in0=ps[:, 0:36], in1=brt, op=ALU.add), reads=[tps, t_brt], writes=R)
                        gmax, ngm, gsum, gw = rt[:, 36:37], rt[:, 37:38], rt[:, 38:39], rt[:, 39:40]
                        goh, pen, ge = rt[:, 40:44], rt[:, 44:48], rt[:, 48:52]
                        el2 = rt[:, 52:84]
                        oh1, el3, oh2 = rt[:, 84:116], rt[:, 116:148], rt[:, 52:84]
                        m1, m2, dd, ee, w1, w2 = (rt[:, 148 + i:149 + i] for i in range(6))
                        A(V_, lambda e: e.reduce_max(out=gmax, in_=lg[:, 0:4], axis=AX.X), reads=R, writes=R)
                        A(V_, lambda e: e.tensor_scalar(out=ngm, in0=gmax, scalar1=-1.0, scalar2=None, op0=ALU.mult), reads=R, writes=R)
                        A(V_, lambda e: e.tensor_scalar(out=goh, in0=lg[:, 0:4], scalar1=gmax, scalar2=None, op0=ALU.is_ge), reads=R, writes=R)
                        A("scalar", lambda e: e.activation(out=ge, in_=lg[:, 0:4], func=AF.Exp, bias=ngm, scale=1.0, accum_out=gsum), reads=R, writes=R)
                        A(V_, lambda e: e.reciprocal(out=gw, in_=gsum), reads=R, writes=R)
                        A(V_, lambda e: e.tensor_scalar(out=pen, in0=goh, scalar1=-1.0, scalar2=1.0e9, op0=ALU.add, op1=ALU.mult), reads=R, writes=R)
                        A(V_, lambda e: e.tensor_tensor(out=el2.rearrange("p (g x) -> p g x", g=4), in0=lg[:, 4:36].rearrange("p (g x) -> p g x", g=4),
                                                        in1=pen.unsqueeze(2).to_broadcast([128, 4, 8]), op=ALU.add), reads=R, writes=R)
                        A(V_, lambda e: e.reduce_max(out=m1, in_=el2, axis=AX.X), reads=R, writes=R)
                        A(V_, lambda e: e.tensor_scalar(out=oh1, in0=el2, scalar1=m1, scalar2=None, op0=ALU.is_ge), reads=R, writes=R)
                        A(V_, lambda e: e.scalar_tensor_tensor(out=el3, in0=oh1, scalar=-1.0e9, in1=el2, op0=ALU.mult, op1=ALU.add), reads=R, writes=R)
                        A(V_, lambda e: e.reduce_max(out=m2, in_=el3, axis=AX.X), reads=R, writes=R)
                        A(V_, lambda e: e.tensor_scalar(out=oh2, in0=el3, scalar1=m2, scalar2=None, op0=ALU.is_ge), reads=R, writes=R)
                        A(V_, lambda e: e.tensor_tensor(out=dd, in0=m2, in1=m1, op=ALU.subtract), reads=R, writes=R)
                        A("scalar", lambda e: e.activation(out=ee, in_=dd, func=AF.Exp), reads=R, writes=R)
                        A(V_, lambda e: e.tensor_scalar(out=w1, in0=ee, scalar1=1.0, scalar2=None, op0=ALU.add), reads=R, writes=R)
                        A(V_, lambda e: e.reciprocal(out=w1, in_=w1), reads=R, writes=R)
                        A(V_, lambda e: e.tensor_tensor(out=w2, in0=ee, in1=w1, op=ALU.mult), reads=R, writes=R)
                        A(V_, lambda e: e.tensor_tensor(out=w1, in0=w1, in1=gw, op=ALU.mult), reads=R, writes=R)
                        A(V_, lambda e: e.tensor_tensor(out=w2, in0=w2, in1=gw, op=ALU.mult), reads=R, writes=R)
                        A(V_, lambda e, lt=lt: e.tensor_scalar(out=comb[:, lt, :], in0=oh1, scalar1=w1, scalar2=None, op0=ALU.mult), reads=R, writes=[t_comb])
                        A(V_, lambda e, lt=lt: e.scalar_tensor_tensor(out=comb[:, lt, :], in0=oh2, scalar=w2, in1=comb[:, lt, :], op0=ALU.mult, op1=ALU.add), reads=R + [t_comb], writes=[t_comb])
                for ex in range(32):
                    W1, t_W1, s1_ = W1r.next()
                    W3, t_W3, s3_ = W3r.next()
                    W2, t_W2, s2_ = W2r.next()
                    P.dma("gpsimd", W1, I["w_exp1"][l, ex].rearrange("(k p) n -> p k n", p=128), s1_, writes=[t_W1])
                    P.dma("gpsimd", W3, I["w_exp3"][l, ex].rearrange("(k p) n -> p k n", p=128), s3_, writes=[t_W3])
                    P.dma("gpsimd", W2, I["w_exp2"][l, ex].rearrange("(k p) n -> p k n", p=128), s2_, writes=[t_W2])
                    for c4 in range(2):
                        hid, t_hid, _ = hidr.next()
                        for fh in range(2):
                            p1, tp1 = ps_next(0, 4)
                            for kc in range(8):
                                mm(p1[:, :], W1[:, kc, fh * 128:(fh + 1) * 128], h2[:, kc, c4 * CH:(c4 + 1) * CH], kc == 0, kc == 7, [t_W1, t_h2], [tp1])
                            p3, tp3 = ps_next(0, 4)
                            for kc in range(8):
                                mm(p3[:, :], W3[:, kc, fh * 128:(fh + 1) * 128], h2[:, kc, c4 * CH:(c4 + 1) * CH], kc == 0, kc == 7, [t_W3, t_h2], [tp3])
                            sl_, t_sl, _ = slr.next()
                            A("scalar", lambda e, sl_=sl_, p1=p1: e.activation(out=sl_, in_=p1[:, :], func=AF.Silu), reads=[tp1], writes=[t_sl])
                            A("vector", lambda e, hid=hid, fh=fh, p3=p3, sl_=sl_: e.tensor_tensor(out=hid[:, fh, :], in0=p3[:, :], in1=sl_, op=ALU.mult), reads=[tp3, t_sl], writes=[t_hid])
                        for t in range(4):
                            lt = c4 * 4 + t
                            for nh in range(2):
                                po_, tpo = ps_next(4, 8)
                                for fh in range(2):
                                    mm(po_[:, :], hid[:, fh, t * 128:(t + 1) * 128], W2[:, fh, nh * 512:(nh + 1) * 512], fh == 0, fh == 1, [t_hid, t_W2], [tpo])
                                asl = acc[:, lt, nh * 512:(nh + 1) * 512]
                                if ex == 0:
                                    A("vector", lambda e, po_=po_, asl=asl, lt=lt: e.tensor_scalar(out=asl, in0=po_[:, :], scalar1=comb[:, lt, 0:1], scalar2=None, op0=ALU.mult),
                                      reads=[tpo, t_comb], writes=[(t_acc, lt)])
                                else:
                                    A("vector", lambda e, po_=po_, asl=asl, lt=lt, ex=ex: e.scalar_tensor_tensor(out=asl, in0=po_[:, :], scalar=comb[:, lt, ex:ex + 1], in1=asl, op0=ALU.mult, op1=ALU.add),
                                      reads=[tpo, t_comb, (t_acc, lt)], writes=[(t_acc, lt)])
                for lt in range(8):
                    tile = sc * 8 + lt
                    xt, t_xt, slx = xrot.next()
                    P.dma("sync", xt, XR[tile * 128:(tile + 1) * 128, :], slx, reads=[("xm", tile)], writes=[t_xt])
                    xn, t_xn, slo = xo.next()
                    A("gpsimd", lambda e, lt=lt, xn=xn: e.tensor_tensor(out=xn, in0=acc[:, lt, :], in1=GT2, op=ALU.mult), reads=[(t_acc, lt), t_mod], writes=[t_xn])
                    A("gpsimd", lambda e, xn=xn, xt=xt: e.tensor_tensor(out=xn, in0=xn, in1=xt, op=ALU.add), reads=[t_xn, t_xt], writes=[t_xn])
                    if l < depth - 1:
                        P.dma("sync", XR[tile * 128:(tile + 1) * 128, :], xn, slo, reads=[t_xn, ("xm", tile)], writes=[("xr", tile)])
                    else:
                        junk, t_junk = nb["junk"]
                        ssq, t_ssq = nb["ss"]
                        A("scalar", lambda e, xn=xn: e.activation(out=junk, in_=xn, func=AF.Square, accum_out=ssq[:, 0:1]), reads=[t_xn], writes=[t_junk, t_ssq])
                        rstd_from(ssq[:, 0:1], ssq[:, 1:2], float(D), [t_ssq], [t_ssq], ssq[:, 2:3])
                        A("vector", lambda e, xn=xn: e.scalar_tensor_tensor(out=xn, in0=xn, scalar=ssq[:, 1:2], in1=gfin, op0=ALU.mult, op1=ALU.mult),
                          reads=[t_xn, t_ssq, t_gfin], writes=[t_xn])
                        out_dmas.append(P.dma("sync", y_out[tile * 128:(tile + 1) * 128, :], xn, slo, reads=[t_xn], writes=[("y", tile)]))
            AR.release(mF); P.next_slot = sl_mF
            P.barrier()

        if not out_dmas:
            d0, t_d0 = AR.alloc([8], F32)
            A("vector", lambda e: e.memset(d0, 0.0), writes=[t_d0])
            out_dmas.append(P.dma("sync", y_out[0:128, 0:8], d0, P.slot(), reads=[t_d0]))
        finals = [o for o in P.dma_last if o is not None]
        P.emit(final_waits=finals)
    return nc


_CACHE = {}


def kernel(**inputs):
    if "nc" not in _CACHE:
        _CACHE["nc"] = build_program()
        _CACHE["consts"] = host_consts()
    nc = _CACHE["nc"]
    consts = _CACHE["consts"]
    x = np.ascontiguousarray(np.asarray(inputs["x"], dtype=np.float32))
    c = np.ascontiguousarray(np.asarray(inputs["c"], dtype=np.float32))
    shared = {name: np.ascontiguousarray(np.asarray(inputs[name], dtype=np.float32)) for name, _ in WEIGHT_SPECS}
    shared.update(consts)
    in_maps = []
    for b in range(8):
        m = dict(shared)
        m["x"] = x[b]
        m["c"] = c[b]
        in_maps.append(m)
    res = run_bass_kernel_spmd(nc, in_maps, core_ids=list(range(8)))
    out = np.stack([np.asarray(res.results[b]["y"], dtype=np.float32) for b in range(8)], axis=0)
    return out
```

```python
import math
from contextlib import ExitStack
import numpy as np
import concourse.bass as bass
import concourse.mybir as mybir
from concourse.bass_utils import run_bass_kernel_spmd

F32 = mybir.dt.float32
BF16 = mybir.dt.bfloat16
AF = mybir.ActivationFunctionType
ALU = mybir.AluOpType
AX = mybir.AxisListType
ENGS = ("sync", "scalar", "vector", "gpsimd", "tensor")

S = 4096
D = 1024
NT = 32
NCH = 8
CH = 512
DEPTH = 4
D_IN = 6368
EPS = 1e-6
NEG = -30000.0


class Op:
    __slots__ = ("eng", "fn", "deps", "signal", "val", "dsem", "dval")

    def __init__(self, eng, fn):
        self.eng = eng
        self.fn = fn
        self.deps = []
        self.signal = False
        self.val = None
        self.dsem = None
        self.dval = None


class Prog:
    def __init__(self, nc, n_dma_sems=56):
        self.nc = nc
        self.ops = {e: [] for e in ENGS}
        self.last_writer = {}
        self.readers = {}
        self.n_dma_sems = n_dma_sems
        self.dma_counts = [0] * n_dma_sems
        self.dma_last = [None] * n_dma_sems
        self.next_slot = 0
        self.pending = {e: [] for e in ENGS}

    def slot(self):
        s = self.next_slot
        self.next_slot += 1
        self.max_slot = max(getattr(self, "max_slot", 0), self.next_slot)
        assert s < self.n_dma_sems, "out of dma sems"
        return s

    def _dep(self, op, d):
        if d is None or d is op:
            return
        if d.dsem is None and d.eng == op.eng and op.eng == "tensor":
            return
        for x in op.deps:
            if x is d:
                return
        op.deps.append(d)
        if d.dsem is None:
            d.signal = True

    def _track(self, op, reads, writes):
        for b in reads:
            w = self.last_writer.get(b)
            if w is not None:
                self._dep(op, w)
        for b in writes:
            w = self.last_writer.get(b)
            if w is not None:
                self._dep(op, w)
            rd = self.readers.get(b)
            if rd:
                for r in rd.values():
                    self._dep(op, r)
        for b in reads:
            key = op.eng if op.dsem is None else ("d", op.dsem)
            self.readers.setdefault(b, {})[key] = op
        for b in writes:
            self.last_writer[b] = op
            self.readers[b] = {}
        pend = self.pending[op.eng]
        if pend:
            for d in pend:
                self._dep(op, d)
            self.pending[op.eng] = []

    def add(self, eng, fn, reads=(), writes=()):
        op = Op(eng, fn)
        self._track(op, reads, writes)
        self.ops[eng].append(op)
        return op

    def dma(self, eng, out, in_, sem, reads=(), writes=(), **kw):
        def fn(e, out=out, in_=in_, kw=kw):
            return e.dma_start(out=out, in_=in_, **kw)
        op = Op(eng, fn)
        op.dsem = sem
        self.dma_counts[sem] += 16
        op.dval = self.dma_counts[sem]
        self.dma_last[sem] = op
        self._track(op, reads, writes)
        self.ops[eng].append(op)
        return op

    def barrier(self):
        lasts = []
        for e in ENGS:
            for op in reversed(self.ops[e]):
                if op.dsem is None:
                    lasts.append(op)
                    break
        for o in self.dma_last:
            if o is not None:
                lasts.append(o)
        for e in ENGS:
            self.pending[e] = list(lasts)

    def emit(self, final_waits=()):
        nc = self.nc
        for e in ENGS:
            c = 0
            for op in self.ops[e]:
                if op.dsem is None and op.signal:
                    c += 1
                    op.val = c
        with ExitStack() as st:
            esem = {e: st.enter_context(nc.semaphore("s_" + e)) for e in ENGS}
            dsem = [st.enter_context(nc.semaphore("d_%d" % i)) for i in range(max(1, self.max_slot))]
            block = st.enter_context(nc.Block())

            def run(e_name):
                def body(eng):
                    waited = {}
                    for op in self.ops[e_name]:
                        for d in op.deps:
                            if d.dsem is not None:
                                key, v, s = ("d", d.dsem), d.dval, dsem[d.dsem]
                            else:
                                key, v, s = ("e", d.eng), d.val, esem[d.eng]
                            if waited.get(key, 0) >= v:
                                continue
                            waited[key] = v
                            eng.wait_ge(s, v)
                        inst = op.fn(eng)
                        if op.dsem is not None:
                            inst.then_inc(dsem[op.dsem], 16)
                        elif op.signal:
                            inst.then_inc(esem[e_name], 1)
                    if e_name == "sync":
                        for d in final_waits:
                            eng.wait_ge(dsem[d.dsem], d.dval)
                return body

            block.sync(run("sync"))
            block.scalar(run("scalar"))
            block.vector(run("vector"))
            block.gpsimd(run("gpsimd"))
            block.tensor(run("tensor"))


class Arena:
    def __init__(self, base, nwords):
        self.base = base
        self.n = nwords
        self.off = 0
        self.cnt = 0

    def mark(self):
        return self.off

    def release(self, m):
        self.off = m

    def alloc(self, free, dt=F32, parts=128):
        n = 1
        for f in free:
            n *= f
        words = n if dt == F32 else (n + 1) // 2
        words = (words + 7) // 8 * 8
        assert self.off + words <= self.n, "arena overflow %d + %d > %d" % (self.off, words, self.n)
        ap = self.base[:, self.off:self.off + (n if dt == F32 else (n + 1) // 2)]
        if dt != F32:
            ap = ap.bitcast(dt)
        if len(free) == 2:
            ap = ap.rearrange("p (a b) -> p a b", a=free[0])
        elif len(free) == 3:
            ap = ap.rearrange("p (a b c) -> p a b c", a=free[0], b=free[1])
        elif len(free) == 4:
            ap = ap.rearrange("p (a b c d) -> p a b c d", a=free[0], b=free[1], c=free[2])
        self.off += words
        self.cnt += 1
        return ap, ("sb", self.cnt)


class Rot:
    def __init__(self, P, arena, n, free, dt=F32):
        self.bufs = []
        for _ in range(n):
            ap, tok = arena.alloc(free, dt)
            self.bufs.append((ap, tok, P.slot()))
        self.i = 0

    def next(self):
        b = self.bufs[self.i % len(self.bufs)]
        self.i += 1
        return b


def _rope_np(pos, dim):
    inv = (np.float32(10000.0) ** (-(np.arange(0, dim, 2, dtype=np.float32)) / np.float32(dim))).astype(np.float32)
    ang = pos.astype(np.float32)[:, None] * inv[None, :]
    return np.cos(ang).astype(np.float32), np.sin(ang).astype(np.float32)


def host_consts():
    pos = np.arange(S)
    ct, st_ = _rope_np(pos, 32)
    cr, sr = _rope_np(pos // 64, 32)
    cc, sc = _rope_np(pos % 64, 32)
    ropeC = np.zeros((2, 96, S), np.float32)
    ropeC[0, 0:64] = 1.0
    ropeC[0, 64:80] = ct.T
    ropeC[0, 80:96] = ct.T
    ropeC[1, 64:80] = st_.T
    ropeC[1, 80:96] = st_.T
    cosD = np.concatenate([cr.T, cr.T, cc.T, cc.T], 0)
    sinD = np.concatenate([sr.T, sr.T, sc.T, sc.T], 0)
    ropeD = np.stack([np.concatenate([cosD, cosD], 0), np.concatenate([sinD, sinD], 0)], 0).astype(np.float32)
    al = np.zeros((128, 3, 4, 128), np.float32)
    slopes = 2.0 ** (-8.0 * (np.arange(4, dtype=np.float32) + 1.0) / 4.0)
    i = np.arange(128)[None, :]
    m = np.arange(128)[:, None]
    for o in range(3):
        rel = (o - 1) * 128 + m - i
        for h in range(4):
            al[:, o, h, :] = np.where(np.abs(rel) <= 128, -slopes[h] * np.abs(rel).astype(np.float32), NEG)
    e2 = np.zeros((32, 64, 128), np.float32)
    for q in range(64):
        cs = min(max(q - 8, 0), 48)
        for k in range(64):
            valid = (k >= cs) and (k < cs + 16)
            if valid:
                idx = min(max(k - q, -15), 15) + 15
                e2[idx, q, k] = 1.0
                e2[idx, q, 64 + k] = 1.0
            else:
                e2[31, q, k] = NEG
                e2[31, q, 64 + k] = NEG
    return {
        "k_ident": np.eye(128, dtype=np.float32),
        "k_ropeC": ropeC,
        "k_ropeD": ropeD,
        "k_al": al,
        "k_e2": e2,
    }


WEIGHT_SPECS = [
    ("w_ada", [DEPTH, D, 6 * D]), ("b_ada", [DEPTH, 6 * D]), ("norm1_g", [DEPTH, D]), ("norm2_g", [DEPTH, D]),
    ("w_in", [DEPTH, D, D_IN]), ("na_rel_bias", [DEPTH, 4, 15, 31]), ("win_sink", [DEPTH, 4]),
    ("mla_q_norm_g", [DEPTH, 192]), ("mla_kv_norm_g", [DEPTH, 256]), ("w_uq", [DEPTH, 192, 384]),
    ("w_ukv", [DEPTH, 256, 512]), ("ax_q_norm_g", [DEPTH, 64]), ("ax_k_norm_g", [DEPTH, 64]),
    ("w_branch", [DEPTH, 4, 256, D]), ("w_out", [DEPTH, D, D]), ("w_group", [DEPTH, D, 4]), ("b_group", [DEPTH, 4]),
    ("w_router", [DEPTH, D, 32]), ("b_router", [DEPTH, 32]), ("w_exp1", [DEPTH, 32, D, 256]),
    ("w_exp3", [DEPTH, 32, D, 256]), ("w_exp2", [DEPTH, 32, 256, D]), ("final_norm_g", [D]),
]
CONST_SPECS = [("k_ident", [128, 128]), ("k_ropeC", [2, 96, S]), ("k_ropeD", [2, 128, S]),
               ("k_al", [128, 3, 4, 128]), ("k_e2", [32, 64, 128])]


def build_program(depth=DEPTH, debug=False, stop_after=None):
    nc = bass.Bass("TRN2", target_bir_lowering=False)
    I = {}
    I["x"] = nc.dram_tensor("x", [S, D], F32, kind="ExternalInput").ap()
    I["c"] = nc.dram_tensor("c", [D], F32, kind="ExternalInput").ap()
    for name, shp in WEIGHT_SPECS + CONST_SPECS:
        if len(shp) > 1 and shp[0] == DEPTH and name not in ("k_e2",):
            shp = [depth] + list(shp[1:])
        I[name] = nc.dram_tensor(name, shp, F32, kind="ExternalInput").ap()
    y_out = nc.dram_tensor("y", [S, D], F32, kind="ExternalOutput").ap()
    skind = "ExternalOutput" if debug else "Internal"

    def scratch(name, shp, dt):
        return nc.dram_tensor(name, shp, dt, kind=skind).ap()

    XR = scratch("xres", [S, D], F32)
    QT_A = scratch("qt_a", [2, 128, S], BF16)
    KT_A = scratch("kt_a", [2, 128, S], BF16)
    QT_B = scratch("qt_b", [2, 128, S], BF16)
    KT_B = scratch("kt_b", [1, 128, S], BF16)
    QT_D = scratch("qt_d", [2, 128, S], BF16)
    KT_D = scratch("kt_d", [1, 128, S], BF16)
    QT_C = scratch("qt_c", [4, 96, S], BF16)
    KT_C = scratch("kt_c", [4, 96, S], BF16)
    V_ABD = scratch("v_abd", [S, 8, 128], BF16)
    V_C = scratch("v_c", [S, 4, 128], BF16)
    YT = scratch("yt", [4, 128, 2, S], BF16)

    P = Prog(nc)
    st = ExitStack()
    with st:
        arena_t = st.enter_context(nc.sbuf_tensor("arena", [128, 52000], F32))
        AR = Arena(arena_t, 52000)
        PS = [st.enter_context(nc.psum_tensor("ps%d" % i, [128, 512], F32)) for i in range(8)]
        PST = [("ps", i) for i in range(8)]

        def A(eng, fn, reads=(), writes=()):
            return P.add(eng, fn, reads, writes)

        def mm(out, lhsT, rhs, start, stop, reads, writes, **kw):
            return P.add("tensor", lambda e: e.matmul(out, lhsT=lhsT, rhs=rhs, start=start, stop=stop, **kw), reads, writes)

        identb, t_identb = AR.alloc([128], BF16)
        ones_f, t_ones_f = AR.alloc([128], F32)
        bd_f, t_bd_f = AR.alloc([128], F32)
        eps_c, t_eps = AR.alloc([1], F32)
        cact, t_cact = AR.alloc([8], F32)
        crep, t_crep = AR.alloc([8, 128], F32)
        mod_sb, t_mod = AR.alloc([6 * D], F32)
        G1, t_G1 = AR.alloc([D], F32)
        G2, t_G2 = AR.alloc([D], F32)
        s_c = P.slot()
        P.dma("gpsimd", identb, I["k_ident"], s_c, writes=[t_identb])
        A("vector", lambda e: e.memset(ones_f, 1.0), writes=[t_ones_f])
        A("vector", lambda e: e.memset(bd_f, 0.0), writes=[t_bd_f])
        A("vector", lambda e: e.memset(bd_f[0:64, 0:64], 1.0), writes=[t_bd_f])
        A("vector", lambda e: e.memset(bd_f[64:128, 64:128], 1.0), writes=[t_bd_f])
        A("vector", lambda e: e.memset(eps_c, EPS), writes=[t_eps])
        s_c2 = P.slot()
        P.dma("sync", cact, I["c"].rearrange("(k p) -> p k", p=128), s_c2, writes=[t_cact], allow_slow_non_contiguous=True)
        A("scalar", lambda e: e.activation(out=cact, in_=cact, func=AF.Silu), reads=[t_cact], writes=[t_cact])
        for kc in range(8):
            A("vector", lambda e, kc=kc: e.tensor_scalar(out=crep[:, kc, :], in0=ones_f, scalar1=cact[:, kc:kc + 1], scalar2=None, op0=ALU.mult),
              reads=[t_cact, t_ones_f], writes=[t_crep])
        base_mark = AR.mark()
        base_slot = P.next_slot

        out_dmas = []

        def rstd_from(ss_ap, out_ap, n, reads, writes, tmp):
            p = ss_ap.shape[0]
            A("scalar", lambda e: e.activation(out=tmp, in_=ss_ap, func=AF.Sqrt, bias=eps_c[0:p, 0:1], scale=1.0 / n), reads=list(reads) + [t_eps], writes=writes)
            A("vector", lambda e: e.reciprocal(out=out_ap, in_=tmp), reads=writes, writes=writes)

        def norm_chunk(xsrc, xtoks, ch, G, SH, gs_tok, hT, t_hT, xrot, nb):
            xts = []
            for t in range(4):
                tile = ch * 4 + t
                xt, t_xt, sl = xrot.next()
                P.dma("sync", xt, xsrc[tile * 128:(tile + 1) * 128, :], sl, reads=[(xtoks, tile)], writes=[t_xt])
                junk, t_junk = nb["junk"]
                ssq, t_ssq = nb["ss"]
                A("scalar", lambda e, xt=xt: e.activation(out=junk, in_=xt, func=AF.Square, accum_out=ssq[:, 0:1]),
                  reads=[t_xt], writes=[t_junk, t_ssq])
                rstd_from(ssq[:, 0:1], ssq[:, 1:2], float(D), [t_ssq], [t_ssq], ssq[:, 2:3])
                hf, t_hf = nb["hf"]
                A("vector", lambda e, xt=xt: e.scalar_tensor_tensor(out=hf, in0=xt, scalar=ssq[:, 1:2], in1=G, op0=ALU.mult, op1=ALU.mult),
                  reads=[t_xt, t_ssq] + gs_tok, writes=[t_hf])
                hb, t_hb = nb["hb"].next()[:2]
                A("gpsimd", lambda e, hb=hb: e.tensor_tensor(out=hb, in0=hf, in1=SH, op=ALU.add), reads=[t_hf] + gs_tok, writes=[t_hb])
                pT = PS[0][:, :].bitcast(BF16)
                for kc in range(8):
                    A("tensor", lambda e, kc=kc, hb=hb: e.transpose(out=pT[:, kc * 128:(kc + 1) * 128], in_=hb[:, kc * 128:(kc + 1) * 128], identity=identb),
                      reads=[t_hb, t_identb], writes=[PST[0]])
                A("scalar", lambda e, t=t: e.copy(out=hT[:, :, t * 128:(t + 1) * 128], in_=pT.rearrange("p (k t) -> p k t", k=8)),
                  reads=[PST[0]], writes=[t_hT])
                xts.append((xt, t_xt, sl))
            return xts

        psrot = [0]

        def ps_next(lo=1, hi=8):
            i = lo + psrot[0] % (hi - lo)
            psrot[0] += 1
            return PS[i], PST[i]

        for l in range(depth):
            xsrc = I["x"] if l == 0 else XR
            xtk = ("xin" if l == 0 else "xr")
            AR.release(base_mark)
            P.next_slot = base_slot
            P.barrier()
            m0 = AR.mark(); sl_m0 = P.next_slot
            wst = Rot(P, AR, 2, [3072], F32)
            brow, t_brow = AR.alloc([6 * D], F32)
            n1g, t_n1g = AR.alloc([D], F32)
            n2g, t_n2g = AR.alloc([D], F32)
            sb_ = P.slot()
            P.dma("sync", brow[0:1, :], I["b_ada"][l:l + 1, :], sb_, writes=[t_brow])
            sn1 = P.slot()
            P.dma("sync", n1g, I["norm1_g"][l:l + 1, :].partition_broadcast(128), sn1, writes=[t_n1g])
            sn2 = P.slot()
            P.dma("sync", n2g, I["norm2_g"][l:l + 1, :].partition_broadcast(128), sn2, writes=[t_n2g])
            for half in range(2):
                for kc in range(8):
                    w, t_w, sl = wst.next()
                    P.dma("sync", w, I["w_ada"][l, kc * 128:(kc + 1) * 128, half * 3072:(half + 1) * 3072], sl, writes=[t_w])
                    for n in range(6):
                        mm(PS[n][:, :], crep[:, kc, :], w[:, n * 512:(n + 1) * 512], kc == 0, False, [t_crep, t_w], [PST[n]])
                for n in range(6):
                    col = half * 3072 + n * 512
                    mm(PS[n][:, :], ones_f[0:1, :], brow[0:1, col:col + 512], False, True, [t_ones_f, t_brow], [PST[n]])
                    A("scalar", lambda e, n=n, col=col: e.copy(out=mod_sb[:, col:col + 512], in_=PS[n][:, :]), reads=[PST[n]], writes=[t_mod])
            A("vector", lambda e: e.scalar_tensor_tensor(out=G1, in0=mod_sb[:, D:2 * D], scalar=1.0, in1=n1g, op0=ALU.add, op1=ALU.mult),
              reads=[t_mod, t_n1g], writes=[t_G1])
            A("vector", lambda e: e.scalar_tensor_tensor(out=G2, in0=mod_sb[:, 4 * D:5 * D], scalar=1.0, in1=n2g, op0=ALU.add, op1=ALU.mult),
              reads=[t_mod, t_n2g], writes=[t_G2])
            SH1 = mod_sb[:, 0:D]
            GT1 = mod_sb[:, 2 * D:3 * D]
            SH2 = mod_sb[:, 3 * D:4 * D]
            GT2 = mod_sb[:, 5 * D:6 * D]
            AR.release(m0); P.next_slot = sl_m0
            P.barrier()

            mA = AR.mark(); sl_mA = P.next_slot
            WC = 2304
            Wfm, t_Wfm = AR.alloc([8, WC], BF16)
            Wv, t_Wv = AR.alloc([8, 512], BF16)
            win_v = I["w_in"][l].rearrange("(k p) n -> p k n", p=128)
            sw = P.slot()

            def wload(dst0, src0, w):
                P.dma("gpsimd", Wfm[:, :, dst0:dst0 + w], win_v[:, :, src0:src0 + w], sw, writes=[t_Wfm])

            O_QA, O_KA, O_QB, O_KB, O_QD, O_KD, O_CQ, O_CKV, O_KR = 0, 256, 512, 768, 896, 1408, 1664, 1856, 2112
            wload(O_QA, 0, 256)
            wload(O_KA, 256, 256)
            for g in range(2):
                for j, h in enumerate((g, g + 2)):
                    wload(O_QB + g * 128 + j * 64, 768 + h * 64, 64)
            wload(O_KB, 1024, 128)
            for g in range(2):
                for j, h in enumerate((g, g + 2)):
                    wload(O_QD + g * 256 + j * 64, 1760 + h * 64, 64)
            wload(O_KD, 2016, 128)
            wload(O_CQ, 1280, 192)
            wload(O_CKV, 1472, 256)
            A("vector", lambda e: e.memset(Wfm[:, :, O_KR:O_KR + 64], 0.0), writes=[t_Wfm])
            A("vector", lambda e: e.memset(Wfm[:, :, O_KR + 96:O_KR + 160], 0.0), writes=[t_Wfm])
            wload(O_KR + 64, 1728, 32)
            P.dma("gpsimd", Wv[:, :, 0:256], win_v[:, :, 512:768], sw, writes=[t_Wv])
            P.dma("gpsimd", Wv[:, :, 256:384], win_v[:, :, 1152:1280], sw, writes=[t_Wv])
            P.dma("gpsimd", Wv[:, :, 384:512], win_v[:, :, 2144:2272], sw, writes=[t_Wv])

            def make_rot(src_o, dst_o, nheads):
                sv = Wfm[:, :, src_o:src_o + 64 * nheads].rearrange("p k (q two s) -> p k q two s", two=2, s=16)
                dv = Wfm[:, :, dst_o:dst_o + 64 * nheads].rearrange("p k (q two s) -> p k q two s", two=2, s=16)
                for kc in range(8):
                    A("vector", lambda e, kc=kc: e.tensor_scalar(out=dv[:, kc, :, 0, :], in0=sv[:, kc, :, 1, :], scalar1=-1.0, scalar2=None, op0=ALU.mult),
                      reads=[t_Wfm], writes=[t_Wfm])
                    A("gpsimd", lambda e, kc=kc: e.tensor_copy(out=dv[:, kc, :, 1, :], in_=sv[:, kc, :, 0, :]), reads=[t_Wfm], writes=[t_Wfm])

            make_rot(O_QD, O_QD + 128, 2)
            make_rot(O_QD + 256, O_QD + 384, 2)
            make_rot(O_KD, O_KD + 128, 2)
            for kc in range(8):
                A("vector", lambda e, kc=kc: e.tensor_scalar(out=Wfm[:, kc, O_KR + 160:O_KR + 176], in0=Wfm[:, kc, O_KR + 80:O_KR + 96], scalar1=-1.0, scalar2=None, op0=ALU.mult),
                  reads=[t_Wfm], writes=[t_Wfm])
                A("gpsimd", lambda e, kc=kc: e.tensor_copy(out=Wfm[:, kc, O_KR + 176:O_KR + 192], in_=Wfm[:, kc, O_KR + 64:O_KR + 80]), reads=[t_Wfm], writes=[t_Wfm])

            wuq_f, t_wuqf = AR.alloc([2, 384], F32)
            wukv_f, t_wukvf = AR.alloc([2, 512], F32)
            gq, t_gq = AR.alloc([2], F32)
            gkv, t_gkv = AR.alloc([2], F32)
            Wuq, t_Wuq = AR.alloc([2, 4, 96], BF16)
            Wuqr, t_Wuqr = AR.alloc([2, 4, 96], BF16)
            Wukk, t_Wukk = AR.alloc([2, 4, 64], BF16)
            Wukv_v, t_Wukv = AR.alloc([2, 4, 64], BF16)
            s1 = P.slot()
            A("vector", lambda e: e.memset(wuq_f, 0.0), writes=[t_wuqf])
            A("vector", lambda e: e.memset(gq, 0.0), writes=[t_gq])
            P.dma("sync", wuq_f[:, 0, :], I["w_uq"][l, 0:128, :], s1, writes=[t_wuqf])
            P.dma("sync", wuq_f[0:64, 1, :], I["w_uq"][l, 128:192, :], s1, writes=[t_wuqf])
            P.dma("sync", wukv_f, I["w_ukv"][l].rearrange("(k p) n -> p k n", p=128), s1, writes=[t_wukvf])
            P.dma("sync", gq[:, 0:1], I["mla_q_norm_g"][l, 0:128].rearrange("(p o) -> p o", o=1), s1, writes=[t_gq])
            P.dma("sync", gq[0:64, 1:2], I["mla_q_norm_g"][l, 128:192].rearrange("(p o) -> p o", o=1), s1, writes=[t_gq])
            P.dma("sync", gkv, I["mla_kv_norm_g"][l].rearrange("(k p) -> p k", p=128), s1, writes=[t_gkv], allow_slow_non_contiguous=True)
            wuq4 = wuq_f.rearrange("p k (h c) -> p k h c", h=4)
            wukv4 = wukv_f.rearrange("p k (h c) -> p k h c", h=4)
            A("vector", lambda e: e.memset(Wuqr, 0.0), writes=[t_Wuqr])
            for k in range(2):
                A("vector", lambda e, k=k: e.tensor_scalar(out=Wuq[:, k], in0=wuq4[:, k], scalar1=gq[:, k:k + 1], scalar2=None, op0=ALU.mult),
                  reads=[t_wuqf, t_gq], writes=[t_Wuq])
                A("vector", lambda e, k=k: e.tensor_scalar(out=Wuqr[:, k, :, 64:80], in0=wuq4[:, k, :, 80:96], scalar1=gq[:, k:k + 1], scalar2=-1.0, op0=ALU.mult, op1=ALU.mult),
                  reads=[t_wuqf, t_gq], writes=[t_Wuqr])
                A("vector", lambda e, k=k: e.tensor_scalar(out=Wuqr[:, k, :, 80:96], in0=wuq4[:, k, :, 64:80], scalar1=gq[:, k:k + 1], scalar2=None, op0=ALU.mult),
                  reads=[t_wuqf, t_gq], writes=[t_Wuqr])
                A("vector", lambda e, k=k: e.tensor_scalar(out=Wukk[:, k], in0=wukv4[:, k, :, 0:64], scalar1=gkv[:, k:k + 1], scalar2=None, op0=ALU.mult),
                  reads=[t_wukvf, t_gkv], writes=[t_Wukk])
                A("vector", lambda e, k=k: e.tensor_scalar(out=Wukv_v[:, k], in0=wukv4[:, k, :, 64:128], scalar1=gkv[:, k:k + 1], scalar2=None, op0=ALU.mult),
                  reads=[t_wukvf, t_gkv], writes=[t_Wukv])
            gD, t_gD = AR.alloc([4], F32)
            for ci, nm in ((0, "ax_q_norm_g"), (2, "ax_k_norm_g")):
                for half in range(2):
                    P.dma("sync", gD[half * 64:(half + 1) * 64, ci:ci + 1], I[nm][l, :].rearrange("(p o) -> p o", o=1), s1, writes=[t_gD])
                    for blk, src in enumerate((1, 0, 3, 2)):
                        P.dma("sync", gD[half * 64 + blk * 16:half * 64 + (blk + 1) * 16, ci + 1:ci + 2],
                              I[nm][l, src * 16:(src + 1) * 16].rearrange("(p o) -> p o", o=1), s1, writes=[t_gD])
            A("vector", lambda e: e.tensor_scalar(out=gD[:, 0:2], in0=gD[:, 0:2], scalar1=0.125, scalar2=None, op0=ALU.mult), reads=[t_gD], writes=[t_gD])

            hTr = Rot(P, AR, 2, [8, CH], BF16)
            xrot = Rot(P, AR, 2, [D], F32)
            nb = {"junk": AR.alloc([D], BF16), "ss": AR.alloc([4], F32), "hf": AR.alloc([D], F32), "hb": Rot(P, AR, 2, [D], BF16)}
            stg = Rot(P, AR, 4, [CH], BF16)
            vst = Rot(P, AR, 2, [8, 128], BF16)
            vcst = Rot(P, AR, 2, [4, 128], BF16)
            for b in vst.bufs + vcst.bufs:
                A("vector", lambda e, b=b: e.memset(b[0], 1.0), writes=[b[1]])
            tabr = Rot(P, AR, 2, [2, CH], F32)
            tabc = Rot(P, AR, 2, [2, CH], F32)
            sq_a, t_sqa = AR.alloc([2, CH], F32)
            rb, t_rb = AR.alloc([CH], F32)
            rtmp, t_rtmp = AR.alloc([CH], F32)
            f1, t_f1 = AR.alloc([CH], F32)
            f2, t_f2 = AR.alloc([CH], F32)
            cqn, t_cqn = AR.alloc([2, CH], BF16)
            ckvn, t_ckvn = AR.alloc([2, CH], BF16)
            kpe, t_kpe = AR.alloc([CH], BF16)

            def fm_group(ps, col0, M, hT, t_hT, tps):
                for kc in range(8):
                    mm(ps[0:M, :], Wfm[:, kc, col0:col0 + M], hT[:, kc, :], kc == 0, kc == 7, [t_Wfm, t_hT], [tps])

            for ch in range(NCH):
                cs = slice(ch * CH, (ch + 1) * CH)
                hT, t_hT, _ = hTr.next()
                norm_chunk(xsrc, xtk, ch, G1, SH1, [t_G1, t_mod], hT, t_hT, xrot, nb)
                simple = [(O_QA, QT_A, 0, 0.125, "qa"), (O_QA + 128, QT_A, 1, 0.125, "qa"), (O_KA, KT_A, 0, 1.0, "ka"), (O_KA + 128, KT_A, 1, 1.0, "ka"),
                          (O_QB, QT_B, 0, 0.125, "qb"), (O_QB + 128, QT_B, 1, 0.125, "qb"), (O_KB, KT_B, 0, 1.0, "kb")]
                for col0, dst, gi, scl, nm in simple:
                    ps, tps = ps_next(1, 5)
                    fm_group(ps, col0, 128, hT, t_hT, tps)
                    sg, t_sg, sl = stg.next()
                    A("scalar", lambda e, ps=ps, sg=sg, scl=scl: e.mul(out=sg, in_=ps[:, :], mul=scl), reads=[tps], writes=[t_sg])
                    P.dma("gpsimd", dst[gi, :, cs], sg, sl, reads=[t_sg], writes=[(nm, gi, ch)])
                tb, t_tb, sl = tabr.next()
                P.dma("sync", tb, I["k_ropeD"][:, :, cs].rearrange("a p t -> p a t"), sl, writes=[t_tb])
                for col0, dst, gi, gc, nm in ((O_QD, QT_D, 0, 0, "qd"), (O_QD + 256, QT_D, 1, 0, "qd"), (O_KD, KT_D, 0, 2, "kd")):
                    psa, tpa = ps_next(1, 5)
                    fm_group(psa, col0, 128, hT, t_hT, tpa)
                    psb, tpb = ps_next(1, 5)
                    fm_group(psb, col0 + 128, 128, hT, t_hT, tpb)
                    A("scalar", lambda e, psa=psa: e.activation(out=sq_a[:, 0, :], in_=psa[:, :], func=AF.Square), reads=[tpa], writes=[t_sqa])
                    mm(PS[5][:, :], bd_f, sq_a[:, 0, :], True, True, [t_bd_f, t_sqa], [PST[5]])
                    rstd_from(PS[5][:, :], rb, 64.0, [PST[5]], [t_rb], rtmp)
                    A("vector", lambda e, psa=psa, gc=gc, tb=tb: e.scalar_tensor_tensor(out=f1, in0=psa[:, :], scalar=gD[:, gc:gc + 1], in1=tb[:, 0, :], op0=ALU.mult, op1=ALU.mult),
                      reads=[tpa, t_gD, t_tb], writes=[t_f1])
                    A("vector", lambda e, psb=psb, gc=gc, tb=tb: e.scalar_tensor_tensor(out=f2, in0=psb[:, :], scalar=gD[:, gc + 1:gc + 2], in1=tb[:, 1, :], op0=ALU.mult, op1=ALU.mult),
                      reads=[tpb, t_gD, t_tb], writes=[t_f2])
                    A("gpsimd", lambda e: e.tensor_tensor(out=f1, in0=f1, in1=f2, op=ALU.add), reads=[t_f1, t_f2], writes=[t_f1])
                    sg, t_sg, sl = stg.next()
                    A("vector", lambda e, sg=sg: e.tensor_tensor(out=sg, in0=f1, in1=rb, op=ALU.mult), reads=[t_f1, t_rb], writes=[t_sg])
                    P.dma("gpsimd", dst[gi, :, cs], sg, sl, reads=[t_sg], writes=[(nm, gi, ch)])
                tc, t_tc, sl = tabc.next()
                P.dma("sync", tc[0:96], I["k_ropeC"][:, :, cs].rearrange("a p t -> p a t"), sl, writes=[t_tc])
                for (col0, widths, dstn, t_dn, nfeat) in ((O_CQ, (128, 64), cqn, t_cqn, 192.0), (O_CKV, (128, 128), ckvn, t_ckvn, 256.0)):
                    pss = []
                    for k, w in enumerate(widths):
                        ps, tps = ps_next(1, 5)
                        fm_group(ps, col0 + k * 128, w, hT, t_hT, tps)
                        A("scalar", lambda e, ps=ps, k=k, w=w: e.activation(out=sq_a[0:w, k, :], in_=ps[0:w, :], func=AF.Square), reads=[tps], writes=[t_sqa])
                        pss.append((ps, tps, w))
                    for k, w in enumerate(widths):
                        mm(PS[5][:, :], ones_f[0:w, :], sq_a[0:w, k, :], k == 0, k == 1, [t_ones_f, t_sqa], [PST[5]])
                    rstd_from(PS[5][:, :], rb, nfeat, [PST[5]], [t_rb], rtmp)
                    for k, (ps, tps, w) in enumerate(pss):
                        A("vector", lambda e, ps=ps, k=k, w=w, dstn=dstn: e.tensor_tensor(out=dstn[0:w, k, :], in0=ps[0:w, :], in1=rb[0:w, :], op=ALU.mult),
                          reads=[tps, t_rb], writes=[t_dn])
                psa, tpa = ps_next(1, 5)
                fm_group(psa, O_KR, 96, hT, t_hT, tpa)
                psb, tpb = ps_next(1, 5)
                fm_group(psb, O_KR + 96, 96, hT, t_hT, tpb)
                A("vector", lambda e, psa=psa, tc=tc: e.tensor_tensor(out=f1[64:96, :], in0=psa[64:96, :], in1=tc[64:96, 0, :], op=ALU.mult), reads=[tpa, t_tc], writes=[t_f1])
                A("vector", lambda e, psb=psb, tc=tc: e.tensor_tensor(out=f2[64:96, :], in0=psb[64:96, :], in1=tc[64:96, 1, :], op=ALU.mult), reads=[tpb, t_tc], writes=[t_f2])
                A("gpsimd", lambda e: e.tensor_tensor(out=kpe[64:96, :], in0=f1[64:96, :], in1=f2[64:96, :], op=ALU.add), reads=[t_f1, t_f2], writes=[t_kpe])
                for h in range(4):
                    psa, tpa = ps_next(1, 5)
                    psb, tpb = ps_next(1, 5)
                    for k, w in enumerate((128, 64)):
                        mm(psa[0:96, :], Wuq[0:w, k, h, :], cqn[0:w, k, :], k == 0, k == 1, [t_Wuq, t_cqn], [tpa])
                    for k, w in enumerate((128, 64)):
                        mm(psb[0:96, :], Wuqr[0:w, k, h, :], cqn[0:w, k, :], k == 0, k == 1, [t_Wuqr, t_cqn], [tpb])
                    A("vector", lambda e, psa=psa, tc=tc: e.tensor_tensor(out=f1[0:96, :], in0=psa[0:96, :], in1=tc[0:96, 0, :], op=ALU.mult), reads=[tpa, t_tc], writes=[t_f1])
                    A("vector", lambda e, psb=psb, tc=tc: e.tensor_tensor(out=f2[0:96, :], in0=psb[0:96, :], in1=tc[0:96, 1, :], op=ALU.mult), reads=[tpb, t_tc], writes=[t_f2])
                    sg, t_sg, sl = stg.next()
                    A("gpsimd", lambda e, sg=sg: e.tensor_tensor(out=sg[0:96, :], in0=f1[0:96, :], in1=f2[0:96, :], op=ALU.add), reads=[t_f1, t_f2], writes=[t_sg])
                    P.dma("gpsimd", QT_C[h, :, cs], sg[0:96, :], sl, reads=[t_sg], writes=[("qc", h, ch)])
                    psk, tpk = ps_next(1, 5)
                    for k in range(2):
                        mm(psk[0:64, :], Wukk[:, k, h, :], ckvn[:, k, :], k == 0, k == 1, [t_Wukk, t_ckvn], [tpk])
                    sg, t_sg, sl = stg.next()
                    A("scalar", lambda e, sg=sg, psk=psk: e.copy(out=sg[0:64, :], in_=psk[0:64, :]), reads=[tpk], writes=[t_sg])
                    A("gpsimd", lambda e, sg=sg: e.tensor_copy(out=sg[64:96, :], in_=kpe[64:96, :]), reads=[t_kpe], writes=[t_sg])
                    P.dma("gpsimd", KT_C[h, :, cs], sg[0:96, :], sl, reads=[t_sg], writes=[("kc", h, ch)])
                for t in range(4):
                    tile = ch * 4 + t
                    ts_ = slice(t * 128, (t + 1) * 128)
                    ps, tps = ps_next(6, 8)
                    for kc in range(8):
                        mm(ps[:, :], hT[:, kc, ts_], Wv[:, kc, :], kc == 0, kc == 7, [t_hT, t_Wv], [tps])
                    vb, t_vb, sl = vst.next()
                    A("scalar", lambda e, vb=vb, ps=ps: e.copy(out=vb[:, :, 0:64], in_=ps[:, :].rearrange("p (h c) -> p h c", h=8)), reads=[tps], writes=[t_vb])
                    P.dma("gpsimd", V_ABD[tile * 128:(tile + 1) * 128], vb, sl, reads=[t_vb], writes=[("vabd", tile)])
                    ps, tps = ps_next(6, 8)
                    for k in range(2):
                        mm(ps[:, 0:256], ckvn[:, k, ts_], Wukv_v[:, k].rearrange("p h c -> p (h c)"), k == 0, k == 1, [t_ckvn, t_Wukv], [tps])
                    vb, t_vb, sl = vcst.next()
                    A("scalar", lambda e, vb=vb, ps=ps: e.copy(out=vb[:, :, 0:64], in_=ps[:, 0:256].rearrange("p (h c) -> p h c", h=4)), reads=[tps], writes=[t_vb])
                    P.dma("gpsimd", V_C[tile * 128:(tile + 1) * 128], vb, sl, reads=[t_vb], writes=[("vc", tile)])
            AR.release(mA); P.next_slot = sl_mA
            P.barrier()
            if stop_after == "A":
                break

            def finish_heads(O, tO, heads_blocks, br, tcol, rc, t_rc, yst, add_sink=None):
                n = sum(w for _, _, w in heads_blocks)
                if add_sink is not None:
                    es, t_es = add_sink
                    A("vector", lambda e: e.tensor_tensor(out=rc[64:128, 0:n].rearrange("p (h q) -> p h q", h=4),
                                                          in0=O[64:128, 0:n].rearrange("p (h q) -> p h q", h=4),
                                                          in1=es[64:128, :].unsqueeze(2).to_broadcast([64, 4, n // 4]), op=ALU.add),
                      reads=[tO, t_es], writes=[t_rc])
                    A("vector", lambda e: e.reciprocal(out=rc[64:128, 0:n], in_=rc[64:128, 0:n]), reads=[t_rc], writes=[t_rc])
                else:
                    A("vector", lambda e: e.reciprocal(out=rc[64:128, 0:n], in_=O[64:128, 0:n]), reads=[tO], writes=[t_rc])
                ys, t_ys, sl = yst.next()
                for (h, c0, w) in heads_blocks:
                    po = (h % 2) * 64
                    if len(heads_blocks) == 1:
                        oap = ys[po:po + 64, 0:w]
                    else:
                        oap = ys[po:po + 64, h // 2, 0:w]
                    A("vector", lambda e, oap=oap, c0=c0, w=w: e.tensor_tensor(out=oap, in0=O[0:64, c0:c0 + w], in1=rc[64:128, c0:c0 + w], op=ALU.mult),
                      reads=[tO, t_rc], writes=[t_ys])
                return ys, t_ys, sl

            for br in (0, 1):
                mB = AR.mark(); sl_mB = P.next_slot
                ngq = 2
                KT, t_KT = AR.alloc([2 if br == 0 else 1, S], BF16)
                QT, t_QT = AR.alloc([2, S], BF16)
                Vt, t_Vt = AR.alloc([NT, 4 if br == 0 else 2, 128], BF16)
                sl0 = P.slot()
                if br == 0:
                    for g in range(2):
                        P.dma("sync", KT[:, g, :], KT_A[g], sl0, reads=[("ka", g, c_) for c_ in range(NCH)], writes=[t_KT])
                        P.dma("sync", QT[:, g, :], QT_A[g], sl0, reads=[("qa", g, c_) for c_ in range(NCH)], writes=[t_QT])
                    for tq in range(4):
                        P.dma("sync", Vt[:, tq * 8:(tq + 1) * 8], V_ABD.rearrange("(t p) h c -> p t h c", p=128)[:, tq * 8:(tq + 1) * 8, 0:4, :], sl0, reads=[("vabd", t_) for t_ in range(NT)], writes=[t_Vt])
                else:
                    P.dma("sync", KT[:, 0, :], KT_B[0], sl0, reads=[("kb", 0, c_) for c_ in range(NCH)], writes=[t_KT])
                    for g in range(2):
                        P.dma("sync", QT[:, g, :], QT_B[g], sl0, reads=[("qb", g, c_) for c_ in range(NCH)], writes=[t_QT])
                    for tq in range(4):
                        P.dma("sync", Vt[:, tq * 8:(tq + 1) * 8], V_ABD.rearrange("(t p) h c -> p t h c", p=128)[:, tq * 8:(tq + 1) * 8, 4:6, :], sl0, reads=[("vabd", t_) for t_ in range(NT)], writes=[t_Vt])
                Sbr = Rot(P, AR, 2, [4, 128], F32)
                Ptr = Rot(P, AR, 3, [4, 128], BF16)
                rc, t_rc = AR.alloc([512], F32)
                yst = Rot(P, AR, 2, [2, 128], BF16)
                if br == 0:
                    TT, t_TT = AR.alloc([60, 64], F32)
                    NEGT, t_NEGT = AR.alloc([4, 64], F32)
                    E2, t_E2 = AR.alloc([64, 128], F32)
                    rbT, t_rbT = AR.alloc([64], F32)
                    A("vector", lambda e: e.memset(NEGT, NEG), writes=[t_NEGT])
                    A("vector", lambda e: e.memset(rbT[0:32, :], 1.0), writes=[t_rbT])
                    P.dma("sync", E2[0:32], I["k_e2"], sl0, writes=[t_E2])
                    P.dma("sync", rbT[0:31, 0:60], I["na_rel_bias"][l].rearrange("h r i -> i (h r)"), sl0, reads=[t_rbT], writes=[t_rbT], allow_slow_non_contiguous=True)
                    for q0 in range(0, 64, 8):
                        ps, tps = ps_next(1, 8)
                        for q in range(8):
                            mm(ps[:, q * 64:q * 64 + 60], E2[0:32, q0 + q, :], rbT[0:32, 0:60], True, True, [t_E2, t_rbT], [tps])
                        A("vector", lambda e, ps=ps, q0=q0: e.tensor_copy(out=TT[:, :, q0:q0 + 8].rearrange("p c q -> p q c"),
                                                                         in_=ps[:, :].rearrange("p (q c) -> p q c", q=8)[:, :, 0:60]),
                          reads=[tps], writes=[t_TT])
                    TTv = TT.rearrange("p (g hf r) q -> p hf g r q", g=2, hf=2)
                else:
                    ALt, t_AL = AR.alloc([3, 4, 128], F32)
                    es, t_es = AR.alloc([4], F32)
                    P.dma("sync", ALt, I["k_al"], sl0, writes=[t_AL])
                    P.dma("sync", es, I["win_sink"][l:l + 1, :].partition_broadcast(128), sl0, writes=[t_es])
                    A("scalar", lambda e: e.activation(out=es, in_=es, func=AF.Exp), reads=[t_es], writes=[t_es])

                def rs(r):
                    return min(max(r - 4, 0), 56)

                stepsAB = []
                for j in range(NT):
                    if br == 0:
                        kts = list(range(rs(2 * j) // 2, (rs(2 * j + 1) + 7) // 2 + 1))
                    else:
                        kts = [k for k in (j - 1, j, j + 1) if 0 <= k < NT]
                    for ki, kt in enumerate(kts):
                        stepsAB.append((j, ki, kt, len(kts)))
                pendAB = {}

                def ab_qk(s_):
                    j, ki, kt, nk = stepsAB[s_]
                    qs = slice(j * 128, (j + 1) * 128)
                    ks = slice(kt * 128, (kt + 1) * 128)
                    banks = (ps_next(0, 6), ps_next(0, 6))
                    for h in range(4):
                        if br == 0:
                            half, slot = h % 2, h // 2
                            kidx = slot
                        else:
                            half, slot = h // 2, h % 2
                            kidx = 0
                        po = half * 64
                        ps_, tps_ = banks[half]
                        mm(ps_[:, slot * 128:(slot + 1) * 128], KT[po:po + 64, kidx, ks], QT[po:po + 64, slot, qs], True, True, [t_KT, t_QT], [tps_])
                    pendAB[s_] = banks

                def ab_rest(s_):
                    j, ki, kt, nk = stepsAB[s_]
                    qs = slice(j * 128, (j + 1) * 128)
                    banks = pendAB.pop(s_)
                    O, tO = PS[6 + (j % 2)], PST[6 + (j % 2)]
                    Sb, t_Sb, _ = Sbr.next()
                    Sb5 = Sb.rearrange("p (hf g) q -> p hf g q", hf=2)
                    for half in range(2):
                        ps_, tps_ = banks[half]
                        pv = ps_[:, 0:256].rearrange("p (g q) -> p g q", g=2)
                        if br == 0:
                            for krl in range(2):
                                for rl in range(2):
                                    r, kr = 2 * j + rl, 2 * kt + krl
                                    pp = slice(krl * 64, (krl + 1) * 64)
                                    if rs(r) <= kr < rs(r) + 8:
                                        dr = kr - r
                                        in1 = TTv[pp, half, :, dr + 7, :]
                                        rd = [t_TT]
                                    else:
                                        in1 = NEGT[pp, 0:2, :]
                                        rd = [t_NEGT]
                                    A("vector", lambda e, pp=pp, rl=rl, in1=in1, pv=pv, half=half, Sb5=Sb5: e.tensor_tensor(out=Sb5[pp, half, :, rl * 64:(rl + 1) * 64], in0=pv[pp, :, rl * 64:(rl + 1) * 64], in1=in1, op=ALU.add),
                                      reads=[tps_] + rd, writes=[t_Sb])
                        else:
                            o = kt - j + 1
                            A("vector", lambda e, pv=pv, o=o, half=half, Sb5=Sb5, ALt=ALt: e.tensor_tensor(out=Sb5[:, half, :, :], in0=pv, in1=ALt[:, o, half * 2:half * 2 + 2, :], op=ALU.add),
                              reads=[tps_, t_AL], writes=[t_Sb])
                    Pt, t_Pt, _ = Ptr.next()
                    A("scalar", lambda e, Pt=Pt, Sb=Sb: e.activation(out=Pt, in_=Sb, func=AF.Exp), reads=[t_Sb], writes=[t_Pt])
                    for h in range(4):
                        vh = h if br == 0 else h // 2
                        hh = (h % 2) * 2 + h // 2 if br == 0 else h
                        mm(O[:, h * 128:(h + 1) * 128], Vt[:, kt, vh, :], Pt[:, hh, :], ki == 0 and h == 0, ki == nk - 1, [t_Vt, t_Pt], [tO],
                           skip_group_check=True)
                    if ki == nk - 1:
                        ys, t_ys, sl = finish_heads(O, tO, [(h, h * 128, 128) for h in range(4)], br, None, rc, t_rc, yst,
                                                    add_sink=(es, t_es) if br == 1 else None)
                        P.dma("gpsimd", YT[br, :, :, qs], ys, sl, reads=[t_ys], writes=[("yt", br, j // 4, j % 4)])

                DPAB = 2
                for s_ in range(len(stepsAB) + DPAB):
                    if s_ < len(stepsAB):
                        ab_qk(s_)
                    if s_ >= DPAB:
                        ab_rest(s_ - DPAB)
                AR.release(mB); P.next_slot = sl_mB
                P.barrier()
                if stop_after == "attn%d" % br:
                    break
            if stop_after in ("attn0", "attn1"):
                break

            for br in (2, 3):
                mC = AR.mark(); sl_mC = P.next_slot
                sl0 = P.slot()
                if br == 2:
                    KT, t_KT = AR.alloc([4, S], BF16)
                    Vt, t_Vt = AR.alloc([NT, 4, 128], BF16)
                    for h in range(4):
                        P.dma("sync", KT[0:96, h, :], KT_C[h], sl0, reads=[("kc", h, c_) for c_ in range(NCH)], writes=[t_KT])
                    for tq in range(4):
                        P.dma("sync", Vt[:, tq * 8:(tq + 1) * 8], V_C.rearrange("(t p) h c -> p t h c", p=128)[:, tq * 8:(tq + 1) * 8], sl0, reads=[("vc", t_) for t_ in range(NT)], writes=[t_Vt])
                    scl = 96.0 ** -0.5
                else:
                    KT, t_KT = AR.alloc([1, S], BF16)
                    Vt, t_Vt = AR.alloc([NT, 2, 128], BF16)
                    P.dma("sync", KT[:, 0, :], KT_D[0], sl0, reads=[("kd", 0, c_) for c_ in range(NCH)], writes=[t_KT])
                    for tq in range(4):
                        P.dma("sync", Vt[:, tq * 8:(tq + 1) * 8], V_ABD.rearrange("(t p) h c -> p t h c", p=128)[:, tq * 8:(tq + 1) * 8, 6:8, :], sl0, reads=[("vabd", t_) for t_ in range(NT)], writes=[t_Vt])
                    scl = 1.0
                Qr = Rot(P, AR, 3, [CH], BF16)
                Ptr = Rot(P, AR, 4, [CH], BF16)
                rc, t_rc = AR.alloc([512], F32)
                yst = Rot(P, AR, 2, [CH], BF16)
                blocksCD = [(h, ch) for h in range(4) for ch in range(NCH)]
                qbuf = {}

                def cd_loadq(bi):
                    h, ch = blocksCD[bi]
                    cs = slice(ch * CH, (ch + 1) * CH)
                    Qc, t_Qc, sl = Qr.next()
                    if br == 2:
                        P.dma("sync", Qc[0:96, :], QT_C[h, :, cs], sl, reads=[("qc", h, ch)], writes=[t_Qc])
                    else:
                        po = (h // 2) * 64
                        P.dma("sync", Qc[po:po + 64, :], QT_D[h % 2, po:po + 64, cs], sl, reads=[("qd", h % 2, ch)], writes=[t_Qc])
                    qbuf[bi] = (Qc, t_Qc)

                pendCD = {}
                nstCD = len(blocksCD) * NT

                def cd_qk(s_):
                    bi, kt = divmod(s_, NT)
                    h, ch = blocksCD[bi]
                    if kt == 0 and bi + 1 < len(blocksCD):
                        cd_loadq(bi + 1)
                    Qc, t_Qc = qbuf[bi]
                    ks = slice(kt * 128, (kt + 1) * 128)
                    ps, tps = ps_next(0, 6)
                    if br == 2:
                        mm(ps[:, :], KT[0:96, h, ks], Qc[0:96, :], True, True, [t_KT, t_Qc], [tps])
                    else:
                        po = (h // 2) * 64
                        mm(ps[:, :], KT[po:po + 64, 0, ks], Qc[po:po + 64, :], True, True, [t_KT, t_Qc], [tps])
                    pendCD[s_] = (ps, tps)

                def cd_rest(s_):
                    bi, kt = divmod(s_, NT)
                    h, ch = blocksCD[bi]
                    cs = slice(ch * CH, (ch + 1) * CH)
                    ps, tps = pendCD.pop(s_)
                    O, tO = PS[6 + (bi % 2)], PST[6 + (bi % 2)]
                    Pt, t_Pt, _ = Ptr.next()
                    A("scalar", lambda e, Pt=Pt, ps=ps, scl=scl: e.activation(out=Pt, in_=ps[:, :], func=AF.Exp, scale=scl), reads=[tps], writes=[t_Pt])
                    vh = h if br == 2 else h // 2
                    mm(O[:, :], Vt[:, kt, vh, :], Pt, kt == 0, kt == NT - 1, [t_Vt, t_Pt], [tO])
                    if kt == NT - 1:
                        ys, t_ys, sl = finish_heads(O, tO, [(h, 0, CH)], br, None, rc, t_rc, yst)
                        po2 = (h % 2) * 64
                        P.dma("gpsimd", YT[br, po2:po2 + 64, h // 2, cs], ys[po2:po2 + 64, :], sl, reads=[t_ys], writes=[("yt", br, ch, h)])

                DPCD = 3
                cd_loadq(0)
                for s_ in range(nstCD + DPCD):
                    if s_ < nstCD:
                        cd_qk(s_)
                    if s_ >= DPCD:
                        cd_rest(s_ - DPCD)
                AR.release(mC); P.next_slot = sl_mC
                P.barrier()
                if stop_after == "attn%d" % br:
                    break
            if stop_after in ("attn", "attn2", "attn3"):
                break

            mM = AR.mark(); sl_mM = P.next_slot
            Wg, t_Wg = AR.alloc([8, 4096], BF16)
            Wb, t_Wb = AR.alloc([4, 2, D], BF16)
            Wo, t_Wo = AR.alloc([8, D], BF16)
            sw = P.slot()
            for n in range(8):
                P.dma("gpsimd", Wg[:, :, n * 512:(n + 1) * 512], win_v[:, :, 2272 + n * 512:2272 + (n + 1) * 512], sw, writes=[t_Wg])
            P.dma("gpsimd", Wb, I["w_branch"][l].rearrange("n (k p) d -> p n k d", p=128), sw, writes=[t_Wb])
            P.dma("gpsimd", Wo, I["w_out"][l].rearrange("(k p) n -> p k n", p=128), sw, writes=[t_Wo])
            hTr = Rot(P, AR, 1, [8, CH], BF16)
            xrot = Rot(P, AR, 4, [D], F32)
            nb = {"junk": AR.alloc([D], BF16), "ss": AR.alloc([4], F32), "hf": AR.alloc([D], F32), "hb": Rot(P, AR, 2, [D], BF16)}
            Yr = Rot(P, AR, 1, [4, 2, CH], BF16)
            gtr = Rot(P, AR, 2, [CH], BF16)
            macc, t_macc = AR.alloc([CH], F32)
            mtmp = Rot(P, AR, 2, [CH], F32)
            mT, t_mT = AR.alloc([8, CH], BF16)
            slxo = P.slot()
            for ch in range(NCH):
                cs = slice(ch * CH, (ch + 1) * CH)
                hT, t_hT, _ = hTr.next()
                xts = norm_chunk(xsrc, xtk, ch, G1, SH1, [t_G1, t_mod], hT, t_hT, xrot, nb)
                Y, t_Y, sly = Yr.next()
                for b4 in range(4):
                    P.dma("sync", Y[:, b4], YT[b4, :, :, cs], sly, reads=[("yt", b4, ch, k_) for k_ in range(4)], writes=[t_Y])
                for dc in range(8):
                    for n in range(4):
                        pg, tpg = ps_next(0, 4)
                        for kc in range(8):
                            mm(pg[:, :], Wg[:, kc, n * D + dc * 128:n * D + (dc + 1) * 128], hT[:, kc, :], kc == 0, kc == 7, [t_Wg, t_hT], [tpg])
                        gt, t_gt, _ = gtr.next()
                        A("scalar", lambda e, gt=gt, pg=pg: e.activation(out=gt, in_=pg[:, :], func=AF.Sigmoid), reads=[tpg], writes=[t_gt])
                        pb, tpb = ps_next(0, 4)
                        for k in range(2):
                            mm(pb[:, :], Wb[:, n, k, dc * 128:(dc + 1) * 128], Y[:, n, k, :], k == 0, k == 1, [t_Wb, t_Y], [tpb])
                        if n == 0:
                            A("vector", lambda e, pb=pb, gt=gt: e.tensor_tensor(out=macc, in0=pb[:, :], in1=gt, op=ALU.mult), reads=[tpb, t_gt], writes=[t_macc])
                        else:
                            mt, t_mt, _ = mtmp.next()
                            A("vector", lambda e, pb=pb, gt=gt, mt=mt: e.tensor_tensor(out=mt, in0=pb[:, :], in1=gt, op=ALU.mult), reads=[tpb, t_gt], writes=[t_mt])
                            if n < 3:
                                A("gpsimd", lambda e, mt=mt: e.tensor_tensor(out=macc, in0=macc, in1=mt, op=ALU.add), reads=[t_macc, t_mt], writes=[t_macc])
                            else:
                                A("gpsimd", lambda e, mt=mt, dc=dc: e.tensor_tensor(out=mT[:, dc, :], in0=macc, in1=mt, op=ALU.add), reads=[t_macc, t_mt], writes=[t_mT])
                for t in range(4):
                    tile = ch * 4 + t
                    xt, t_xt, slx = xts[t]
                    xn, t_xn = xt, t_xt
                    for nh in range(2):
                        po_, tpo = ps_next(4, 8)
                        for dc in range(8):
                            mm(po_[:, :], mT[:, dc, t * 128:(t + 1) * 128], Wo[:, dc, nh * 512:(nh + 1) * 512], dc == 0, dc == 7, [t_mT, t_Wo], [tpo])
                        mt, t_mt, _ = mtmp.next()
                        A("vector", lambda e, po_=po_, mt=mt, nh=nh: e.tensor_tensor(out=mt, in0=po_[:, :], in1=GT1[:, nh * 512:(nh + 1) * 512], op=ALU.mult),
                          reads=[tpo, t_mod], writes=[t_mt])
                        A("gpsimd", lambda e, xn=xn, xt=xt, mt=mt, nh=nh: e.tensor_tensor(out=xn[:, nh * 512:(nh + 1) * 512], in0=xt[:, nh * 512:(nh + 1) * 512], in1=mt, op=ALU.add),
                          reads=[t_xt, t_mt], writes=[t_xn])
                    P.dma("gpsimd", XR[tile * 128:(tile + 1) * 128, :], xn, slx, reads=[t_xn, ("xr", tile)], writes=[("xr", tile), ("xm", tile)])
            AR.release(mM); P.next_slot = sl_mM
            P.barrier()
            if stop_after == "merge":
                break

            mF = AR.mark(); sl_mF = P.next_slot
            SC = 1024
            h2, t_h2 = AR.alloc([8, SC], BF16)
            acc, t_acc = AR.alloc([8, D], F32)
            comb, t_comb = AR.alloc([8, 32], F32)
            Wr, t_Wr = AR.alloc([8, 36], BF16)
            brt, t_brt = AR.alloc([36], F32)
            sw = P.slot()
            P.dma("gpsimd", Wr[:, :, 0:4], I["w_group"][l].rearrange("(k p) n -> p k n", p=128), sw, writes=[t_Wr])
            P.dma("gpsimd", Wr[:, :, 4:36], I["w_router"][l].rearrange("(k p) n -> p k n", p=128), sw, writes=[t_Wr])
            swb = P.slot()
            P.dma("sync", brt[:, 0:4], I["b_group"][l:l + 1, :].partition_broadcast(128), swb, writes=[t_brt])
            P.dma("sync", brt[:, 4:36], I["b_router"][l:l + 1, :].partition_broadcast(128), swb, writes=[t_brt])
            hTr = Rot(P, AR, 1, [8, CH], BF16)
            xrot = Rot(P, AR, 2, [D], F32)
            nb = {"junk": AR.alloc([D], BF16), "ss": AR.alloc([4], F32), "hf": AR.alloc([D], F32), "hb": Rot(P, AR, 2, [D], BF16)}
            W1r = Rot(P, AR, 2, [8, 256], BF16)
            W3r = Rot(P, AR, 2, [8, 256], BF16)
            W2r = Rot(P, AR, 2, [2, D], BF16)
            slr = Rot(P, AR, 2, [CH], F32)
            hidr = Rot(P, AR, 2, [2, CH], BF16)
            rt, t_rt = AR.alloc([160], F32)
            xo = Rot(P, AR, 2, [D], F32)
            gfin, t_gfin = AR.alloc([D], F32)
            if l == depth - 1:
                P.dma("sync", gfin, I["final_norm_g"].rearrange("(o d) -> o d", o=1).partition_broadcast(128), swb, writes=[t_gfin])
            for sc in range(4):
                for c4 in range(2):
                    ch = sc * 2 + c4
                    hT, t_hT, _ = hTr.next()
                    norm_chunk(XR, "xm", ch, G2, SH2, [t_G2, t_mod], hT, t_hT, xrot, nb)
                    A("gpsimd", lambda e, c4=c4, hT=hT: e.tensor_copy(out=h2[:, :, c4 * CH:(c4 + 1) * CH], in_=hT), reads=[t_hT], writes=[t_h2])
                    for t in range(4):
                        lt = c4 * 4 + t
                        ps, tps = ps_next(0, 4)
                        for kc in range(8):
                            mm(ps[:, 0:36], hT[:, kc, t * 128:(t + 1) * 128], Wr[:, kc, :], kc == 0, kc == 7, [t_hT, t_Wr], [tps])
                        lg = rt[:, 0:36]
                        V_ = "vector"
                        R = [t_rt]
                        A(V_, lambda e, ps=ps: e.tensor_tensor(out=lg, in0=ps[:, 0:36], in1=brt, op=ALU.add), reads=[tps, t_brt], writes=R)
                        gmax, ngm, gsum, gw = rt[:, 36:37], rt[:, 37:38], rt[:, 38:39], rt[:, 39:40]
                        goh, pen, ge = rt[:, 40:44], rt[:, 44:48], rt[:, 48:52]
                        el2 = rt[:, 52:84]
                        oh1, el3, oh2 = rt[:, 84:116], rt[:, 116:148], rt[:, 52:84]
                        m1, m2, dd, ee, w1, w2 = (rt[:, 148 + i:149 + i] for i in range(6))
                        A(V_, lambda e: e.reduce_max(out=gmax, in_=lg[:, 0:4], axis=AX.X), reads=R, writes=R)
                        A(V_, lambda e: e.tensor_scalar(out=ngm, in0=gmax, scalar1=-1.0, scalar2=None, op0=ALU.mult), reads=R, writes=R)
                        A(V_, lambda e: e.tensor_scalar(out=goh, in0=lg[:, 0:4], scalar1=gmax, scalar2=None, op0=ALU.is_ge), reads=R, writes=R)
                        A("scalar", lambda e: e.activation(out=ge, in_=lg[:, 0:4], func=AF.Exp, bias=ngm, scale=1.0, accum_out=gsum), reads=R, writes=R)
                        A(V_, lambda e: e.reciprocal(out=gw, in_=gsum), reads=R, writes=R)
                        A(V_, lambda e: e.tensor_scalar(out=pen, in0=goh, scalar1=-1.0, scalar2=1.0e9, op0=ALU.add, op1=ALU.mult), reads=R, writes=R)
                        A(V_, lambda e: e.tensor_tensor(out=el2.rearrange("p (g x) -> p g x", g=4), in0=lg[:, 4:36].rearrange("p (g x) -> p g x", g=4),
                                                        in1=pen.unsqueeze(2).to_broadcast([128, 4, 8]), op=ALU.add), reads=R, writes=R)
                        A(V_, lambda e: e.reduce_max(out=m1, in_=el2, axis=AX.X), reads=R, writes=R)
                        A(V_, lambda e: e.tensor_scalar(out=oh1, in0=el2, scalar1=m1, scalar2=None, op0=ALU.is_ge), reads=R, writes=R)
                        A(V_, lambda e: e.scalar_tensor_tensor(out=el3, in0=oh1, scalar=-1.0e9, in1=el2, op0=ALU.mult, op1=ALU.add), reads=R, writes=R)
                        A(V_, lambda e: e.reduce_max(out=m2, in_=el3, axis=AX.X), reads=R, writes=R)
                        A(V_, lambda e: e.tensor_scalar(out=oh2, in0=el3, scalar1=m2, scalar2=None, op0=ALU.is_ge), reads=R, writes=R)
                        A(V_, lambda e: e.tensor_tensor(out=dd, in0=m2, in1=m1, op=ALU.subtract), reads=R, writes=R)
                        A("scalar", lambda e: e.activation(out=ee, in_=dd, func=AF.Exp), reads=R, writes=R)
                        A(V_, lambda e: e.tensor_scalar(out=w1, in0=ee, scalar1=1.0, scalar2=None, op0=ALU.add), reads=R, writes=R)
                        A(V_, lambda e: e.reciprocal(out=w1, in_=w1), reads=R, writes=R)
                        A(V_, lambda e: e.tensor_tensor(out=w2, in0=ee, in1=w1, op=ALU.mult), reads=R, writes=R)
                        A(V_, lambda e: e.tensor_tensor(out=w1, in0=w1, in1=gw, op=ALU.mult), reads=R, writes=R)
                        A(V_, lambda e: e.tensor_tensor(out=w2, in0=w2, in1=gw, op=ALU.mult), reads=R, writes=R)
                        A(V_, lambda e, lt=lt: e.tensor_scalar(out=comb[:, lt, :], in0=oh1, scalar1=w1, scalar2=None, op0=ALU.mult), reads=R, writes=[t_comb])
                        A(V_, lambda e, lt=lt: e.scalar_tensor_tensor(out=comb[:, lt, :], in0=oh2, scalar=w2, in1=comb[:, lt, :], op0=ALU.mult, op1=ALU.add), reads=R + [t_comb], writes=[t_comb])
                for ex in range(32):
                    W1, t_W1, s1_ = W1r.next()
                    W3, t_W3, s3_ = W3r.next()
                    W2, t_W2, s2_ = W2r.next()
                    P.dma("gpsimd", W1, I["w_exp1"][l, ex].rearrange("(k p) n -> p k n", p=128), s1_, writes=[t_W1])
                    P.dma("gpsimd", W3, I["w_exp3"][l, ex].rearrange("(k p) n -> p k n", p=128), s3_, writes=[t_W3])
                    P.dma("gpsimd", W2, I["w_exp2"][l, ex].rearrange("(k p) n -> p k n", p=128), s2_, writes=[t_W2])
                    for c4 in range(2):
                        hid, t_hid, _ = hidr.next()
                        for fh in range(2):
                            p1, tp1 = ps_next(0, 4)
                            for kc in range(8):
                                mm(p1[:, :], W1[:, kc, fh * 128:(fh + 1) * 128], h2[:, kc, c4 * CH:(c4 + 1) * CH], kc == 0, kc == 7, [t_W1, t_h2], [tp1])
                            p3, tp3 = ps_next(0, 4)
                            for kc in range(8):
                                mm(p3[:, :], W3[:, kc, fh * 128:(fh + 1) * 128], h2[:, kc, c4 * CH:(c4 + 1) * CH], kc == 0, kc == 7, [t_W3, t_h2], [tp3])
                            sl_, t_sl, _ = slr.next()
                            A("scalar", lambda e, sl_=sl_, p1=p1: e.activation(out=sl_, in_=p1[:, :], func=AF.Silu), reads=[tp1], writes=[t_sl])
                            A("vector", lambda e, hid=hid, fh=fh, p3=p3, sl_=sl_: e.tensor_tensor(out=hid[:, fh, :], in0=p3[:, :], in1=sl_, op=ALU.mult), reads=[tp3, t_sl], writes=[t_hid])
                        for t in range(4):
                            lt = c4 * 4 + t
                            for nh in range(2):
                                po_, tpo = ps_next(4, 8)
                                for fh in range(2):
                                    mm(po_[:, :], hid[:, fh, t * 128:(t + 1) * 128], W2[:, fh, nh * 512:(nh + 1) * 512], fh == 0, fh == 1, [t_hid, t_W2], [tpo])
                                asl = acc[:, lt, nh * 512:(nh + 1) * 512]
                                if ex == 0:
                                    A("vector", lambda e, po_=po_, asl=asl, lt=lt: e.tensor_scalar(out=asl, in0=po_[:, :], scalar1=comb[:, lt, 0:1], scalar2=None, op0=ALU.mult),
                                      reads=[tpo, t_comb], writes=[(t_acc, lt)])
                                else:
                                    A("vector", lambda e, po_=po_, asl=asl, lt=lt, ex=ex: e.scalar_tensor_tensor(out=asl, in0=po_[:, :], scalar=comb[:, lt, ex:ex + 1], in1=asl, op0=ALU.mult, op1=ALU.add),
                                      reads=[tpo, t_comb, (t_acc, lt)], writes=[(t_acc, lt)])
                for lt in range(8):
                    tile = sc * 8 + lt
                    xt, t_xt, slx = xrot.next()
                    P.dma("sync", xt, XR[tile * 128:(tile + 1) * 128, :], slx, reads=[("xm", tile)], writes=[t_xt])
                    xn, t_xn, slo = xo.next()
                    A("gpsimd", lambda e, lt=lt, xn=xn: e.tensor_tensor(out=xn, in0=acc[:, lt, :], in1=GT2, op=ALU.mult), reads=[(t_acc, lt), t_mod], writes=[t_xn])
                    A("gpsimd", lambda e, xn=xn, xt=xt: e.tensor_tensor(out=xn, in0=xn, in1=xt, op=ALU.add), reads=[t_xn, t_xt], writes=[t_xn])
                    if l < depth - 1:
                        P.dma("sync", XR[tile * 128:(tile + 1) * 128, :], xn, slo, reads=[t_xn, ("xm", tile)], writes=[("xr", tile)])
                    else:
                        junk, t_junk = nb["junk"]
                        ssq, t_ssq = nb["ss"]
                        A("scalar", lambda e, xn=xn: e.activation(out=junk, in_=xn, func=AF.Square, accum_out=ssq[:, 0:1]), reads=[t_xn], writes=[t_junk, t_ssq])
                        rstd_from(ssq[:, 0:1], ssq[:, 1:2], float(D), [t_ssq], [t_ssq], ssq[:, 2:3])
                        A("vector", lambda e, xn=xn: e.scalar_tensor_tensor(out=xn, in0=xn, scalar=ssq[:, 1:2], in1=gfin, op0=ALU.mult, op1=ALU.mult),
                          reads=[t_xn, t_ssq, t_gfin], writes=[t_xn])
                        out_dmas.append(P.dma("sync", y_out[tile * 128:(tile + 1) * 128, :], xn, slo, reads=[t_xn], writes=[("y", tile)]))
            AR.release(mF); P.next_slot = sl_mF
            P.barrier()

        if not out_dmas:
            d0, t_d0 = AR.alloc([8], F32)
            A("vector", lambda e: e.memset(d0, 0.0), writes=[t_d0])
            out_dmas.append(P.dma("sync", y_out[0:128, 0:8], d0, P.slot(), reads=[t_d0]))
        finals = [o for o in P.dma_last if o is not None]
        P.emit(final_waits=finals)
    return nc


_CACHE = {}


def kernel(**inputs):
    if "nc" not in _CACHE:
        _CACHE["nc"] = build_program()
        _CACHE["consts"] = host_consts()
    nc = _CACHE["nc"]
    consts = _CACHE["consts"]
    x = np.ascontiguousarray(np.asarray(inputs["x"], dtype=np.float32))
    c = np.ascontiguousarray(np.asarray(inputs["c"], dtype=np.float32))
    shared = {name: np.ascontiguousarray(np.asarray(inputs[name], dtype=np.float32)) for name, _ in WEIGHT_SPECS}
    shared.update(consts)
    in_maps = []
    for b in range(8):
        m = dict(shared)
        m["x"] = x[b]
        m["c"] = c[b]
        in_maps.append(m)
    res = run_bass_kernel_spmd(nc, in_maps, core_ids=list(range(8)))
    out = np.stack([np.asarray(res.results[b]["y"], dtype=np.float32) for b in range(8)], axis=0)
    return out
```

```python
import math
from contextlib import ExitStack
import numpy as np
import concourse.bass as bass
import concourse.mybir as mybir
from concourse.bass_utils import run_bass_kernel_spmd

F32 = mybir.dt.float32
BF16 = mybir.dt.bfloat16
AF = mybir.ActivationFunctionType
ALU = mybir.AluOpType
AX = mybir.AxisListType
ENGS = ("sync", "scalar", "vector", "gpsimd", "tensor")

S = 4096
D = 1024
NT = 32
NCH = 8
CH = 512
DEPTH = 4
D_IN = 6368
EPS = 1e-6
NEG = -30000.0


class Op:
    __slots__ = ("eng", "fn", "deps", "signal", "val", "dsem", "dval")

    def __init__(self, eng, fn):
        self.eng = eng
        self.fn = fn
        self.deps = []
        self.signal = False
        self.val = None
        self.dsem = None
        self.dval = None


class Prog:
    def __init__(self, nc, n_dma_sems=56):
        self.nc = nc
        self.ops = {e: [] for e in ENGS}
        self.last_writer = {}
        self.readers = {}
        self.n_dma_sems = n_dma_sems
        self.dma_counts = [0] * n_dma_sems
        self.dma_last = [None] * n_dma_sems
        self.next_slot = 0
        self.pending = {e: [] for e in ENGS}

    def slot(self):
        s = self.next_slot
        self.next_slot += 1
        self.max_slot = max(getattr(self, "max_slot", 0), self.next_slot)
        assert s < self.n_dma_sems, "out of dma sems"
        return s

    def _dep(self, op, d):
        if d is None or d is op:
            return
        if d.dsem is None and d.eng == op.eng and op.eng == "tensor":
            return
        for x in op.deps:
            if x is d:
                return
        op.deps.append(d)
        if d.dsem is None:
            d.signal = True

    def _track(self, op, reads, writes):
        for b in reads:
            w = self.last_writer.get(b)
            if w is not None:
                self._dep(op, w)
        for b in writes:
            w = self.last_writer.get(b)
            if w is not None:
                self._dep(op, w)
            rd = self.readers.get(b)
            if rd:
                for r in rd.values():
                    self._dep(op, r)
        for b in reads:
            key = op.eng if op.dsem is None else ("d", op.dsem)
            self.readers.setdefault(b, {})[key] = op
        for b in writes:
            self.last_writer[b] = op
            self.readers[b] = {}
        pend = self.pending[op.eng]
        if pend:
            for d in pend:
                self._dep(op, d)
            self.pending[op.eng] = []

    def add(self, eng, fn, reads=(), writes=()):
        op = Op(eng, fn)
        self._track(op, reads, writes)
        self.ops[eng].append(op)
        return op

    def dma(self, eng, out, in_, sem, reads=(), writes=(), **kw):
        def fn(e, out=out, in_=in_, kw=kw):
            return e.dma_start(out=out, in_=in_, **kw)
        op = Op(eng, fn)
        op.dsem = sem
        self.dma_counts[sem] += 16
        op.dval = self.dma_counts[sem]
        self.dma_last[sem] = op
        self._track(op, reads, writes)
        self.ops[eng].append(op)
        return op

    def barrier(self):
        lasts = []
        for e in ENGS:
            for op in reversed(self.ops[e]):
                if op.dsem is None:
                    lasts.append(op)
                    break
        for o in self.dma_last:
            if o is not None:
                lasts.append(o)
        for e in ENGS:
            self.pending[e] = list(lasts)

    def emit(self, final_waits=()):
        nc = self.nc
        for e in ENGS:
            c = 0
            for op in self.ops[e]:
                if op.dsem is None and op.signal:
                    c += 1
                    op.val = c
        with ExitStack() as st:
            esem = {e: st.enter_context(nc.semaphore("s_" + e)) for e in ENGS}
            dsem = [st.enter_context(nc.semaphore("d_%d" % i)) for i in range(max(1, self.max_slot))]
            block = st.enter_context(nc.Block())

            def run(e_name):
                def body(eng):
                    waited = {}
                    for op in self.ops[e_name]:
                        for d in op.deps:
                            if d.dsem is not None:
                                key, v, s = ("d", d.dsem), d.dval, dsem[d.dsem]
                            else:
                                key, v, s = ("e", d.eng), d.val, esem[d.eng]
                            if waited.get(key, 0) >= v:
                                continue
                            waited[key] = v
                            eng.wait_ge(s, v)
                        inst = op.fn(eng)
                        if op.dsem is not None:
                            inst.then_inc(dsem[op.dsem], 16)
                        elif op.signal:
                            inst.then_inc(esem[e_name], 1)
                    if e_name == "sync":
                        for d in final_waits:
                            eng.wait_ge(dsem[d.dsem], d.dval)
                return body

            block.sync(run("sync"))
            block.scalar(run("scalar"))
            block.vector(run("vector"))
            block.gpsimd(run("gpsimd"))
            block.tensor(run("tensor"))


class Arena:
    def __init__(self, base, nwords):
        self.base = base
        self.n = nwords
        self.off = 0
        self.cnt = 0

    def mark(self):
        return self.off

    def release(self, m):
        self.off = m

    def alloc(self, free, dt=F32, parts=128):
        n = 1
        for f in free:
            n *= f
        words = n if dt == F32 else (n + 1) // 2
        words = (words + 7) // 8 * 8
        assert self.off + words <= self.n, "arena overflow %d + %d > %d" % (self.off, words, self.n)
        ap = self.base[:, self.off:self.off + (n if dt == F32 else (n + 1) // 2)]
        if dt != F32:
            ap = ap.bitcast(dt)
        if len(free) == 2:
            ap = ap.rearrange("p (a b) -> p a b", a=free[0])
        elif len(free) == 3:
            ap = ap.rearrange("p (a b c) -> p a b c", a=free[0], b=free[1])
        elif len(free) == 4:
            ap = ap.rearrange("p (a b c d) -> p a b c d", a=free[0], b=free[1], c=free[2])
        self.off += words
        self.cnt += 1
        return ap, ("sb", self.cnt)


class Rot:
    def __init__(self, P, arena, n, free, dt=F32):
        self.bufs = []
        for _ in range(n):
            ap, tok = arena.alloc(free, dt)
            self.bufs.append((ap, tok, P.slot()))
        self.i = 0

    def next(self):
        b = self.bufs[self.i % len(self.bufs)]
        self.i += 1
        return b


def _rope_np(pos, dim):
    inv = (np.float32(10000.0) ** (-(np.arange(0, dim, 2, dtype=np.float32)) / np.float32(dim))).astype(np.float32)
    ang = pos.astype(np.float32)[:, None] * inv[None, :]
    return np.cos(ang).astype(np.float32), np.sin(ang).astype(np.float32)


def host_consts():
    pos = np.arange(S)
    ct, st_ = _rope_np(pos, 32)
    cr, sr = _rope_np(pos // 64, 32)
    cc, sc = _rope_np(pos % 64, 32)
    ropeC = np.zeros((2, 96, S), np.float32)
    ropeC[0, 0:64] = 1.0
    ropeC[0, 64:80] = ct.T
    ropeC[0, 80:96] = ct.T
    ropeC[1, 64:80] = st_.T
    ropeC[1, 80:96] = st_.T
    cosD = np.concatenate([cr.T, cr.T, cc.T, cc.T], 0)
    sinD = np.concatenate([sr.T, sr.T, sc.T, sc.T], 0)
    ropeD = np.stack([np.concatenate([cosD, cosD], 0), np.concatenate([sinD, sinD], 0)], 0).astype(np.float32)
    al = np.zeros((128, 3, 4, 128), np.float32)
    slopes = 2.0 ** (-8.0 * (np.arange(4, dtype=np.float32) + 1.0) / 4.0)
    i = np.arange(128)[None, :]
    m = np.arange(128)[:, None]
    for o in range(3):
        rel = (o - 1) * 128 + m - i
        for h in range(4):
            al[:, o, h, :] = np.where(np.abs(rel) <= 128, -slopes[h] * np.abs(rel).astype(np.float32), NEG)
    e2 = np.zeros((32, 64, 128), np.float32)
    for q in range(64):
        cs = min(max(q - 8, 0), 48)
        for k in range(64):
            valid = (k >= cs) and (k < cs + 16)
            if valid:
                idx = min(max(k - q, -15), 15) + 15
                e2[idx, q, k] = 1.0
                e2[idx, q, 64 + k] = 1.0
            else:
                e2[31, q, k] = NEG
                e2[31, q, 64 + k] = NEG
    return {
        "k_ident": np.eye(128, dtype=np.float32),
        "k_ropeC": ropeC,
        "k_ropeD": ropeD,
        "k_al": al,
        "k_e2": e2,
    }


WEIGHT_SPECS = [
    ("w_ada", [DEPTH, D, 6 * D]), ("b_ada", [DEPTH, 6 * D]), ("norm1_g", [DEPTH, D]), ("norm2_g", [DEPTH, D]),
    ("w_in", [DEPTH, D, D_IN]), ("na_rel_bias", [DEPTH, 4, 15, 31]), ("win_sink", [DEPTH, 4]),
    ("mla_q_norm_g", [DEPTH, 192]), ("mla_kv_norm_g", [DEPTH, 256]), ("w_uq", [DEPTH, 192, 384]),
    ("w_ukv", [DEPTH, 256, 512]), ("ax_q_norm_g", [DEPTH, 64]), ("ax_k_norm_g", [DEPTH, 64]),
    ("w_branch", [DEPTH, 4, 256, D]), ("w_out", [DEPTH, D, D]), ("w_group", [DEPTH, D, 4]), ("b_group", [DEPTH, 4]),
    ("w_router", [DEPTH, D, 32]), ("b_router", [DEPTH, 32]), ("w_exp1", [DEPTH, 32, D, 256]),
    ("w_exp3", [DEPTH, 32, D, 256]), ("w_exp2", [DEPTH, 32, 256, D]), ("final_norm_g", [D]),
]
CONST_SPECS = [("k_ident", [128, 128]), ("k_ropeC", [2, 96, S]), ("k_ropeD", [2, 128, S]),
               ("k_al", [128, 3, 4, 128]), ("k_e2", [32, 64, 128])]


def build_program(depth=DEPTH, debug=False, stop_after=None):
    nc = bass.Bass("TRN2", target_bir_lowering=False)
    I = {}
    I["x"] = nc.dram_tensor("x", [S, D], F32, kind="ExternalInput").ap()
    I["c"] = nc.dram_tensor("c", [D], F32, kind="ExternalInput").ap()
    for name, shp in WEIGHT_SPECS + CONST_SPECS:
        if len(shp) > 1 and shp[0] == DEPTH and name not in ("k_e2",):
            shp = [depth] + list(shp[1:])
        I[name] = nc.dram_tensor(name, shp, F32, kind="ExternalInput").ap()
    y_out = nc.dram_tensor("y", [S, D], F32, kind="ExternalOutput").ap()
    skind = "ExternalOutput" if debug else "Internal"

    def scratch(name, shp, dt):
        return nc.dram_tensor(name, shp, dt, kind=skind).ap()

    XR = scratch("xres", [S, D], F32)
    QT_A = scratch("qt_a", [2, 128, S], BF16)
    KT_A = scratch("kt_a", [2, 128, S], BF16)
    QT_B = scratch("qt_b", [2, 128, S], BF16)
    KT_B = scratch("kt_b", [1, 128, S], BF16)
    QT_D = scratch("qt_d", [2, 128, S], BF16)
    KT_D = scratch("kt_d", [1, 128, S], BF16)
    QT_C = scratch("qt_c", [4, 96, S], BF16)
    KT_C = scratch("kt_c", [4, 96, S], BF16)
    V_ABD = scratch("v_abd", [S, 8, 128], BF16)
    V_C = scratch("v_c", [S, 4, 128], BF16)
    YT = scratch("yt", [4, 128, 2, S], BF16)

    P = Prog(nc)
    st = ExitStack()
    with st:
        arena_t = st.enter_context(nc.sbuf_tensor("arena", [128, 52000], F32))
        AR = Arena(arena_t, 52000)
        PS = [st.enter_context(nc.psum_tensor("ps%d" % i, [128, 512], F32)) for i in range(2)]
        PB = [None] + [st.enter_context(nc.psum_tensor("pb%d" % k, [128, 1024], F32)) for k in range(1, 4)]
        for k in range(1, 4):
            PS.append(PB[k][:, 0:512])
            PS.append(PB[k][:, 512:1024])
        PST = [("ps", i) for i in range(8)]

        def A(eng, fn, reads=(), writes=()):
            return P.add(eng, fn, reads, writes)

        def mm(out, lhsT, rhs, start, stop, reads, writes, **kw):
            return P.add("tensor", lambda e: e.matmul(out, lhsT=lhsT, rhs=rhs, start=start, stop=stop, **kw), reads, writes)

        identb, t_identb = AR.alloc([128], BF16)
        ones_f, t_ones_f = AR.alloc([128], F32)
        bd_f, t_bd_f = AR.alloc([128], F32)
        eps_c, t_eps = AR.alloc([1], F32)
        cact, t_cact = AR.alloc([8], F32)
        crep, t_crep = AR.alloc([8, 128], F32)
        mod_sb, t_mod = AR.alloc([6 * D], F32)
        G1, t_G1 = AR.alloc([D], F32)
        G2, t_G2 = AR.alloc([D], F32)
        s_c = P.slot()
        P.dma("gpsimd", identb, I["k_ident"], s_c, writes=[t_identb])
        A("vector", lambda e: e.memset(ones_f, 1.0), writes=[t_ones_f])
        A("vector", lambda e: e.memset(bd_f, 0.0), writes=[t_bd_f])
        A("vector", lambda e: e.memset(bd_f[0:64, 0:64], 1.0), writes=[t_bd_f])
        A("vector", lambda e: e.memset(bd_f[64:128, 64:128], 1.0), writes=[t_bd_f])
        A("vector", lambda e: e.memset(eps_c, EPS), writes=[t_eps])
        s_c2 = P.slot()
        P.dma("sync", cact, I["c"].rearrange("(k p) -> p k", p=128), s_c2, writes=[t_cact], allow_slow_non_contiguous=True)
        A("scalar", lambda e: e.activation(out=cact, in_=cact, func=AF.Silu), reads=[t_cact], writes=[t_cact])
        for kc in range(8):
            A("vector", lambda e, kc=kc: e.tensor_scalar(out=crep[:, kc, :], in0=ones_f, scalar1=cact[:, kc:kc + 1], scalar2=None, op0=ALU.mult),
              reads=[t_cact, t_ones_f], writes=[t_crep])
        base_mark = AR.mark()
        base_slot = P.next_slot

        out_dmas = []

        def rstd_from(ss_ap, out_ap, n, reads, writes, tmp):
            p = ss_ap.shape[0]
            A("scalar", lambda e: e.activation(out=tmp, in_=ss_ap, func=AF.Sqrt, bias=eps_c[0:p, 0:1], scale=1.0 / n), reads=list(reads) + [t_eps], writes=writes)
            A("vector", lambda e: e.reciprocal(out=out_ap, in_=tmp), reads=writes, writes=writes)

        def norm_chunk(xsrc, xtoks, ch, G, SH, gs_tok, hT, t_hT, xrot, nb):
            xts = []
            for t in range(4):
                tile = ch * 4 + t
                xt, t_xt, sl = xrot.next()
                P.dma("sync", xt, xsrc[tile * 128:(tile + 1) * 128, :], sl, reads=[(xtoks, tile)], writes=[t_xt])
                junk, t_junk = nb["junk"]
                ssq, t_ssq = nb["ss"]
                A("scalar", lambda e, xt=xt: e.activation(out=junk, in_=xt, func=AF.Square, accum_out=ssq[:, 0:1]),
                  reads=[t_xt], writes=[t_junk, t_ssq])
                rstd_from(ssq[:, 0:1], ssq[:, 1:2], float(D), [t_ssq], [t_ssq], ssq[:, 2:3])
                hf, t_hf = nb["hf"]
                A("vector", lambda e, xt=xt: e.scalar_tensor_tensor(out=hf, in0=xt, scalar=ssq[:, 1:2], in1=G, op0=ALU.mult, op1=ALU.mult),
                  reads=[t_xt, t_ssq] + gs_tok, writes=[t_hf])
                hb, t_hb = nb["hb"].next()[:2]
                A("gpsimd", lambda e, hb=hb: e.tensor_tensor(out=hb, in0=hf, in1=SH, op=ALU.add), reads=[t_hf] + gs_tok, writes=[t_hb])
                pT = PS[0][:, :].bitcast(BF16)
                for kc in range(8):
                    A("tensor", lambda e, kc=kc, hb=hb: e.transpose(out=pT[:, kc * 128:(kc + 1) * 128], in_=hb[:, kc * 128:(kc + 1) * 128], identity=identb),
                      reads=[t_hb, t_identb], writes=[PST[0]])
                A("scalar", lambda e, t=t: e.copy(out=hT[:, :, t * 128:(t + 1) * 128], in_=pT.rearrange("p (k t) -> p k t", k=8)),
                  reads=[PST[0]], writes=[t_hT])
                xts.append((xt, t_xt, sl))
            return xts

        psrot = [0]

        def ps_next(lo=1, hi=8):
            i = lo + psrot[0] % (hi - lo)
            psrot[0] += 1
            return PS[i], PST[i]

        for l in range(depth):
            xsrc = I["x"] if l == 0 else XR
            xtk = ("xin" if l == 0 else "xr")
            AR.release(base_mark)
            P.next_slot = base_slot
            P.barrier()
            m0 = AR.mark(); sl_m0 = P.next_slot
            wst = Rot(P, AR, 2, [3072], F32)
            brow, t_brow = AR.alloc([6 * D], F32)
            n1g, t_n1g = AR.alloc([D], F32)
            n2g, t_n2g = AR.alloc([D], F32)
            sb_ = P.slot()
            P.dma("sync", brow[0:1, :], I["b_ada"][l:l + 1, :], sb_, writes=[t_brow])
            sn1 = P.slot()
            P.dma("sync", n1g, I["norm1_g"][l:l + 1, :].partition_broadcast(128), sn1, writes=[t_n1g])
            sn2 = P.slot()
            P.dma("sync", n2g, I["norm2_g"][l:l + 1, :].partition_broadcast(128), sn2, writes=[t_n2g])
            for half in range(2):
                for kc in range(8):
                    w, t_w, sl = wst.next()
                    P.dma("sync", w, I["w_ada"][l, kc * 128:(kc + 1) * 128, half * 3072:(half + 1) * 3072], sl, writes=[t_w])
                    for n in range(6):
                        mm(PS[n][:, :], crep[:, kc, :], w[:, n * 512:(n + 1) * 512], kc == 0, False, [t_crep, t_w], [PST[n]])
                for n in range(6):
                    col = half * 3072 + n * 512
                    mm(PS[n][:, :], ones_f[0:1, :], brow[0:1, col:col + 512], False, True, [t_ones_f, t_brow], [PST[n]])
                    A("scalar", lambda e, n=n, col=col: e.copy(out=mod_sb[:, col:col + 512], in_=PS[n][:, :]), reads=[PST[n]], writes=[t_mod])
            A("vector", lambda e: e.scalar_tensor_tensor(out=G1, in0=mod_sb[:, D:2 * D], scalar=1.0, in1=n1g, op0=ALU.add, op1=ALU.mult),
              reads=[t_mod, t_n1g], writes=[t_G1])
            A("vector", lambda e: e.scalar_tensor_tensor(out=G2, in0=mod_sb[:, 4 * D:5 * D], scalar=1.0, in1=n2g, op0=ALU.add, op1=ALU.mult),
              reads=[t_mod, t_n2g], writes=[t_G2])
            SH1 = mod_sb[:, 0:D]
            GT1 = mod_sb[:, 2 * D:3 * D]
            SH2 = mod_sb[:, 3 * D:4 * D]
            GT2 = mod_sb[:, 5 * D:6 * D]
            AR.release(m0); P.next_slot = sl_m0
            P.barrier()

            mA = AR.mark(); sl_mA = P.next_slot
            WC = 2304
            Wfm, t_Wfm = AR.alloc([8, WC], BF16)
            Wv, t_Wv = AR.alloc([8, 512], BF16)
            win_v = I["w_in"][l].rearrange("(k p) n -> p k n", p=128)
            sw = P.slot()

            def wload(dst0, src0, w):
                P.dma("gpsimd", Wfm[:, :, dst0:dst0 + w], win_v[:, :, src0:src0 + w], sw, writes=[t_Wfm])

            O_QA, O_KA, O_QB, O_KB, O_QD, O_KD, O_CQ, O_CKV, O_KR = 0, 256, 512, 768, 896, 1408, 1664, 1856, 2112
            wload(O_QA, 0, 256)
            wload(O_KA, 256, 256)
            for g in range(2):
                for j, h in enumerate((g, g + 2)):
                    wload(O_QB + g * 128 + j * 64, 768 + h * 64, 64)
            wload(O_KB, 1024, 128)
            for g in range(2):
                for j, h in enumerate((g, g + 2)):
                    wload(O_QD + g * 256 + j * 64, 1760 + h * 64, 64)
            wload(O_KD, 2016, 128)
            wload(O_CQ, 1280, 192)
            wload(O_CKV, 1472, 256)
            A("vector", lambda e: e.memset(Wfm[:, :, O_KR:O_KR + 64], 0.0), writes=[t_Wfm])
            A("vector", lambda e: e.memset(Wfm[:, :, O_KR + 96:O_KR + 160], 0.0), writes=[t_Wfm])
            wload(O_KR + 64, 1728, 32)
            P.dma("gpsimd", Wv[:, :, 0:256], win_v[:, :, 512:768], sw, writes=[t_Wv])
            P.dma("gpsimd", Wv[:, :, 256:384], win_v[:, :, 1152:1280], sw, writes=[t_Wv])
            P.dma("gpsimd", Wv[:, :, 384:512], win_v[:, :, 2144:2272], sw, writes=[t_Wv])

            def make_rot(src_o, dst_o, nheads):
                sv = Wfm[:, :, src_o:src_o + 64 * nheads].rearrange("p k (q two s) -> p k q two s", two=2, s=16)
                dv = Wfm[:, :, dst_o:dst_o + 64 * nheads].rearrange("p k (q two s) -> p k q two s", two=2, s=16)
                for kc in range(8):
                    A("vector", lambda e, kc=kc: e.tensor_scalar(out=dv[:, kc, :, 0, :], in0=sv[:, kc, :, 1, :], scalar1=-1.0, scalar2=None, op0=ALU.mult),
                      reads=[t_Wfm], writes=[t_Wfm])
                    A("gpsimd", lambda e, kc=kc: e.tensor_copy(out=dv[:, kc, :, 1, :], in_=sv[:, kc, :, 0, :]), reads=[t_Wfm], writes=[t_Wfm])

            make_rot(O_QD, O_QD + 128, 2)
            make_rot(O_QD + 256, O_QD + 384, 2)
            make_rot(O_KD, O_KD + 128, 2)
            for kc in range(8):
                A("vector", lambda e, kc=kc: e.tensor_scalar(out=Wfm[:, kc, O_KR + 160:O_KR + 176], in0=Wfm[:, kc, O_KR + 80:O_KR + 96], scalar1=-1.0, scalar2=None, op0=ALU.mult),
                  reads=[t_Wfm], writes=[t_Wfm])
                A("gpsimd", lambda e, kc=kc: e.tensor_copy(out=Wfm[:, kc, O_KR + 176:O_KR + 192], in_=Wfm[:, kc, O_KR + 64:O_KR + 80]), reads=[t_Wfm], writes=[t_Wfm])

            wuq_f, t_wuqf = AR.alloc([2, 384], F32)
            wukv_f, t_wukvf = AR.alloc([2, 512], F32)
            gq, t_gq = AR.alloc([2], F32)
            gkv, t_gkv = AR.alloc([2], F32)
            Wuq, t_Wuq = AR.alloc([2, 4, 96], BF16)
            Wuqr, t_Wuqr = AR.alloc([2, 4, 96], BF16)
            Wukk, t_Wukk = AR.alloc([2, 4, 64], BF16)
            Wukv_v, t_Wukv = AR.alloc([2, 4, 64], BF16)
            s1 = P.slot()
            A("vector", lambda e: e.memset(wuq_f, 0.0), writes=[t_wuqf])
            A("vector", lambda e: e.memset(gq, 0.0), writes=[t_gq])
            P.dma("sync", wuq_f[:, 0, :], I["w_uq"][l, 0:128, :], s1, writes=[t_wuqf])
            P.dma("sync", wuq_f[0:64, 1, :], I["w_uq"][l, 128:192, :], s1, writes=[t_wuqf])
            P.dma("sync", wukv_f, I["w_ukv"][l].rearrange("(k p) n -> p k n", p=128), s1, writes=[t_wukvf])
            P.dma("sync", gq[:, 0:1], I["mla_q_norm_g"][l, 0:128].rearrange("(p o) -> p o", o=1), s1, writes=[t_gq])
            P.dma("sync", gq[0:64, 1:2], I["mla_q_norm_g"][l, 128:192].rearrange("(p o) -> p o", o=1), s1, writes=[t_gq])
            P.dma("sync", gkv, I["mla_kv_norm_g"][l].rearrange("(k p) -> p k", p=128), s1, writes=[t_gkv], allow_slow_non_contiguous=True)
            wuq4 = wuq_f.rearrange("p k (h c) -> p k h c", h=4)
            wukv4 = wukv_f.rearrange("p k (h c) -> p k h c", h=4)
            A("vector", lambda e: e.memset(Wuqr, 0.0), writes=[t_Wuqr])
            for k in range(2):
                A("vector", lambda e, k=k: e.tensor_scalar(out=Wuq[:, k], in0=wuq4[:, k], scalar1=gq[:, k:k + 1], scalar2=None, op0=ALU.mult),
                  reads=[t_wuqf, t_gq], writes=[t_Wuq])
                A("vector", lambda e, k=k: e.tensor_scalar(out=Wuqr[:, k, :, 64:80], in0=wuq4[:, k, :, 80:96], scalar1=gq[:, k:k + 1], scalar2=-1.0, op0=ALU.mult, op1=ALU.mult),
                  reads=[t_wuqf, t_gq], writes=[t_Wuqr])
                A("vector", lambda e, k=k: e.tensor_scalar(out=Wuqr[:, k, :, 80:96], in0=wuq4[:, k, :, 64:80], scalar1=gq[:, k:k + 1], scalar2=None, op0=ALU.mult),
                  reads=[t_wuqf, t_gq], writes=[t_Wuqr])
                A("vector", lambda e, k=k: e.tensor_scalar(out=Wukk[:, k], in0=wukv4[:, k, :, 0:64], scalar1=gkv[:, k:k + 1], scalar2=None, op0=ALU.mult),
                  reads=[t_wukvf, t_gkv], writes=[t_Wukk])
                A("vector", lambda e, k=k: e.tensor_scalar(out=Wukv_v[:, k], in0=wukv4[:, k, :, 64:128], scalar1=gkv[:, k:k + 1], scalar2=None, op0=ALU.mult),
                  reads=[t_wukvf, t_gkv], writes=[t_Wukv])
            gD, t_gD = AR.alloc([4], F32)
            for ci, nm in ((0, "ax_q_norm_g"), (2, "ax_k_norm_g")):
                for half in range(2):
                    P.dma("sync", gD[half * 64:(half + 1) * 64, ci:ci + 1], I[nm][l, :].rearrange("(p o) -> p o", o=1), s1, writes=[t_gD])
                    for blk, src in enumerate((1, 0, 3, 2)):
                        P.dma("sync", gD[half * 64 + blk * 16:half * 64 + (blk + 1) * 16, ci + 1:ci + 2],
                              I[nm][l, src * 16:(src + 1) * 16].rearrange("(p o) -> p o", o=1), s1, writes=[t_gD])
            A("vector", lambda e: e.tensor_scalar(out=gD[:, 0:2], in0=gD[:, 0:2], scalar1=0.125, scalar2=None, op0=ALU.mult), reads=[t_gD], writes=[t_gD])

            hTr = Rot(P, AR, 2, [8, CH], BF16)
            xrot = Rot(P, AR, 2, [D], F32)
            nb = {"junk": AR.alloc([D], BF16), "ss": AR.alloc([4], F32), "hf": AR.alloc([D], F32), "hb": Rot(P, AR, 2, [D], BF16)}
            stg = Rot(P, AR, 4, [CH], BF16)
            vst = Rot(P, AR, 2, [8, 128], BF16)
            vcst = Rot(P, AR, 2, [4, 128], BF16)
            for b in vst.bufs + vcst.bufs:
                A("vector", lambda e, b=b: e.memset(b[0], 1.0), writes=[b[1]])
            tabr = Rot(P, AR, 2, [2, CH], F32)
            tabc = Rot(P, AR, 2, [2, CH], F32)
            sq_a, t_sqa = AR.alloc([2, CH], F32)
            rb, t_rb = AR.alloc([CH], F32)
            rtmp, t_rtmp = AR.alloc([CH], F32)
            f1, t_f1 = AR.alloc([CH], F32)
            f2, t_f2 = AR.alloc([CH], F32)
            cqn, t_cqn = AR.alloc([2, CH], BF16)
            ckvn, t_ckvn = AR.alloc([2, CH], BF16)
            kpe, t_kpe = AR.alloc([CH], BF16)

            def fm_group(ps, col0, M, hT, t_hT, tps):
                for kc in range(8):
                    mm(ps[0:M, :], Wfm[:, kc, col0:col0 + M], hT[:, kc, :], kc == 0, kc == 7, [t_Wfm, t_hT], [tps])

            for ch in range(NCH):
                cs = slice(ch * CH, (ch + 1) * CH)
                hT, t_hT, _ = hTr.next()
                norm_chunk(xsrc, xtk, ch, G1, SH1, [t_G1, t_mod], hT, t_hT, xrot, nb)
                simple = [(O_QA, QT_A, 0, 0.125, "qa"), (O_QA + 128, QT_A, 1, 0.125, "qa"), (O_KA, KT_A, 0, 1.0, "ka"), (O_KA + 128, KT_A, 1, 1.0, "ka"),
                          (O_QB, QT_B, 0, 0.125, "qb"), (O_QB + 128, QT_B, 1, 0.125, "qb"), (O_KB, KT_B, 0, 1.0, "kb")]
                for col0, dst, gi, scl, nm in simple:
                    ps, tps = ps_next(1, 5)
                    fm_group(ps, col0, 128, hT, t_hT, tps)
                    sg, t_sg, sl = stg.next()
                    A("scalar", lambda e, ps=ps, sg=sg, scl=scl: e.mul(out=sg, in_=ps[:, :], mul=scl), reads=[tps], writes=[t_sg])
                    P.dma("gpsimd", dst[gi, :, cs], sg, sl, reads=[t_sg], writes=[(nm, gi, ch)])
                tb, t_tb, sl = tabr.next()
                P.dma("sync", tb, I["k_ropeD"][:, :, cs].rearrange("a p t -> p a t"), sl, writes=[t_tb])
                for col0, dst, gi, gc, nm in ((O_QD, QT_D, 0, 0, "qd"), (O_QD + 256, QT_D, 1, 0, "qd"), (O_KD, KT_D, 0, 2, "kd")):
                    psa, tpa = ps_next(1, 5)
                    fm_group(psa, col0, 128, hT, t_hT, tpa)
                    psb, tpb = ps_next(1, 5)
                    fm_group(psb, col0 + 128, 128, hT, t_hT, tpb)
                    A("scalar", lambda e, psa=psa: e.activation(out=sq_a[:, 0, :], in_=psa[:, :], func=AF.Square), reads=[tpa], writes=[t_sqa])
                    mm(PS[5][:, :], bd_f, sq_a[:, 0, :], True, True, [t_bd_f, t_sqa], [PST[5]])
                    rstd_from(PS[5][:, :], rb, 64.0, [PST[5]], [t_rb], rtmp)
                    A("vector", lambda e, psa=psa, gc=gc, tb=tb: e.scalar_tensor_tensor(out=f1, in0=psa[:, :], scalar=gD[:, gc:gc + 1], in1=tb[:, 0, :], op0=ALU.mult, op1=ALU.mult),
                      reads=[tpa, t_gD, t_tb], writes=[t_f1])
                    A("vector", lambda e, psb=psb, gc=gc, tb=tb: e.scalar_tensor_tensor(out=f2, in0=psb[:, :], scalar=gD[:, gc + 1:gc + 2], in1=tb[:, 1, :], op0=ALU.mult, op1=ALU.mult),
                      reads=[tpb, t_gD, t_tb], writes=[t_f2])
                    A("gpsimd", lambda e: e.tensor_tensor(out=f1, in0=f1, in1=f2, op=ALU.add), reads=[t_f1, t_f2], writes=[t_f1])
                    sg, t_sg, sl = stg.next()
                    A("vector", lambda e, sg=sg: e.tensor_tensor(out=sg, in0=f1, in1=rb, op=ALU.mult), reads=[t_f1, t_rb], writes=[t_sg])
                    P.dma("gpsimd", dst[gi, :, cs], sg, sl, reads=[t_sg], writes=[(nm, gi, ch)])
                tc, t_tc, sl = tabc.next()
                P.dma("sync", tc[0:96], I["k_ropeC"][:, :, cs].rearrange("a p t -> p a t"), sl, writes=[t_tc])
                for (col0, widths, dstn, t_dn, nfeat) in ((O_CQ, (128, 64), cqn, t_cqn, 192.0), (O_CKV, (128, 128), ckvn, t_ckvn, 256.0)):
                    pss = []
                    for k, w in enumerate(widths):
                        ps, tps = ps_next(1, 5)
                        fm_group(ps, col0 + k * 128, w, hT, t_hT, tps)
                        A("scalar", lambda e, ps=ps, k=k, w=w: e.activation(out=sq_a[0:w, k, :], in_=ps[0:w, :], func=AF.Square), reads=[tps], writes=[t_sqa])
                        pss.append((ps, tps, w))
                    for k, w in enumerate(widths):
                        mm(PS[5][:, :], ones_f[0:w, :], sq_a[0:w, k, :], k == 0, k == 1, [t_ones_f, t_sqa], [PST[5]])
                    rstd_from(PS[5][:, :], rb, nfeat, [PST[5]], [t_rb], rtmp)
                    for k, (ps, tps, w) in enumerate(pss):
                        A("vector", lambda e, ps=ps, k=k, w=w, dstn=dstn: e.tensor_tensor(out=dstn[0:w, k, :], in0=ps[0:w, :], in1=rb[0:w, :], op=ALU.mult),
                          reads=[tps, t_rb], writes=[t_dn])
                psa, tpa = ps_next(1, 5)
                fm_group(psa, O_KR, 96, hT, t_hT, tpa)
                psb, tpb = ps_next(1, 5)
                fm_group(psb, O_KR + 96, 96, hT, t_hT, tpb)
                A("vector", lambda e, psa=psa, tc=tc: e.tensor_tensor(out=f1[64:96, :], in0=psa[64:96, :], in1=tc[64:96, 0, :], op=ALU.mult), reads=[tpa, t_tc], writes=[t_f1])
                A("vector", lambda e, psb=psb, tc=tc: e.tensor_tensor(out=f2[64:96, :], in0=psb[64:96, :], in1=tc[64:96, 1, :], op=ALU.mult), reads=[tpb, t_tc], writes=[t_f2])
                A("gpsimd", lambda e: e.tensor_tensor(out=kpe[64:96, :], in0=f1[64:96, :], in1=f2[64:96, :], op=ALU.add), reads=[t_f1, t_f2], writes=[t_kpe])
                for h in range(4):
                    psa, tpa = ps_next(1, 5)
                    psb, tpb = ps_next(1, 5)
                    for k, w in enumerate((128, 64)):
                        mm(psa[0:96, :], Wuq[0:w, k, h, :], cqn[0:w, k, :], k == 0, k == 1, [t_Wuq, t_cqn], [tpa])
                    for k, w in enumerate((128, 64)):
                        mm(psb[0:96, :], Wuqr[0:w, k, h, :], cqn[0:w, k, :], k == 0, k == 1, [t_Wuqr, t_cqn], [tpb])
                    A("vector", lambda e, psa=psa, tc=tc: e.tensor_tensor(out=f1[0:96, :], in0=psa[0:96, :], in1=tc[0:96, 0, :], op=ALU.mult), reads=[tpa, t_tc], writes=[t_f1])
                    A("vector", lambda e, psb=psb, tc=tc: e.tensor_tensor(out=f2[0:96, :], in0=psb[0:96, :], in1=tc[0:96, 1, :], op=ALU.mult), reads=[tpb, t_tc], writes=[t_f2])
                    sg, t_sg, sl = stg.next()
                    A("gpsimd", lambda e, sg=sg: e.tensor_tensor(out=sg[0:96, :], in0=f1[0:96, :], in1=f2[0:96, :], op=ALU.add), reads=[t_f1, t_f2], writes=[t_sg])
                    P.dma("gpsimd", QT_C[h, :, cs], sg[0:96, :], sl, reads=[t_sg], writes=[("qc", h, ch)])
                    psk, tpk = ps_next(1, 5)
                    for k in range(2):
                        mm(psk[0:64, :], Wukk[:, k, h, :], ckvn[:, k, :], k == 0, k == 1, [t_Wukk, t_ckvn], [tpk])
                    sg, t_sg, sl = stg.next()
                    A("scalar", lambda e, sg=sg, psk=psk: e.copy(out=sg[0:64, :], in_=psk[0:64, :]), reads=[tpk], writes=[t_sg])
                    A("gpsimd", lambda e, sg=sg: e.tensor_copy(out=sg[64:96, :], in_=kpe[64:96, :]), reads=[t_kpe], writes=[t_sg])
                    P.dma("gpsimd", KT_C[h, :, cs], sg[0:96, :], sl, reads=[t_sg], writes=[("kc", h, ch)])
                for t in range(4):
                    tile = ch * 4 + t
                    ts_ = slice(t * 128, (t + 1) * 128)
                    ps, tps = ps_next(6, 8)
                    for kc in range(8):
                        mm(ps[:, :], hT[:, kc, ts_], Wv[:, kc, :], kc == 0, kc == 7, [t_hT, t_Wv], [tps])
                    vb, t_vb, sl = vst.next()
                    A("scalar", lambda e, vb=vb, ps=ps: e.copy(out=vb[:, :, 0:64], in_=ps[:, :].rearrange("p (h c) -> p h c", h=8)), reads=[tps], writes=[t_vb])
                    P.dma("gpsimd", V_ABD[tile * 128:(tile + 1) * 128], vb, sl, reads=[t_vb], writes=[("vabd", tile)])
                    ps, tps = ps_next(6, 8)
                    for k in range(2):
                        mm(ps[:, 0:256], ckvn[:, k, ts_], Wukv_v[:, k].rearrange("p h c -> p (h c)"), k == 0, k == 1, [t_ckvn, t_Wukv], [tps])
                    vb, t_vb, sl = vcst.next()
                    A("scalar", lambda e, vb=vb, ps=ps: e.copy(out=vb[:, :, 0:64], in_=ps[:, 0:256].rearrange("p (h c) -> p h c", h=4)), reads=[tps], writes=[t_vb])
                    P.dma("gpsimd", V_C[tile * 128:(tile + 1) * 128], vb, sl, reads=[t_vb], writes=[("vc", tile)])
            AR.release(mA); P.next_slot = sl_mA
            P.barrier()
            if stop_after == "A":
                break

            def finish_heads(O, tO, heads_blocks, br, tcol, rc, t_rc, yst, add_sink=None):
                n = sum(w for _, _, w in heads_blocks)
                if add_sink is not None:
                    es, t_es = add_sink
                    A("vector", lambda e: e.tensor_tensor(out=rc[64:128, 0:n].rearrange("p (h q) -> p h q", h=4),
                                                          in0=O[64:128, 0:n].rearrange("p (h q) -> p h q", h=4),
                                                          in1=es[64:128, :].unsqueeze(2).to_broadcast([64, 4, n // 4]), op=ALU.add),
                      reads=[tO, t_es], writes=[t_rc])
                    A("vector", lambda e: e.reciprocal(out=rc[64:128, 0:n], in_=rc[64:128, 0:n]), reads=[t_rc], writes=[t_rc])
                else:
                    A("vector", lambda e: e.reciprocal(out=rc[64:128, 0:n], in_=O[64:128, 0:n]), reads=[tO], writes=[t_rc])
                ys, t_ys, sl = yst.next()
                for (h, c0, w) in heads_blocks:
                    po = (h % 2) * 64
                    if len(heads_blocks) == 1:
                        oap = ys[po:po + 64, 0:w]
                    else:
                        oap = ys[po:po + 64, h // 2, 0:w]
                    A("vector", lambda e, oap=oap, c0=c0, w=w: e.tensor_tensor(out=oap, in0=O[0:64, c0:c0 + w], in1=rc[64:128, c0:c0 + w], op=ALU.mult),
                      reads=[tO, t_rc], writes=[t_ys])
                return ys, t_ys, sl

            for br in (0, 1):
                mB = AR.mark(); sl_mB = P.next_slot
                ngq = 2
                KT, t_KT = AR.alloc([2 if br == 0 else 1, S], BF16)
                QT, t_QT = AR.alloc([2, S], BF16)
                Vt, t_Vt = AR.alloc([NT, 4 if br == 0 else 2, 128], BF16)
                sl0 = P.slot()
                if br == 0:
                    for g in range(2):
                        P.dma("sync", KT[:, g, :], KT_A[g], sl0, reads=[("ka", g, c_) for c_ in range(NCH)], writes=[t_KT])
                        P.dma("sync", QT[:, g, :], QT_A[g], sl0, reads=[("qa", g, c_) for c_ in range(NCH)], writes=[t_QT])
                    for tq in range(4):
                        P.dma("sync", Vt[:, tq * 8:(tq + 1) * 8], V_ABD.rearrange("(t p) h c -> p t h c", p=128)[:, tq * 8:(tq + 1) * 8, 0:4, :], sl0, reads=[("vabd", t_) for t_ in range(NT)], writes=[t_Vt])
                else:
                    P.dma("sync", KT[:, 0, :], KT_B[0], sl0, reads=[("kb", 0, c_) for c_ in range(NCH)], writes=[t_KT])
                    for g in range(2):
                        P.dma("sync", QT[:, g, :], QT_B[g], sl0, reads=[("qb", g, c_) for c_ in range(NCH)], writes=[t_QT])
                    for tq in range(4):
                        P.dma("sync", Vt[:, tq * 8:(tq + 1) * 8], V_ABD.rearrange("(t p) h c -> p t h c", p=128)[:, tq * 8:(tq + 1) * 8, 4:6, :], sl0, reads=[("vabd", t_) for t_ in range(NT)], writes=[t_Vt])
                Sbr = Rot(P, AR, 2, [4, 128], F32)
                Ptr = Rot(P, AR, 3, [4, 128], BF16)
                rc, t_rc = AR.alloc([512], F32)
                yst = Rot(P, AR, 2, [2, 128], BF16)
                if br == 0:
                    TT, t_TT = AR.alloc([60, 64], F32)
                    NEGT, t_NEGT = AR.alloc([4, 64], F32)
                    E2, t_E2 = AR.alloc([64, 128], F32)
                    rbT, t_rbT = AR.alloc([64], F32)
                    A("vector", lambda e: e.memset(NEGT, NEG), writes=[t_NEGT])
                    A("vector", lambda e: e.memset(rbT[0:32, :], 1.0), writes=[t_rbT])
                    P.dma("sync", E2[0:32], I["k_e2"], sl0, writes=[t_E2])
                    P.dma("sync", rbT[0:31, 0:60], I["na_rel_bias"][l].rearrange("h r i -> i (h r)"), sl0, reads=[t_rbT], writes=[t_rbT], allow_slow_non_contiguous=True)
                    for q0 in range(0, 64, 8):
                        ps, tps = ps_next(1, 8)
                        for q in range(8):
                            mm(ps[:, q * 64:q * 64 + 60], E2[0:32, q0 + q, :], rbT[0:32, 0:60], True, True, [t_E2, t_rbT], [tps])
                        A("vector", lambda e, ps=ps, q0=q0: e.tensor_copy(out=TT[:, :, q0:q0 + 8].rearrange("p c q -> p q c"),
                                                                         in_=ps[:, :].rearrange("p (q c) -> p q c", q=8)[:, :, 0:60]),
                          reads=[tps], writes=[t_TT])
                    TTv = TT.rearrange("p (g hf r) q -> p hf g r q", g=2, hf=2)
                else:
                    ALt, t_AL = AR.alloc([3, 4, 128], F32)
                    es, t_es = AR.alloc([4], F32)
                    P.dma("sync", ALt, I["k_al"], sl0, writes=[t_AL])
                    P.dma("sync", es, I["win_sink"][l:l + 1, :].partition_broadcast(128), sl0, writes=[t_es])
                    A("scalar", lambda e: e.activation(out=es, in_=es, func=AF.Exp), reads=[t_es], writes=[t_es])

                def rs(r):
                    return min(max(r - 4, 0), 56)

                stepsAB = []
                for j in range(NT):
                    if br == 0:
                        kts = list(range(rs(2 * j) // 2, (rs(2 * j + 1) + 7) // 2 + 1))
                    else:
                        kts = [k for k in (j - 1, j, j + 1) if 0 <= k < NT]
                    for ki, kt in enumerate(kts):
                        stepsAB.append((j, ki, kt, len(kts)))
                pendAB = {}

                def ab_qk(s_):
                    j, ki, kt, nk = stepsAB[s_]
                    qs = slice(j * 128, (j + 1) * 128)
                    ks = slice(kt * 128, (kt + 1) * 128)
                    banks = (ps_next(0, 6), ps_next(0, 6))
                    for h in range(4):
                        if br == 0:
                            half, slot = h % 2, h // 2
                            kidx = slot
                        else:
                            half, slot = h // 2, h % 2
                            kidx = 0
                        po = half * 64
                        ps_, tps_ = banks[half]
                        mm(ps_[:, slot * 128:(slot + 1) * 128], KT[po:po + 64, kidx, ks], QT[po:po + 64, slot, qs], True, True, [t_KT, t_QT], [tps_])
                    pendAB[s_] = banks

                def ab_rest(s_):
                    j, ki, kt, nk = stepsAB[s_]
                    qs = slice(j * 128, (j + 1) * 128)
                    banks = pendAB.pop(s_)
                    O, tO = PS[6 + (j % 2)], PST[6 + (j % 2)]
                    Sb, t_Sb, _ = Sbr.next()
                    Sb5 = Sb.rearrange("p (hf g) q -> p hf g q", hf=2)
                    for half in range(2):
                        ps_, tps_ = banks[half]
                        pv = ps_[:, 0:256].rearrange("p (g q) -> p g q", g=2)
                        if br == 0:
                            for krl in range(2):
                                for rl in range(2):
                                    r, kr = 2 * j + rl, 2 * kt + krl
                                    pp = slice(krl * 64, (krl + 1) * 64)
                                    if rs(r) <= kr < rs(r) + 8:
                                        dr = kr - r
                                        in1 = TTv[pp, half, :, dr + 7, :]
                                        rd = [t_TT]
                                    else:
                                        in1 = NEGT[pp, 0:2, :]
                                        rd = [t_NEGT]
                                    A("vector", lambda e, pp=pp, rl=rl, in1=in1, pv=pv, half=half, Sb5=Sb5: e.tensor_tensor(out=Sb5[pp, half, :, rl * 64:(rl + 1) * 64], in0=pv[pp, :, rl * 64:(rl + 1) * 64], in1=in1, op=ALU.add),
                                      reads=[tps_] + rd, writes=[t_Sb])
                        else:
                            o = kt - j + 1
                            A("vector", lambda e, pv=pv, o=o, half=half, Sb5=Sb5, ALt=ALt: e.tensor_tensor(out=Sb5[:, half, :, :], in0=pv, in1=ALt[:, o, half * 2:half * 2 + 2, :], op=ALU.add),
                              reads=[tps_, t_AL], writes=[t_Sb])
                    Pt, t_Pt, _ = Ptr.next()
                    A("scalar", lambda e, Pt=Pt, Sb=Sb: e.activation(out=Pt, in_=Sb, func=AF.Exp), reads=[t_Sb], writes=[t_Pt])
                    for h in range(4):
                        vh = h if br == 0 else h // 2
                        hh = (h % 2) * 2 + h // 2 if br == 0 else h
                        mm(O[:, h * 128:(h + 1) * 128], Vt[:, kt, vh, :], Pt[:, hh, :], ki == 0 and h == 0, ki == nk - 1, [t_Vt, t_Pt], [tO],
                           skip_group_check=True)
                    if ki == nk - 1:
                        ys, t_ys, sl = finish_heads(O, tO, [(h, h * 128, 128) for h in range(4)], br, None, rc, t_rc, yst,
                                                    add_sink=(es, t_es) if br == 1 else None)
                        P.dma("gpsimd", YT[br, :, :, qs], ys, sl, reads=[t_ys], writes=[("yt", br, j // 4, j % 4)])

                DPAB = 2
                for s_ in range(len(stepsAB) + DPAB):
                    if s_ < len(stepsAB):
                        ab_qk(s_)
                    if s_ >= DPAB:
                        ab_rest(s_ - DPAB)
                AR.release(mB); P.next_slot = sl_mB
                P.barrier()
                if stop_after == "attn%d" % br:
                    break
            if stop_after in ("attn0", "attn1"):
                break

            for br in (2, 3):
                mC = AR.mark(); sl_mC = P.next_slot
                sl0 = P.slot()
                if br == 2:
                    KT, t_KT = AR.alloc([4, S], BF16)
                    Vt, t_Vt = AR.alloc([NT, 4, 128], BF16)
                    for h in range(4):
                        P.dma("sync", KT[0:96, h, :], KT_C[h], sl0, reads=[("kc", h, c_) for c_ in range(NCH)], writes=[t_KT])
                    for tq in range(4):
                        P.dma("sync", Vt[:, tq * 8:(tq + 1) * 8], V_C.rearrange("(t p) h c -> p t h c", p=128)[:, tq * 8:(tq + 1) * 8], sl0, reads=[("vc", t_) for t_ in range(NT)], writes=[t_Vt])
                    scl = 96.0 ** -0.5
                else:
                    KT, t_KT = AR.alloc([1, S], BF16)
                    Vt, t_Vt = AR.alloc([NT, 2, 128], BF16)
                    P.dma("sync", KT[:, 0, :], KT_D[0], sl0, reads=[("kd", 0, c_) for c_ in range(NCH)], writes=[t_KT])
                    for tq in range(4):
                        P.dma("sync", Vt[:, tq * 8:(tq + 1) * 8], V_ABD.rearrange("(t p) h c -> p t h c", p=128)[:, tq * 8:(tq + 1) * 8, 6:8, :], sl0, reads=[("vabd", t_) for t_ in range(NT)], writes=[t_Vt])
                    scl = 1.0
                Qr = Rot(P, AR, 3, [CH], BF16)
                Ptr = Rot(P, AR, 3, [2 * CH], BF16)
                rc, t_rc = AR.alloc([512], F32)
                yst = Rot(P, AR, 2, [CH], BF16)
                blocksCD = [(h, ch) for h in range(4) for ch in range(NCH)]
                qbuf = {}

                def cd_loadq(bi):
                    h, ch = blocksCD[bi]
                    cs = slice(ch * CH, (ch + 1) * CH)
                    Qc, t_Qc, sl = Qr.next()
                    if br == 2:
                        P.dma("sync", Qc[0:96, :], QT_C[h, :, cs], sl, reads=[("qc", h, ch)], writes=[t_Qc])
                    else:
                        po = (h // 2) * 64
                        P.dma("sync", Qc[po:po + 64, :], QT_D[h % 2, po:po + 64, cs], sl, reads=[("qd", h % 2, ch)], writes=[t_Qc])
                    qbuf[bi] = (Qc, t_Qc)

                NP2 = NT // 2
                npairs = len(blocksCD) * NP2
                pairrot = [0]
                pendCD = {}

                def cd_qk(p_):
                    bi, pp_ = divmod(p_, NP2)
                    h, ch = blocksCD[bi]
                    if pp_ == 0 and bi + 1 < len(blocksCD):
                        cd_loadq(bi + 1)
                    Qc, t_Qc = qbuf[bi]
                    k = 1 + pairrot[0] % 3
                    pairrot[0] += 1
                    for half in range(2):
                        kt = pp_ * 2 + half
                        ks = slice(kt * 128, (kt + 1) * 128)
                        ps, tps = PS[2 * k + half], PST[2 * k + half]
                        if br == 2:
                            mm(ps[:, :], KT[0:96, h, ks], Qc[0:96, :], True, True, [t_KT, t_Qc], [tps])
                        else:
                            po = (h // 2) * 64
                            mm(ps[:, :], KT[po:po + 64, 0, ks], Qc[po:po + 64, :], True, True, [t_KT, t_Qc], [tps])
                    pendCD[p_] = k

                def cd_rest(p_):
                    bi, pp_ = divmod(p_, NP2)
                    h, ch = blocksCD[bi]
                    cs = slice(ch * CH, (ch + 1) * CH)
                    k = pendCD.pop(p_)
                    O, tO = PS[bi % 2], PST[bi % 2]
                    Pt, t_Pt, _ = Ptr.next()
                    pbk = PB[k]
                    A("scalar", lambda e, Pt=Pt, pbk=pbk, scl=scl: e.activation(out=Pt, in_=pbk[:, :], func=AF.Exp, scale=scl),
                      reads=[PST[2 * k], PST[2 * k + 1]], writes=[t_Pt])
                    vh = h if br == 2 else h // 2
                    for half in range(2):
                        kt = pp_ * 2 + half
                        mm(O[:, :], Vt[:, kt, vh, :], Pt[:, half * CH:(half + 1) * CH], kt == 0, kt == NT - 1, [t_Vt, t_Pt], [tO])
                    if pp_ == NP2 - 1:
                        ys, t_ys, sl = finish_heads(O, tO, [(h, 0, CH)], br, None, rc, t_rc, yst)
                        po2 = (h % 2) * 64
                        P.dma("gpsimd", YT[br, po2:po2 + 64, h // 2, cs], ys[po2:po2 + 64, :], sl, reads=[t_ys], writes=[("yt", br, ch, h)])

                DPCD = 2
                cd_loadq(0)
                for p_ in range(npairs + DPCD):
                    if p_ < npairs:
                        cd_qk(p_)
                    if p_ >= DPCD:
                        cd_rest(p_ - DPCD)
                AR.release(mC); P.next_slot = sl_mC
                P.barrier()
                if stop_after == "attn%d" % br:
                    break
            if stop_after in ("attn", "attn2", "attn3"):
                break

            mM = AR.mark(); sl_mM = P.next_slot
            Wg, t_Wg = AR.alloc([8, 4096], BF16)
            Wb, t_Wb = AR.alloc([4, 2, D], BF16)
            Wo, t_Wo = AR.alloc([8, D], BF16)
            sw = P.slot()
            for n in range(8):
                P.dma("gpsimd", Wg[:, :, n * 512:(n + 1) * 512], win_v[:, :, 2272 + n * 512:2272 + (n + 1) * 512], sw, writes=[t_Wg])
            P.dma("gpsimd", Wb, I["w_branch"][l].rearrange("n (k p) d -> p n k d", p=128), sw, writes=[t_Wb])
            P.dma("gpsimd", Wo, I["w_out"][l].rearrange("(k p) n -> p k n", p=128), sw, writes=[t_Wo])
            hTr = Rot(P, AR, 1, [8, CH], BF16)
            xrot = Rot(P, AR, 4, [D], F32)
            nb = {"junk": AR.alloc([D], BF16), "ss": AR.alloc([4], F32), "hf": AR.alloc([D], F32), "hb": Rot(P, AR, 2, [D], BF16)}
            Yr = Rot(P, AR, 1, [4, 2, CH], BF16)
            gtr = Rot(P, AR, 2, [CH], BF16)
            macc, t_macc = AR.alloc([CH], F32)
            mtmp = Rot(P, AR, 2, [CH], F32)
            mT, t_mT = AR.alloc([8, CH], BF16)
            slxo = P.slot()
            for ch in range(NCH):
                cs = slice(ch * CH, (ch + 1) * CH)
                hT, t_hT, _ = hTr.next()
                xts = norm_chunk(xsrc, xtk, ch, G1, SH1, [t_G1, t_mod], hT, t_hT, xrot, nb)
                Y, t_Y, sly = Yr.next()
                for b4 in range(4):
                    P.dma("sync", Y[:, b4], YT[b4, :, :, cs], sly, reads=[("yt", b4, ch, k_) for k_ in range(4)], writes=[t_Y])
                for dc in range(8):
                    for n in range(4):
                        pg, tpg = ps_next(0, 4)
                        for kc in range(8):
                            mm(pg[:, :], Wg[:, kc, n * D + dc * 128:n * D + (dc + 1) * 128], hT[:, kc, :], kc == 0, kc == 7, [t_Wg, t_hT], [tpg])
                        gt, t_gt, _ = gtr.next()
                        A("scalar", lambda e, gt=gt, pg=pg: e.activation(out=gt, in_=pg[:, :], func=AF.Sigmoid), reads=[tpg], writes=[t_gt])
                        pb, tpb = ps_next(0, 4)
                        for k in range(2):
                            mm(pb[:, :], Wb[:, n, k, dc * 128:(dc + 1) * 128], Y[:, n, k, :], k == 0, k == 1, [t_Wb, t_Y], [tpb])
                        if n == 0:
                            A("vector", lambda e, pb=pb, gt=gt: e.tensor_tensor(out=macc, in0=pb[:, :], in1=gt, op=ALU.mult), reads=[tpb, t_gt], writes=[t_macc])
                        else:
                            mt, t_mt, _ = mtmp.next()
                            A("vector", lambda e, pb=pb, gt=gt, mt=mt: e.tensor_tensor(out=mt, in0=pb[:, :], in1=gt, op=ALU.mult), reads=[tpb, t_gt], writes=[t_mt])
                            if n < 3:
                                A("gpsimd", lambda e, mt=mt: e.tensor_tensor(out=macc, in0=macc, in1=mt, op=ALU.add), reads=[t_macc, t_mt], writes=[t_macc])
                            else:
                                A("gpsimd", lambda e, mt=mt, dc=dc: e.tensor_tensor(out=mT[:, dc, :], in0=macc, in1=mt, op=ALU.add), reads=[t_macc, t_mt], writes=[t_mT])
                for t in range(4):
                    tile = ch * 4 + t
                    xt, t_xt, slx = xts[t]
                    xn, t_xn = xt, t_xt
                    for nh in range(2):
                        po_, tpo = ps_next(4, 8)
                        for dc in range(8):
                            mm(po_[:, :], mT[:, dc, t * 128:(t + 1) * 128], Wo[:, dc, nh * 512:(nh + 1) * 512], dc == 0, dc == 7, [t_mT, t_Wo], [tpo])
                        mt, t_mt, _ = mtmp.next()
                        A("vector", lambda e, po_=po_, mt=mt, nh=nh: e.tensor_tensor(out=mt, in0=po_[:, :], in1=GT1[:, nh * 512:(nh + 1) * 512], op=ALU.mult),
                          reads=[tpo, t_mod], writes=[t_mt])
                        A("gpsimd", lambda e, xn=xn, xt=xt, mt=mt, nh=nh: e.tensor_tensor(out=xn[:, nh * 512:(nh + 1) * 512], in0=xt[:, nh * 512:(nh + 1) * 512], in1=mt, op=ALU.add),
                          reads=[t_xt, t_mt], writes=[t_xn])
                    P.dma("gpsimd", XR[tile * 128:(tile + 1) * 128, :], xn, slx, reads=[t_xn, ("xr", tile)], writes=[("xr", tile), ("xm", tile)])
            AR.release(mM); P.next_slot = sl_mM
            P.barrier()
            if stop_after == "merge":
                break

            mF = AR.mark(); sl_mF = P.next_slot
            SC = 1024
            h2, t_h2 = AR.alloc([8, SC], BF16)
            acc, t_acc = AR.alloc([8, D], F32)
            comb, t_comb = AR.alloc([8, 32], F32)
            Wr, t_Wr = AR.alloc([8, 36], BF16)
            brt, t_brt = AR.alloc([36], F32)
            sw = P.slot()
            P.dma("gpsimd", Wr[:, :, 0:4], I["w_group"][l].rearrange("(k p) n -> p k n", p=128), sw, writes=[t_Wr])
            P.dma("gpsimd", Wr[:, :, 4:36], I["w_router"][l].rearrange("(k p) n -> p k n", p=128), sw, writes=[t_Wr])
            swb = P.slot()
            P.dma("sync", brt[:, 0:4], I["b_group"][l:l + 1, :].partition_broadcast(128), swb, writes=[t_brt])
            P.dma("sync", brt[:, 4:36], I["b_router"][l:l + 1, :].partition_broadcast(128), swb, writes=[t_brt])
            hTr = Rot(P, AR, 1, [8, CH], BF16)
            xrot = Rot(P, AR, 2, [D], F32)
            nb = {"junk": AR.alloc([D], BF16), "ss": AR.alloc([4], F32), "hf": AR.alloc([D], F32), "hb": Rot(P, AR, 2, [D], BF16)}
            W1r = Rot(P, AR, 2, [8, 256], BF16)
            W3r = Rot(P, AR, 2, [8, 256], BF16)
            W2r = Rot(P, AR, 2, [2, D], BF16)
            slr = Rot(P, AR, 2, [CH], F32)
            hidr = Rot(P, AR, 3, [2, CH], BF16)
            rt, t_rt = AR.alloc([160], F32)
            xo = Rot(P, AR, 2, [D], F32)
            gfin, t_gfin = AR.alloc([D], F32)
            if l == depth - 1:
                P.dma("sync", gfin, I["final_norm_g"].rearrange("(o d) -> o d", o=1).partition_broadcast(128), swb, writes=[t_gfin])
            for sc in range(4):
                for c4 in range(2):
                    ch = sc * 2 + c4
                    hT, t_hT, _ = hTr.next()
                    norm_chunk(XR, "xm", ch, G2, SH2, [t_G2, t_mod], hT, t_hT, xrot, nb)
                    A("gpsimd", lambda e, c4=c4, hT=hT: e.tensor_copy(out=h2[:, :, c4 * CH:(c4 + 1) * CH], in_=hT), reads=[t_hT], writes=[t_h2])
                    for t in range(4):
                        lt = c4 * 4 + t
                        ps, tps = ps_next(0, 4)
                        for kc in range(8):
                            mm(ps[:, 0:36], hT[:, kc, t * 128:(t + 1) * 128], Wr[:, kc, :], kc == 0, kc == 7, [t_hT, t_Wr], [tps])
                        lg = rt[:, 0:36]
                        V_ = "vector"
                        R = [t_rt]
                        A(V_, lambda e, ps=ps: e.tensor_tensor(out=lg, in0=ps[:, 0:36], in1=brt, op=ALU.add), reads=[tps, t_brt], writes=R)
                        gmax, ngm, gsum, gw = rt[:, 36:37], rt[:, 37:38], rt[:, 38:39], rt[:, 39:40]
                        goh, pen, ge = rt[:, 40:44], rt[:, 44:48], rt[:, 48:52]
                        el2 = rt[:, 52:84]
                        oh1, el3, oh2 = rt[:, 84:116], rt[:, 116:148], rt[:, 52:84]
                        m1, m2, dd, ee, w1, w2 = (rt[:, 148 + i:149 + i] for i in range(6))
                        A(V_, lambda e: e.reduce_max(out=gmax, in_=lg[:, 0:4], axis=AX.X), reads=R, writes=R)
                        A(V_, lambda e: e.tensor_scalar(out=ngm, in0=gmax, scalar1=-1.0, scalar2=None, op0=ALU.mult), reads=R, writes=R)
                        A(V_, lambda e: e.tensor_scalar(out=goh, in0=lg[:, 0:4], scalar1=gmax, scalar2=None, op0=ALU.is_ge), reads=R, writes=R)
                        A("scalar", lambda e: e.activation(out=ge, in_=lg[:, 0:4], func=AF.Exp, bias=ngm, scale=1.0, accum_out=gsum), reads=R, writes=R)
                        A(V_, lambda e: e.reciprocal(out=gw, in_=gsum), reads=R, writes=R)
                        A(V_, lambda e: e.tensor_scalar(out=pen, in0=goh, scalar1=-1.0, scalar2=1.0e9, op0=ALU.add, op1=ALU.mult), reads=R, writes=R)
                        A(V_, lambda e: e.tensor_tensor(out=el2.rearrange("p (g x) -> p g x", g=4), in0=lg[:, 4:36].rearrange("p (g x) -> p g x", g=4),
                                                        in1=pen.unsqueeze(2).to_broadcast([128, 4, 8]), op=ALU.add), reads=R, writes=R)
                        A(V_, lambda e: e.reduce_max(out=m1, in_=el2, axis=AX.X), reads=R, writes=R)
                        A(V_, lambda e: e.tensor_scalar(out=oh1, in0=el2, scalar1=m1, scalar2=None, op0=ALU.is_ge), reads=R, writes=R)
                        A(V_, lambda e: e.scalar_tensor_tensor(out=el3, in0=oh1, scalar=-1.0e9, in1=el2, op0=ALU.mult, op1=ALU.add), reads=R, writes=R)
                        A(V_, lambda e: e.reduce_max(out=m2, in_=el3, axis=AX.X), reads=R, writes=R)
                        A(V_, lambda e: e.tensor_scalar(out=oh2, in0=el3, scalar1=m2, scalar2=None, op0=ALU.is_ge), reads=R, writes=R)
                        A(V_, lambda e: e.tensor_tensor(out=dd, in0=m2, in1=m1, op=ALU.subtract), reads=R, writes=R)
                        A("scalar", lambda e: e.activation(out=ee, in_=dd, func=AF.Exp), reads=R, writes=R)
                        A(V_, lambda e: e.tensor_scalar(out=w1, in0=ee, scalar1=1.0, scalar2=None, op0=ALU.add), reads=R, writes=R)
                        A(V_, lambda e: e.reciprocal(out=w1, in_=w1), reads=R, writes=R)
                        A(V_, lambda e: e.tensor_tensor(out=w2, in0=ee, in1=w1, op=ALU.mult), reads=R, writes=R)
                        A(V_, lambda e: e.tensor_tensor(out=w1, in0=w1, in1=gw, op=ALU.mult), reads=R, writes=R)
                        A(V_, lambda e: e.tensor_tensor(out=w2, in0=w2, in1=gw, op=ALU.mult), reads=R, writes=R)
                        A(V_, lambda e, lt=lt: e.tensor_scalar(out=comb[:, lt, :], in0=oh1, scalar1=w1, scalar2=None, op0=ALU.mult), reads=R, writes=[t_comb])
                        A(V_, lambda e, lt=lt: e.scalar_tensor_tensor(out=comb[:, lt, :], in0=oh2, scalar=w2, in1=comb[:, lt, :], op0=ALU.mult, op1=ALU.add), reads=R + [t_comb], writes=[t_comb])
                stepsM = [(ex, c4) for ex in range(32) for c4 in range(2)]
                wbuf = {}
                hbuf = {}

                def moe_s1(i_):
                    ex, c4 = stepsM[i_]
                    if c4 == 0:
                        W1, t_W1, s1_ = W1r.next()
                        W3, t_W3, s3_ = W3r.next()
                        W2, t_W2, s2_ = W2r.next()
                        P.dma("gpsimd", W1, I["w_exp1"][l, ex].rearrange("(k p) n -> p k n", p=128), s1_, writes=[t_W1])
                        P.dma("gpsimd", W3, I["w_exp3"][l, ex].rearrange("(k p) n -> p k n", p=128), s3_, writes=[t_W3])
                        P.dma("gpsimd", W2, I["w_exp2"][l, ex].rearrange("(k p) n -> p k n", p=128), s2_, writes=[t_W2])
                        wbuf[ex] = (W1, t_W1, W3, t_W3, W2, t_W2)
                    W1, t_W1, W3, t_W3, W2, t_W2 = wbuf[ex]
                    hid, t_hid, _ = hidr.next()
                    for fh in range(2):
                        p1, tp1 = ps_next(0, 4)
                        for kc in range(8):
                            mm(p1[:, :], W1[:, kc, fh * 128:(fh + 1) * 128], h2[:, kc, c4 * CH:(c4 + 1) * CH], kc == 0, kc == 7, [t_W1, t_h2], [tp1])
                        p3, tp3 = ps_next(0, 4)
                        for kc in range(8):
                            mm(p3[:, :], W3[:, kc, fh * 128:(fh + 1) * 128], h2[:, kc, c4 * CH:(c4 + 1) * CH], kc == 0, kc == 7, [t_W3, t_h2], [tp3])
                        sl_, t_sl, _ = slr.next()
                        A("scalar", lambda e, sl_=sl_, p1=p1: e.activation(out=sl_, in_=p1[:, :], func=AF.Silu), reads=[tp1], writes=[t_sl])
                        A("vector", lambda e, hid=hid, fh=fh, p3=p3, sl_=sl_: e.tensor_tensor(out=hid[:, fh, :], in0=p3[:, :], in1=sl_, op=ALU.mult), reads=[tp3, t_sl], writes=[t_hid])
                    hbuf[i_] = (hid, t_hid)

                def moe_s2(i_):
                    ex, c4 = stepsM[i_]
                    W1, t_W1, W3, t_W3, W2, t_W2 = wbuf[ex]
                    hid, t_hid = hbuf.pop(i_)
                    for t in range(4):
                        lt = c4 * 4 + t
                        for nh in range(2):
                            po_, tpo = ps_next(4, 8)
                            for fh in range(2):
                                mm(po_[:, :], hid[:, fh, t * 128:(t + 1) * 128], W2[:, fh, nh * 512:(nh + 1) * 512], fh == 0, fh == 1, [t_hid, t_W2], [tpo])
                            asl = acc[:, lt, nh * 512:(nh + 1) * 512]
                            if ex == 0:
                                A("vector", lambda e, po_=po_, asl=asl, lt=lt: e.tensor_scalar(out=asl, in0=po_[:, :], scalar1=comb[:, lt, 0:1], scalar2=None, op0=ALU.mult),
                                  reads=[tpo, t_comb], writes=[(t_acc, lt)])
                            else:
                                A("vector", lambda e, po_=po_, asl=asl, lt=lt, ex=ex: e.scalar_tensor_tensor(out=asl, in0=po_[:, :], scalar=comb[:, lt, ex:ex + 1], in1=asl, op0=ALU.mult, op1=ALU.add),
                                  reads=[tpo, t_comb, (t_acc, lt)], writes=[(t_acc, lt)])

                for i_ in range(len(stepsM) + 1):
                    if i_ < len(stepsM):
                        moe_s1(i_)
                    if i_ >= 1:
                        moe_s2(i_ - 1)
                for lt in range(8):
                    tile = sc * 8 + lt
                    xt, t_xt, slx = xrot.next()
                    P.dma("sync", xt, XR[tile * 128:(tile + 1) * 128, :], slx, reads=[("xm", tile)], writes=[t_xt])
                    xn, t_xn, slo = xo.next()
                    A("gpsimd", lambda e, lt=lt, xn=xn: e.tensor_tensor(out=xn, in0=acc[:, lt, :], in1=GT2, op=ALU.mult), reads=[(t_acc, lt), t_mod], writes=[t_xn])
                    A("gpsimd", lambda e, xn=xn, xt=xt: e.tensor_tensor(out=xn, in0=xn, in1=xt, op=ALU.add), reads=[t_xn, t_xt], writes=[t_xn])
                    if l < depth - 1:
                        P.dma("sync", XR[tile * 128:(tile + 1) * 128, :], xn, slo, reads=[t_xn, ("xm", tile)], writes=[("xr", tile)])
                    else:
                        junk, t_junk = nb["junk"]
                        ssq, t_ssq = nb["ss"]
                        A("scalar", lambda e, xn=xn: e.activation(out=junk, in_=xn, func=AF.Square, accum_out=ssq[:, 0:1]), reads=[t_xn], writes=[t_junk, t_ssq])
                        rstd_from(ssq[:, 0:1], ssq[:, 1:2], float(D), [t_ssq], [t_ssq], ssq[:, 2:3])
                        A("vector", lambda e, xn=xn: e.scalar_tensor_tensor(out=xn, in0=xn, scalar=ssq[:, 1:2], in1=gfin, op0=ALU.mult, op1=ALU.mult),
                          reads=[t_xn, t_ssq, t_gfin], writes=[t_xn])
                        out_dmas.append(P.dma("sync", y_out[tile * 128:(tile + 1) * 128, :], xn, slo, reads=[t_xn], writes=[("y", tile)]))
            AR.release(mF); P.next_slot = sl_mF
            P.barrier()

        if not out_dmas:
            d0, t_d0 = AR.alloc([8], F32)
            A("vector", lambda e: e.memset(d0, 0.0), writes=[t_d0])
            out_dmas.append(P.dma("sync", y_out[0:128, 0:8], d0, P.slot(), reads=[t_d0]))
        finals = [o for o in P.dma_last if o is not None]
        P.emit(final_waits=finals)
    return nc


_CACHE = {}


def kernel(**inputs):
    if "nc" not in _CACHE:
        _CACHE["nc"] = build_program()
        _CACHE["consts"] = host_consts()
    nc = _CACHE["nc"]
    consts = _CACHE["consts"]
    x = np.ascontiguousarray(np.asarray(inputs["x"], dtype=np.float32))
    c = np.ascontiguousarray(np.asarray(inputs["c"], dtype=np.float32))
    shared = {name: np.ascontiguousarray(np.asarray(inputs[name], dtype=np.float32)) for name, _ in WEIGHT_SPECS}
    shared.update(consts)
    in_maps = []
    for b in range(8):
        m = dict(shared)
        m["x"] = x[b]
        m["c"] = c[b]
        in_maps.append(m)
    res = run_bass_kernel_spmd(nc, in_maps, core_ids=list(range(8)))
    out = np.stack([np.asarray(res.results[b]["y"], dtype=np.float32) for b in range(8)], axis=0)
    return out
```

```python
import math
from contextlib import ExitStack
import numpy as np
import concourse.bass as bass
import concourse.mybir as mybir
from concourse.bass_utils import run_bass_kernel_spmd

F32 = mybir.dt.float32
BF16 = mybir.dt.bfloat16
AF = mybir.ActivationFunctionType
ALU = mybir.AluOpType
AX = mybir.AxisListType
ENGS = ("sync", "scalar", "vector", "gpsimd", "tensor")

S = 4096
D = 1024
NT = 32
NCH = 8
CH = 512
DEPTH = 4
D_IN = 6368
EPS = 1e-6
NEG = -30000.0


class Op:
    __slots__ = ("eng", "fn", "deps", "signal", "val", "dsem", "dval")

    def __init__(self, eng, fn):
        self.eng = eng
        self.fn = fn
        self.deps = []
        self.signal = False
        self.val = None
        self.dsem = None
        self.dval = None


class Prog:
    def __init__(self, nc, n_dma_sems=56):
        self.nc = nc
        self.ops = {e: [] for e in ENGS}
        self.last_writer = {}
        self.readers = {}
        self.n_dma_sems = n_dma_sems
        self.dma_counts = {}
        self.dma_last = {}
        self.next_slot = 0
        self.pending = {e: [] for e in ENGS}

    def slot(self):
        s = self.next_slot
        self.next_slot += 1
        self.max_slot = max(getattr(self, "max_slot", 0), self.next_slot)
        assert s < self.n_dma_sems, "out of dma sems"
        return s

    def _dep(self, op, d):
        if d is None or d is op:
            return
        if d.dsem is None and d.eng == op.eng and op.eng == "tensor":
            return
        for x in op.deps:
            if x is d:
                return
        op.deps.append(d)
        if d.dsem is None:
            d.signal = True

    def _track(self, op, reads, writes):
        for b in reads:
            w = self.last_writer.get(b)
            if w is not None:
                self._dep(op, w)
        for b in writes:
            w = self.last_writer.get(b)
            if w is not None:
                self._dep(op, w)
            rd = self.readers.get(b)
            if rd:
                for r in rd.values():
                    self._dep(op, r)
        for b in reads:
            key = op.eng if op.dsem is None else ("d", op.dsem)
            self.readers.setdefault(b, {})[key] = op
        for b in writes:
            self.last_writer[b] = op
            self.readers[b] = {}
        pend = self.pending[op.eng]
        if pend:
            for d in pend:
                self._dep(op, d)
            self.pending[op.eng] = []

    def add(self, eng, fn, reads=(), writes=()):
        op = Op(eng, fn)
        self._track(op, reads, writes)
        self.ops[eng].append(op)
        return op

    def dma(self, eng, out, in_, sem, reads=(), writes=(), **kw):
        def fn(e, out=out, in_=in_, kw=kw):
            return e.dma_start(out=out, in_=in_, **kw)
        op = Op(eng, fn)
        sem = (sem, eng == "gpsimd")
        op.dsem = sem
        self.dma_counts[sem] = self.dma_counts.get(sem, 0) + 16
        op.dval = self.dma_counts[sem]
        self.dma_last[sem] = op
        self._track(op, reads, writes)
        self.ops[eng].append(op)
        return op

    def barrier(self):
        lasts = []
        for e in ENGS:
            for op in reversed(self.ops[e]):
                if op.dsem is None:
                    lasts.append(op)
                    break
        for o in self.dma_last.values():
            lasts.append(o)
        for e in ENGS:
            self.pending[e] = list(lasts)

    def emit(self, final_waits=()):
        nc = self.nc
        for e in ENGS:
            c = 0
            for op in self.ops[e]:
                if op.dsem is None and op.signal:
                    c += 1
                    op.val = c
        with ExitStack() as st:
            esem = {e: st.enter_context(nc.semaphore("s_" + e)) for e in ENGS}
            dsem = {key: st.enter_context(nc.semaphore("d%s_%d" % ("g" if key[1] else "s", key[0]))) for key in sorted(self.dma_counts)}
            block = st.enter_context(nc.Block())

            def run(e_name):
                def body(eng):
                    waited = {}
                    for op in self.ops[e_name]:
                        for d in op.deps:
                            if d.dsem is not None:
                                key, v, s = ("d", d.dsem), d.dval, dsem[d.dsem]
                            else:
                                key, v, s = ("e", d.eng), d.val, esem[d.eng]
                            if waited.get(key, 0) >= v:
                                continue
                            waited[key] = v
                            eng.wait_ge(s, v)
                        inst = op.fn(eng)
                        if op.dsem is not None:
                            inst.then_inc(dsem[op.dsem], 16)
                        elif op.signal:
                            inst.then_inc(esem[e_name], 1)
                    if e_name == "sync":
                        for d in final_waits:
                            eng.wait_ge(dsem[d.dsem], d.dval)
                return body

            block.sync(run("sync"))
            block.scalar(run("scalar"))
            block.vector(run("vector"))
            block.gpsimd(run("gpsimd"))
            block.tensor(run("tensor"))


class Arena:
    def __init__(self, base, nwords):
        self.base = base
        self.n = nwords
        self.off = 0
        self.cnt = 0

    def mark(self):
        return self.off

    def release(self, m):
        self.off = m

    def alloc(self, free, dt=F32, parts=128):
        n = 1
        for f in free:
            n *= f
        words = n if dt == F32 else (n + 1) // 2
        words = (words + 7) // 8 * 8
        assert self.off + words <= self.n, "arena overflow %d + %d > %d" % (self.off, words, self.n)
        ap = self.base[:, self.off:self.off + (n if dt == F32 else (n + 1) // 2)]
        if dt != F32:
            ap = ap.bitcast(dt)
        if len(free) == 2:
            ap = ap.rearrange("p (a b) -> p a b", a=free[0])
        elif len(free) == 3:
            ap = ap.rearrange("p (a b c) -> p a b c", a=free[0], b=free[1])
        elif len(free) == 4:
            ap = ap.rearrange("p (a b c d) -> p a b c d", a=free[0], b=free[1], c=free[2])
        self.off += words
        self.cnt += 1
        return ap, ("sb", self.cnt)


class Rot:
    def __init__(self, P, arena, n, free, dt=F32):
        self.bufs = []
        for _ in range(n):
            ap, tok = arena.alloc(free, dt)
            self.bufs.append((ap, tok, P.slot()))
        self.i = 0

    def next(self):
        b = self.bufs[self.i % len(self.bufs)]
        self.i += 1
        return b


def _rope_np(pos, dim):
    inv = (np.float32(10000.0) ** (-(np.arange(0, dim, 2, dtype=np.float32)) / np.float32(dim))).astype(np.float32)
    ang = pos.astype(np.float32)[:, None] * inv[None, :]
    return np.cos(ang).astype(np.float32), np.sin(ang).astype(np.float32)


def host_consts():
    pos = np.arange(S)
    ct, st_ = _rope_np(pos, 32)
    cr, sr = _rope_np(pos // 64, 32)
    cc, sc = _rope_np(pos % 64, 32)
    ropeC = np.zeros((2, 96, S), np.float32)
    ropeC[0, 0:64] = 1.0
    ropeC[0, 64:80] = ct.T
    ropeC[0, 80:96] = ct.T
    ropeC[1, 64:80] = st_.T
    ropeC[1, 80:96] = st_.T
    cosD = np.concatenate([cr.T, cr.T, cc.T, cc.T], 0)
    sinD = np.concatenate([sr.T, sr.T, sc.T, sc.T], 0)
    ropeD = np.stack([np.concatenate([cosD, cosD], 0), np.concatenate([sinD, sinD], 0)], 0).astype(np.float32)
    al = np.zeros((128, 3, 4, 128), np.float32)
    slopes = 2.0 ** (-8.0 * (np.arange(4, dtype=np.float32) + 1.0) / 4.0)
    i = np.arange(128)[None, :]
    m = np.arange(128)[:, None]
    for o in range(3):
        rel = (o - 1) * 128 + m - i
        for h in range(4):
            al[:, o, h, :] = np.where(np.abs(rel) <= 128, -slopes[h] * np.abs(rel).astype(np.float32), NEG)
    e2 = np.zeros((32, 64, 128), np.float32)
    for q in range(64):
        cs = min(max(q - 8, 0), 48)
        for k in range(64):
            valid = (k >= cs) and (k < cs + 16)
            if valid:
                idx = min(max(k - q, -15), 15) + 15
                e2[idx, q, k] = 1.0
                e2[idx, q, 64 + k] = 1.0
            else:
                e2[31, q, k] = NEG
                e2[31, q, 64 + k] = NEG
    return {
        "k_ident": np.eye(128, dtype=np.float32),
        "k_ropeC": ropeC,
        "k_ropeD": ropeD,
        "k_al": al,
        "k_e2": e2,
    }


WEIGHT_SPECS = [
    ("w_ada", [DEPTH, D, 6 * D]), ("b_ada", [DEPTH, 6 * D]), ("norm1_g", [DEPTH, D]), ("norm2_g", [DEPTH, D]),
    ("w_in", [DEPTH, D, D_IN]), ("na_rel_bias", [DEPTH, 4, 15, 31]), ("win_sink", [DEPTH, 4]),
    ("mla_q_norm_g", [DEPTH, 192]), ("mla_kv_norm_g", [DEPTH, 256]), ("w_uq", [DEPTH, 192, 384]),
    ("w_ukv", [DEPTH, 256, 512]), ("ax_q_norm_g", [DEPTH, 64]), ("ax_k_norm_g", [DEPTH, 64]),
    ("w_branch", [DEPTH, 4, 256, D]), ("w_out", [DEPTH, D, D]), ("w_group", [DEPTH, D, 4]), ("b_group", [DEPTH, 4]),
    ("w_router", [DEPTH, D, 32]), ("b_router", [DEPTH, 32]), ("w_exp1", [DEPTH, 32, D, 256]),
    ("w_exp3", [DEPTH, 32, D, 256]), ("w_exp2", [DEPTH, 32, 256, D]), ("final_norm_g", [D]),
]
CONST_SPECS = [("k_ident", [128, 128]), ("k_ropeC", [2, 96, S]), ("k_ropeD", [2, 128, S]),
               ("k_al", [128, 3, 4, 128]), ("k_e2", [32, 64, 128])]


def build_program(depth=DEPTH, debug=False, stop_after=None):
    nc = bass.Bass("TRN2", target_bir_lowering=False)
    I = {}
    I["x"] = nc.dram_tensor("x", [S, D], F32, kind="ExternalInput").ap()
    I["c"] = nc.dram_tensor("c", [D], F32, kind="ExternalInput").ap()
    for name, shp in WEIGHT_SPECS + CONST_SPECS:
        if len(shp) > 1 and shp[0] == DEPTH and name not in ("k_e2",):
            shp = [depth] + list(shp[1:])
        I[name] = nc.dram_tensor(name, shp, F32, kind="ExternalInput").ap()
    y_out = nc.dram_tensor("y", [S, D], F32, kind="ExternalOutput").ap()
    skind = "ExternalOutput" if debug else "Internal"

    def scratch(name, shp, dt):
        return nc.dram_tensor(name, shp, dt, kind=skind).ap()

    XR = scratch("xres", [S, D], F32)
    QT_A = scratch("qt_a", [2, 128, S], BF16)
    KT_A = scratch("kt_a", [2, 128, S], BF16)
    QT_B = scratch("qt_b", [2, 128, S], BF16)
    KT_B = scratch("kt_b", [1, 128, S], BF16)
    QT_D = scratch("qt_d", [2, 128, S], BF16)
    KT_D = scratch("kt_d", [1, 128, S], BF16)
    QT_C = scratch("qt_c", [4, 96, S], BF16)
    KT_C = scratch("kt_c", [4, 96, S], BF16)
    V_ABD = scratch("v_abd", [S, 8, 128], BF16)
    V_C = scratch("v_c", [S, 4, 128], BF16)
    YT = scratch("yt", [4, 128, 2, S], BF16)

    P = Prog(nc)
    st = ExitStack()
    with st:
        arena_t = st.enter_context(nc.sbuf_tensor("arena", [128, 52000], F32))
        AR = Arena(arena_t, 52000)
        PS = [st.enter_context(nc.psum_tensor("ps%d" % i, [128, 512], F32)) for i in range(2)]
        PB = [None] + [st.enter_context(nc.psum_tensor("pb%d" % k, [128, 1024], F32)) for k in range(1, 4)]
        for k in range(1, 4):
            PS.append(PB[k][:, 0:512])
            PS.append(PB[k][:, 512:1024])
        PST = [("ps", i) for i in range(8)]

        def A(eng, fn, reads=(), writes=()):
            return P.add(eng, fn, reads, writes)

        def mm(out, lhsT, rhs, start, stop, reads, writes, **kw):
            return P.add("tensor", lambda e: e.matmul(out, lhsT=lhsT, rhs=rhs, start=start, stop=stop, **kw), reads, writes)

        identb, t_identb = AR.alloc([128], BF16)
        ones_f, t_ones_f = AR.alloc([128], F32)
        bd_f, t_bd_f = AR.alloc([128], F32)
        eps_c, t_eps = AR.alloc([1], F32)
        cact, t_cact = AR.alloc([8], F32)
        crep, t_crep = AR.alloc([8, 128], F32)
        mod_sb, t_mod = AR.alloc([6 * D], F32)
        G1, t_G1 = AR.alloc([D], F32)
        G2, t_G2 = AR.alloc([D], F32)
        s_c = P.slot()
        P.dma("gpsimd", identb, I["k_ident"], s_c, writes=[t_identb])
        A("vector", lambda e: e.memset(ones_f, 1.0), writes=[t_ones_f])
        A("vector", lambda e: e.memset(bd_f, 0.0), writes=[t_bd_f])
        A("vector", lambda e: e.memset(bd_f[0:64, 0:64], 1.0), writes=[t_bd_f])
        A("vector", lambda e: e.memset(bd_f[64:128, 64:128], 1.0), writes=[t_bd_f])
        A("vector", lambda e: e.memset(eps_c, EPS), writes=[t_eps])
        s_c2 = P.slot()
        P.dma("sync", cact, I["c"].rearrange("(k p) -> p k", p=128), s_c2, writes=[t_cact], allow_slow_non_contiguous=True)
        A("scalar", lambda e: e.activation(out=cact, in_=cact, func=AF.Silu), reads=[t_cact], writes=[t_cact])
        for kc in range(8):
            A("vector", lambda e, kc=kc: e.tensor_scalar(out=crep[:, kc, :], in0=ones_f, scalar1=cact[:, kc:kc + 1], scalar2=None, op0=ALU.mult),
              reads=[t_cact, t_ones_f], writes=[t_crep])
        base_mark = AR.mark()
        base_slot = P.next_slot

        out_dmas = []

        def rstd_from(ss_ap, out_ap, n, reads, writes, tmp):
            p = ss_ap.shape[0]
            A("scalar", lambda e: e.activation(out=tmp, in_=ss_ap, func=AF.Sqrt, bias=eps_c[0:p, 0:1], scale=1.0 / n), reads=list(reads) + [t_eps], writes=writes)
            A("vector", lambda e: e.reciprocal(out=out_ap, in_=tmp), reads=writes, writes=writes)

        def norm_chunk(xsrc, xtoks, ch, G, SH, gs_tok, hT, t_hT, xrot, nb):
            xts = []
            for t in range(4):
                tile = ch * 4 + t
                xt, t_xt, sl = xrot.next()
                P.dma("sync", xt, xsrc[tile * 128:(tile + 1) * 128, :], sl, reads=[(xtoks, tile)], writes=[t_xt])
                junk, t_junk = nb["junk"]
                ssq, t_ssq = nb["ss"]
                A("scalar", lambda e, xt=xt: e.activation(out=junk, in_=xt, func=AF.Square, accum_out=ssq[:, 0:1]),
                  reads=[t_xt], writes=[t_junk, t_ssq])
                rstd_from(ssq[:, 0:1], ssq[:, 1:2], float(D), [t_ssq], [t_ssq], ssq[:, 2:3])
                hf, t_hf = nb["hf"]
                A("vector", lambda e, xt=xt: e.scalar_tensor_tensor(out=hf, in0=xt, scalar=ssq[:, 1:2], in1=G, op0=ALU.mult, op1=ALU.mult),
                  reads=[t_xt, t_ssq] + gs_tok, writes=[t_hf])
                hb, t_hb = nb["hb"].next()[:2]
                A("gpsimd", lambda e, hb=hb: e.tensor_tensor(out=hb, in0=hf, in1=SH, op=ALU.add), reads=[t_hf] + gs_tok, writes=[t_hb])
                pT = PS[0][:, :].bitcast(BF16)
                for kc in range(8):
                    A("tensor", lambda e, kc=kc, hb=hb: e.transpose(out=pT[:, kc * 128:(kc + 1) * 128], in_=hb[:, kc * 128:(kc + 1) * 128], identity=identb),
                      reads=[t_hb, t_identb], writes=[PST[0]])
                A("scalar", lambda e, t=t: e.copy(out=hT[:, :, t * 128:(t + 1) * 128], in_=pT.rearrange("p (k t) -> p k t", k=8)),
                  reads=[PST[0]], writes=[t_hT])
                xts.append((xt, t_xt, sl))
            return xts

        psrot = [0]

        def ps_next(lo=1, hi=8):
            i = lo + psrot[0] % (hi - lo)
            psrot[0] += 1
            return PS[i], PST[i]

        for l in range(depth):
            xsrc = I["x"] if l == 0 else XR
            xtk = ("xin" if l == 0 else "xr")
            AR.release(base_mark)
            P.next_slot = base_slot
            P.barrier()
            m0 = AR.mark(); sl_m0 = P.next_slot
            wst = Rot(P, AR, 2, [3072], F32)
            brow, t_brow = AR.alloc([6 * D], F32)
            n1g, t_n1g = AR.alloc([D], F32)
            n2g, t_n2g = AR.alloc([D], F32)
            sb_ = P.slot()
            P.dma("sync", brow[0:1, :], I["b_ada"][l:l + 1, :], sb_, writes=[t_brow])
            sn1 = P.slot()
            P.dma("sync", n1g, I["norm1_g"][l:l + 1, :].partition_broadcast(128), sn1, writes=[t_n1g])
            sn2 = P.slot()
            P.dma("sync", n2g, I["norm2_g"][l:l + 1, :].partition_broadcast(128), sn2, writes=[t_n2g])
            for half in range(2):
                for kc in range(8):
                    w, t_w, sl = wst.next()
                    P.dma("sync", w, I["w_ada"][l, kc * 128:(kc + 1) * 128, half * 3072:(half + 1) * 3072], sl, writes=[t_w])
                    for n in range(6):
                        mm(PS[n][:, :], crep[:, kc, :], w[:, n * 512:(n + 1) * 512], kc == 0, False, [t_crep, t_w], [PST[n]])
                for n in range(6):
                    col = half * 3072 + n * 512
                    mm(PS[n][:, :], ones_f[0:1, :], brow[0:1, col:col + 512], False, True, [t_ones_f, t_brow], [PST[n]])
                    A("scalar", lambda e, n=n, col=col: e.copy(out=mod_sb[:, col:col + 512], in_=PS[n][:, :]), reads=[PST[n]], writes=[t_mod])
            A("vector", lambda e: e.scalar_tensor_tensor(out=G1, in0=mod_sb[:, D:2 * D], scalar=1.0, in1=n1g, op0=ALU.add, op1=ALU.mult),
              reads=[t_mod, t_n1g], writes=[t_G1])
            A("vector", lambda e: e.scalar_tensor_tensor(out=G2, in0=mod_sb[:, 4 * D:5 * D], scalar=1.0, in1=n2g, op0=ALU.add, op1=ALU.mult),
              reads=[t_mod, t_n2g], writes=[t_G2])
            SH1 = mod_sb[:, 0:D]
            GT1 = mod_sb[:, 2 * D:3 * D]
            SH2 = mod_sb[:, 3 * D:4 * D]
            GT2 = mod_sb[:, 5 * D:6 * D]
            AR.release(m0); P.next_slot = sl_m0
            P.barrier()

            mA = AR.mark(); sl_mA = P.next_slot
            WC = 2304
            Wfm, t_Wfm = AR.alloc([8, WC], BF16)
            Wv, t_Wv = AR.alloc([8, 512], BF16)
            win_v = I["w_in"][l].rearrange("(k p) n -> p k n", p=128)
            sw = P.slot()

            def wload(dst0, src0, w):
                P.dma("gpsimd", Wfm[:, :, dst0:dst0 + w], win_v[:, :, src0:src0 + w], sw, writes=[t_Wfm])

            O_QA, O_KA, O_QB, O_KB, O_QD, O_KD, O_CQ, O_CKV, O_KR = 0, 256, 512, 768, 896, 1408, 1664, 1856, 2112
            wload(O_QA, 0, 256)
            wload(O_KA, 256, 256)
            for g in range(2):
                for j, h in enumerate((g, g + 2)):
                    wload(O_QB + g * 128 + j * 64, 768 + h * 64, 64)
            wload(O_KB, 1024, 128)
            for g in range(2):
                for j, h in enumerate((g, g + 2)):
                    wload(O_QD + g * 256 + j * 64, 1760 + h * 64, 64)
            wload(O_KD, 2016, 128)
            wload(O_CQ, 1280, 192)
            wload(O_CKV, 1472, 256)
            A("vector", lambda e: e.memset(Wfm[:, :, O_KR:O_KR + 64], 0.0), writes=[t_Wfm])
            A("vector", lambda e: e.memset(Wfm[:, :, O_KR + 96:O_KR + 160], 0.0), writes=[t_Wfm])
            wload(O_KR + 64, 1728, 32)
            P.dma("gpsimd", Wv[:, :, 0:256], win_v[:, :, 512:768], sw, writes=[t_Wv])
            P.dma("gpsimd", Wv[:, :, 256:384], win_v[:, :, 1152:1280], sw, writes=[t_Wv])
            P.dma("gpsimd", Wv[:, :, 384:512], win_v[:, :, 2144:2272], sw, writes=[t_Wv])

            def make_rot(src_o, dst_o, nheads):
                sv = Wfm[:, :, src_o:src_o + 64 * nheads].rearrange("p k (q two s) -> p k q two s", two=2, s=16)
                dv = Wfm[:, :, dst_o:dst_o + 64 * nheads].rearrange("p k (q two s) -> p k q two s", two=2, s=16)
                for kc in range(8):
                    A("vector", lambda e, kc=kc: e.tensor_scalar(out=dv[:, kc, :, 0, :], in0=sv[:, kc, :, 1, :], scalar1=-1.0, scalar2=None, op0=ALU.mult),
                      reads=[t_Wfm], writes=[t_Wfm])
                    A("gpsimd", lambda e, kc=kc: e.tensor_copy(out=dv[:, kc, :, 1, :], in_=sv[:, kc, :, 0, :]), reads=[t_Wfm], writes=[t_Wfm])

            make_rot(O_QD, O_QD + 128, 2)
            make_rot(O_QD + 256, O_QD + 384, 2)
            make_rot(O_KD, O_KD + 128, 2)
            for kc in range(8):
                A("vector", lambda e, kc=kc: e.tensor_scalar(out=Wfm[:, kc, O_KR + 160:O_KR + 176], in0=Wfm[:, kc, O_KR + 80:O_KR + 96], scalar1=-1.0, scalar2=None, op0=ALU.mult),
                  reads=[t_Wfm], writes=[t_Wfm])
                A("gpsimd", lambda e, kc=kc: e.tensor_copy(out=Wfm[:, kc, O_KR + 176:O_KR + 192], in_=Wfm[:, kc, O_KR + 64:O_KR + 80]), reads=[t_Wfm], writes=[t_Wfm])

            wuq_f, t_wuqf = AR.alloc([2, 384], F32)
            wukv_f, t_wukvf = AR.alloc([2, 512], F32)
            gq, t_gq = AR.alloc([2], F32)
            gkv, t_gkv = AR.alloc([2], F32)
            Wuq, t_Wuq = AR.alloc([2, 4, 96], BF16)
            Wuqr, t_Wuqr = AR.alloc([2, 4, 96], BF16)
            Wukk, t_Wukk = AR.alloc([2, 4, 64], BF16)
            Wukv_v, t_Wukv = AR.alloc([2, 4, 64], BF16)
            s1 = P.slot()
            A("vector", lambda e: e.memset(wuq_f, 0.0), writes=[t_wuqf])
            A("vector", lambda e: e.memset(gq, 0.0), writes=[t_gq])
            P.dma("sync", wuq_f[:, 0, :], I["w_uq"][l, 0:128, :], s1, writes=[t_wuqf])
            P.dma("sync", wuq_f[0:64, 1, :], I["w_uq"][l, 128:192, :], s1, writes=[t_wuqf])
            P.dma("sync", wukv_f, I["w_ukv"][l].rearrange("(k p) n -> p k n", p=128), s1, writes=[t_wukvf])
            P.dma("sync", gq[:, 0:1], I["mla_q_norm_g"][l, 0:128].rearrange("(p o) -> p o", o=1), s1, writes=[t_gq])
            P.dma("sync", gq[0:64, 1:2], I["mla_q_norm_g"][l, 128:192].rearrange("(p o) -> p o", o=1), s1, writes=[t_gq])
            P.dma("sync", gkv, I["mla_kv_norm_g"][l].rearrange("(k p) -> p k", p=128), s1, writes=[t_gkv], allow_slow_non_contiguous=True)
            wuq4 = wuq_f.rearrange("p k (h c) -> p k h c", h=4)
            wukv4 = wukv_f.rearrange("p k (h c) -> p k h c", h=4)
            A("vector", lambda e: e.memset(Wuqr, 0.0), writes=[t_Wuqr])
            for k in range(2):
                A("vector", lambda e, k=k: e.tensor_scalar(out=Wuq[:, k], in0=wuq4[:, k], scalar1=gq[:, k:k + 1], scalar2=None, op0=ALU.mult),
                  reads=[t_wuqf, t_gq], writes=[t_Wuq])
                A("vector", lambda e, k=k: e.tensor_scalar(out=Wuqr[:, k, :, 64:80], in0=wuq4[:, k, :, 80:96], scalar1=gq[:, k:k + 1], scalar2=-1.0, op0=ALU.mult, op1=ALU.mult),
                  reads=[t_wuqf, t_gq], writes=[t_Wuqr])
                A("vector", lambda e, k=k: e.tensor_scalar(out=Wuqr[:, k, :, 80:96], in0=wuq4[:, k, :, 64:80], scalar1=gq[:, k:k + 1], scalar2=None, op0=ALU.mult),
                  reads=[t_wuqf, t_gq], writes=[t_Wuqr])
                A("vector", lambda e, k=k: e.tensor_scalar(out=Wukk[:, k], in0=wukv4[:, k, :, 0:64], scalar1=gkv[:, k:k + 1], scalar2=None, op0=ALU.mult),
                  reads=[t_wukvf, t_gkv], writes=[t_Wukk])
                A("vector", lambda e, k=k: e.tensor_scalar(out=Wukv_v[:, k], in0=wukv4[:, k, :, 64:128], scalar1=gkv[:, k:k + 1], scalar2=None, op0=ALU.mult),
                  reads=[t_wukvf, t_gkv], writes=[t_Wukv])
            gD, t_gD = AR.alloc([4], F32)
            for ci, nm in ((0, "ax_q_norm_g"), (2, "ax_k_norm_g")):
                for half in range(2):
                    P.dma("sync", gD[half * 64:(half + 1) * 64, ci:ci + 1], I[nm][l, :].rearrange("(p o) -> p o", o=1), s1, writes=[t_gD])
                    for blk, src in enumerate((1, 0, 3, 2)):
                        P.dma("sync", gD[half * 64 + blk * 16:half * 64 + (blk + 1) * 16, ci + 1:ci + 2],
                              I[nm][l, src * 16:(src + 1) * 16].rearrange("(p o) -> p o", o=1), s1, writes=[t_gD])
            A("vector", lambda e: e.tensor_scalar(out=gD[:, 0:2], in0=gD[:, 0:2], scalar1=0.125, scalar2=None, op0=ALU.mult), reads=[t_gD], writes=[t_gD])

            hTr = Rot(P, AR, 2, [8, CH], BF16)
            xrot = Rot(P, AR, 2, [D], F32)
            nb = {"junk": AR.alloc([D], BF16), "ss": AR.alloc([4], F32), "hf": AR.alloc([D], F32), "hb": Rot(P, AR, 2, [D], BF16)}
            stg = Rot(P, AR, 4, [CH], BF16)
            vst = Rot(P, AR, 2, [8, 128], BF16)
            vcst = Rot(P, AR, 2, [4, 128], BF16)
            for b in vst.bufs + vcst.bufs:
                A("vector", lambda e, b=b: e.memset(b[0], 1.0), writes=[b[1]])
            tabr = Rot(P, AR, 2, [2, CH], F32)
            tabc = Rot(P, AR, 2, [2, CH], F32)
            sq_a, t_sqa = AR.alloc([2, CH], F32)
            rb, t_rb = AR.alloc([CH], F32)
            rtmp, t_rtmp = AR.alloc([CH], F32)
            f1, t_f1 = AR.alloc([CH], F32)
            f2, t_f2 = AR.alloc([CH], F32)
            cqn, t_cqn = AR.alloc([2, CH], BF16)
            ckvn, t_ckvn = AR.alloc([2, CH], BF16)
            kpe, t_kpe = AR.alloc([CH], BF16)

            def fm_group(ps, col0, M, hT, t_hT, tps):
                for kc in range(8):
                    mm(ps[0:M, :], Wfm[:, kc, col0:col0 + M], hT[:, kc, :], kc == 0, kc == 7, [t_Wfm, t_hT], [tps])

            for ch in range(NCH):
                cs = slice(ch * CH, (ch + 1) * CH)
                hT, t_hT, _ = hTr.next()
                norm_chunk(xsrc, xtk, ch, G1, SH1, [t_G1, t_mod], hT, t_hT, xrot, nb)
                simple = [(O_QA, QT_A, 0, 0.125, "qa"), (O_QA + 128, QT_A, 1, 0.125, "qa"), (O_KA, KT_A, 0, 1.0, "ka"), (O_KA + 128, KT_A, 1, 1.0, "ka"),
                          (O_QB, QT_B, 0, 0.125, "qb"), (O_QB + 128, QT_B, 1, 0.125, "qb"), (O_KB, KT_B, 0, 1.0, "kb")]
                for col0, dst, gi, scl, nm in simple:
                    ps, tps = ps_next(1, 5)
                    fm_group(ps, col0, 128, hT, t_hT, tps)
                    sg, t_sg, sl = stg.next()
                    A("scalar", lambda e, ps=ps, sg=sg, scl=scl: e.mul(out=sg, in_=ps[:, :], mul=scl), reads=[tps], writes=[t_sg])
                    P.dma("gpsimd", dst[gi, :, cs], sg, sl, reads=[t_sg], writes=[(nm, gi, ch)])
                tb, t_tb, sl = tabr.next()
                P.dma("sync", tb, I["k_ropeD"][:, :, cs].rearrange("a p t -> p a t"), sl, writes=[t_tb])
                for col0, dst, gi, gc, nm in ((O_QD, QT_D, 0, 0, "qd"), (O_QD + 256, QT_D, 1, 0, "qd"), (O_KD, KT_D, 0, 2, "kd")):
                    psa, tpa = ps_next(1, 5)
                    fm_group(psa, col0, 128, hT, t_hT, tpa)
                    psb, tpb = ps_next(1, 5)
                    fm_group(psb, col0 + 128, 128, hT, t_hT, tpb)
                    A("scalar", lambda e, psa=psa: e.activation(out=sq_a[:, 0, :], in_=psa[:, :], func=AF.Square), reads=[tpa], writes=[t_sqa])
                    mm(PS[5][:, :], bd_f, sq_a[:, 0, :], True, True, [t_bd_f, t_sqa], [PST[5]])
                    rstd_from(PS[5][:, :], rb, 64.0, [PST[5]], [t_rb], rtmp)
                    A("vector", lambda e, psa=psa, gc=gc, tb=tb: e.scalar_tensor_tensor(out=f1, in0=psa[:, :], scalar=gD[:, gc:gc + 1], in1=tb[:, 0, :], op0=ALU.mult, op1=ALU.mult),
                      reads=[tpa, t_gD, t_tb], writes=[t_f1])
                    A("vector", lambda e, psb=psb, gc=gc, tb=tb: e.scalar_tensor_tensor(out=f2, in0=psb[:, :], scalar=gD[:, gc + 1:gc + 2], in1=tb[:, 1, :], op0=ALU.mult, op1=ALU.mult),
                      reads=[tpb, t_gD, t_tb], writes=[t_f2])
                    A("gpsimd", lambda e: e.tensor_tensor(out=f1, in0=f1, in1=f2, op=ALU.add), reads=[t_f1, t_f2], writes=[t_f1])
                    sg, t_sg, sl = stg.next()
                    A("vector", lambda e, sg=sg: e.tensor_tensor(out=sg, in0=f1, in1=rb, op=ALU.mult), reads=[t_f1, t_rb], writes=[t_sg])
                    P.dma("gpsimd", dst[gi, :, cs], sg, sl, reads=[t_sg], writes=[(nm, gi, ch)])
                tc, t_tc, sl = tabc.next()
                P.dma("sync", tc[0:96], I["k_ropeC"][:, :, cs].rearrange("a p t -> p a t"), sl, writes=[t_tc])
                for (col0, widths, dstn, t_dn, nfeat) in ((O_CQ, (128, 64), cqn, t_cqn, 192.0), (O_CKV, (128, 128), ckvn, t_ckvn, 256.0)):
                    pss = []
                    for k, w in enumerate(widths):
                        ps, tps = ps_next(1, 5)
                        fm_group(ps, col0 + k * 128, w, hT, t_hT, tps)
                        A("scalar", lambda e, ps=ps, k=k, w=w: e.activation(out=sq_a[0:w, k, :], in_=ps[0:w, :], func=AF.Square), reads=[tps], writes=[t_sqa])
                        pss.append((ps, tps, w))
                    for k, w in enumerate(widths):
                        mm(PS[5][:, :], ones_f[0:w, :], sq_a[0:w, k, :], k == 0, k == 1, [t_ones_f, t_sqa], [PST[5]])
                    rstd_from(PS[5][:, :], rb, nfeat, [PST[5]], [t_rb], rtmp)
                    for k, (ps, tps, w) in enumerate(pss):
                        A("vector", lambda e, ps=ps, k=k, w=w, dstn=dstn: e.tensor_tensor(out=dstn[0:w, k, :], in0=ps[0:w, :], in1=rb[0:w, :], op=ALU.mult),
                          reads=[tps, t_rb], writes=[t_dn])
                psa, tpa = ps_next(1, 5)
                fm_group(psa, O_KR, 96, hT, t_hT, tpa)
                psb, tpb = ps_next(1, 5)
                fm_group(psb, O_KR + 96, 96, hT, t_hT, tpb)
                A("vector", lambda e, psa=psa, tc=tc: e.tensor_tensor(out=f1[64:96, :], in0=psa[64:96, :], in1=tc[64:96, 0, :], op=ALU.mult), reads=[tpa, t_tc], writes=[t_f1])
                A("vector", lambda e, psb=psb, tc=tc: e.tensor_tensor(out=f2[64:96, :], in0=psb[64:96, :], in1=tc[64:96, 1, :], op=ALU.mult), reads=[tpb, t_tc], writes=[t_f2])
                A("gpsimd", lambda e: e.tensor_tensor(out=kpe[64:96, :], in0=f1[64:96, :], in1=f2[64:96, :], op=ALU.add), reads=[t_f1, t_f2], writes=[t_kpe])
                for h in range(4):
                    psa, tpa = ps_next(1, 5)
                    psb, tpb = ps_next(1, 5)
                    for k, w in enumerate((128, 64)):
                        mm(psa[0:96, :], Wuq[0:w, k, h, :], cqn[0:w, k, :], k == 0, k == 1, [t_Wuq, t_cqn], [tpa])
                    for k, w in enumerate((128, 64)):
                        mm(psb[0:96, :], Wuqr[0:w, k, h, :], cqn[0:w, k, :], k == 0, k == 1, [t_Wuqr, t_cqn], [tpb])
                    A("vector", lambda e, psa=psa, tc=tc: e.tensor_tensor(out=f1[0:96, :], in0=psa[0:96, :], in1=tc[0:96, 0, :], op=ALU.mult), reads=[tpa, t_tc], writes=[t_f1])
                    A("vector", lambda e, psb=psb, tc=tc: e.tensor_tensor(out=f2[0:96, :], in0=psb[0:96, :], in1=tc[0:96, 1, :], op=ALU.mult), reads=[tpb, t_tc], writes=[t_f2])
                    sg, t_sg, sl = stg.next()
                    A("gpsimd", lambda e, sg=sg: e.tensor_tensor(out=sg[0:96, :], in0=f1[0:96, :], in1=f2[0:96, :], op=ALU.add), reads=[t_f1, t_f2], writes=[t_sg])
                    P.dma("gpsimd", QT_C[h, :, cs], sg[0:96, :], sl, reads=[t_sg], writes=[("qc", h, ch)])
                    psk, tpk = ps_next(1, 5)
                    for k in range(2):
                        mm(psk[0:64, :], Wukk[:, k, h, :], ckvn[:, k, :], k == 0, k == 1, [t_Wukk, t_ckvn], [tpk])
                    sg, t_sg, sl = stg.next()
                    A("scalar", lambda e, sg=sg, psk=psk: e.copy(out=sg[0:64, :], in_=psk[0:64, :]), reads=[tpk], writes=[t_sg])
                    A("gpsimd", lambda e, sg=sg: e.tensor_copy(out=sg[64:96, :], in_=kpe[64:96, :]), reads=[t_kpe], writes=[t_sg])
                    P.dma("gpsimd", KT_C[h, :, cs], sg[0:96, :], sl, reads=[t_sg], writes=[("kc", h, ch)])
                for t in range(4):
                    tile = ch * 4 + t
                    ts_ = slice(t * 128, (t + 1) * 128)
                    ps, tps = ps_next(6, 8)
                    for kc in range(8):
                        mm(ps[:, :], hT[:, kc, ts_], Wv[:, kc, :], kc == 0, kc == 7, [t_hT, t_Wv], [tps])
                    vb, t_vb, sl = vst.next()
                    A("scalar", lambda e, vb=vb, ps=ps: e.copy(out=vb[:, :, 0:64], in_=ps[:, :].rearrange("p (h c) -> p h c", h=8)), reads=[tps], writes=[t_vb])
                    P.dma("gpsimd", V_ABD[tile * 128:(tile + 1) * 128], vb, sl, reads=[t_vb], writes=[("vabd", tile)])
                    ps, tps = ps_next(6, 8)
                    for k in range(2):
                        mm(ps[:, 0:256], ckvn[:, k, ts_], Wukv_v[:, k].rearrange("p h c -> p (h c)"), k == 0, k == 1, [t_ckvn, t_Wukv], [tps])
                    vb, t_vb, sl = vcst.next()
                    A("scalar", lambda e, vb=vb, ps=ps: e.copy(out=vb[:, :, 0:64], in_=ps[:, 0:256].rearrange("p (h c) -> p h c", h=4)), reads=[tps], writes=[t_vb])
                    P.dma("gpsimd", V_C[tile * 128:(tile + 1) * 128], vb, sl, reads=[t_vb], writes=[("vc", tile)])
            AR.release(mA); P.next_slot = sl_mA
            P.barrier()
            if stop_after == "A":
                break

            def finish_heads(O, tO, heads_blocks, br, tcol, rc, t_rc, yst, add_sink=None):
                n = sum(w for _, _, w in heads_blocks)
                if add_sink is not None:
                    es, t_es = add_sink
                    A("vector", lambda e: e.tensor_tensor(out=rc[64:128, 0:n].rearrange("p (h q) -> p h q", h=4),
                                                          in0=O[64:128, 0:n].rearrange("p (h q) -> p h q", h=4),
                                                          in1=es[64:128, :].unsqueeze(2).to_broadcast([64, 4, n // 4]), op=ALU.add),
                      reads=[tO, t_es], writes=[t_rc])
                    A("vector", lambda e: e.reciprocal(out=rc[64:128, 0:n], in_=rc[64:128, 0:n]), reads=[t_rc], writes=[t_rc])
                else:
                    A("vector", lambda e: e.reciprocal(out=rc[64:128, 0:n], in_=O[64:128, 0:n]), reads=[tO], writes=[t_rc])
                ys, t_ys, sl = yst.next()
                for (h, c0, w) in heads_blocks:
                    po = (h % 2) * 64
                    if len(heads_blocks) == 1:
                        oap = ys[po:po + 64, 0:w]
                    else:
                        oap = ys[po:po + 64, h // 2, 0:w]
                    A("vector", lambda e, oap=oap, c0=c0, w=w: e.tensor_tensor(out=oap, in0=O[0:64, c0:c0 + w], in1=rc[64:128, c0:c0 + w], op=ALU.mult),
                      reads=[tO, t_rc], writes=[t_ys])
                return ys, t_ys, sl

            for br in (0, 1):
                mB = AR.mark(); sl_mB = P.next_slot
                ngq = 2
                KT, t_KT = AR.alloc([2 if br == 0 else 1, S], BF16)
                QT, t_QT = AR.alloc([2, S], BF16)
                Vt, t_Vt = AR.alloc([NT, 4 if br == 0 else 2, 128], BF16)
                sl0 = P.slot()
                if br == 0:
                    for g in range(2):
                        P.dma("sync", KT[:, g, :], KT_A[g], sl0, reads=[("ka", g, c_) for c_ in range(NCH)], writes=[t_KT])
                        P.dma("sync", QT[:, g, :], QT_A[g], sl0, reads=[("qa", g, c_) for c_ in range(NCH)], writes=[t_QT])
                    for tq in range(4):
                        P.dma("sync", Vt[:, tq * 8:(tq + 1) * 8], V_ABD.rearrange("(t p) h c -> p t h c", p=128)[:, tq * 8:(tq + 1) * 8, 0:4, :], sl0, reads=[("vabd", t_) for t_ in range(NT)], writes=[t_Vt])
                else:
                    P.dma("sync", KT[:, 0, :], KT_B[0], sl0, reads=[("kb", 0, c_) for c_ in range(NCH)], writes=[t_KT])
                    for g in range(2):
                        P.dma("sync", QT[:, g, :], QT_B[g], sl0, reads=[("qb", g, c_) for c_ in range(NCH)], writes=[t_QT])
                    for tq in range(4):
                        P.dma("sync", Vt[:, tq * 8:(tq + 1) * 8], V_ABD.rearrange("(t p) h c -> p t h c", p=128)[:, tq * 8:(tq + 1) * 8, 4:6, :], sl0, reads=[("vabd", t_) for t_ in range(NT)], writes=[t_Vt])
                Sbr = Rot(P, AR, 2, [4, 128], F32)
                Ptr = Rot(P, AR, 3, [4, 128], BF16)
                rc, t_rc = AR.alloc([512], F32)
                yst = Rot(P, AR, 2, [2, 128], BF16)
                if br == 0:
                    TT, t_TT = AR.alloc([60, 64], F32)
                    NEGT, t_NEGT = AR.alloc([4, 64], F32)
                    E2, t_E2 = AR.alloc([64, 128], F32)
                    rbT, t_rbT = AR.alloc([64], F32)
                    A("vector", lambda e: e.memset(NEGT, NEG), writes=[t_NEGT])
                    A("vector", lambda e: e.memset(rbT[0:32, :], 1.0), writes=[t_rbT])
                    P.dma("sync", E2[0:32], I["k_e2"], sl0, writes=[t_E2])
                    P.dma("sync", rbT[0:31, 0:60], I["na_rel_bias"][l].rearrange("h r i -> i (h r)"), sl0, reads=[t_rbT], writes=[t_rbT], allow_slow_non_contiguous=True)
                    for q0 in range(0, 64, 8):
                        ps, tps = ps_next(1, 8)
                        for q in range(8):
                            mm(ps[:, q * 64:q * 64 + 60], E2[0:32, q0 + q, :], rbT[0:32, 0:60], True, True, [t_E2, t_rbT], [tps])
                        A("vector", lambda e, ps=ps, q0=q0: e.tensor_copy(out=TT[:, :, q0:q0 + 8].rearrange("p c q -> p q c"),
                                                                         in_=ps[:, :].rearrange("p (q c) -> p q c", q=8)[:, :, 0:60]),
                          reads=[tps], writes=[t_TT])
                    TTv = TT.rearrange("p (g hf r) q -> p hf g r q", g=2, hf=2)
                else:
                    ALt, t_AL = AR.alloc([3, 4, 128], F32)
                    es, t_es = AR.alloc([4], F32)
                    P.dma("sync", ALt, I["k_al"], sl0, writes=[t_AL])
                    P.dma("sync", es, I["win_sink"][l:l + 1, :].partition_broadcast(128), sl0, writes=[t_es])
                    A("scalar", lambda e: e.activation(out=es, in_=es, func=AF.Exp), reads=[t_es], writes=[t_es])

                def rs(r):
                    return min(max(r - 4, 0), 56)

                stepsAB = []
                for j in range(NT):
                    if br == 0:
                        kts = list(range(rs(2 * j) // 2, (rs(2 * j + 1) + 7) // 2 + 1))
                    else:
                        kts = [k for k in (j - 1, j, j + 1) if 0 <= k < NT]
                    for ki, kt in enumerate(kts):
                        stepsAB.append((j, ki, kt, len(kts)))
                pendAB = {}

                def ab_qk(s_):
                    j, ki, kt, nk = stepsAB[s_]
                    qs = slice(j * 128, (j + 1) * 128)
                    ks = slice(kt * 128, (kt + 1) * 128)
                    banks = (ps_next(0, 6), ps_next(0, 6))
                    for h in range(4):
                        if br == 0:
                            half, slot = h % 2, h // 2
                            kidx = slot
                        else:
                            half, slot = h // 2, h % 2
                            kidx = 0
                        po = half * 64
                        ps_, tps_ = banks[half]
                        mm(ps_[:, slot * 128:(slot + 1) * 128], KT[po:po + 64, kidx, ks], QT[po:po + 64, slot, qs], True, True, [t_KT, t_QT], [tps_])
                    pendAB[s_] = banks

                def ab_rest(s_):
                    j, ki, kt, nk = stepsAB[s_]
                    qs = slice(j * 128, (j + 1) * 128)
                    banks = pendAB.pop(s_)
                    O, tO = PS[6 + (j % 2)], PST[6 + (j % 2)]
                    Sb, t_Sb, _ = Sbr.next()
                    Sb5 = Sb.rearrange("p (hf g) q -> p hf g q", hf=2)
                    for half in range(2):
                        ps_, tps_ = banks[half]
                        pv = ps_[:, 0:256].rearrange("p (g q) -> p g q", g=2)
                        if br == 0:
                            for krl in range(2):
                                for rl in range(2):
                                    r, kr = 2 * j + rl, 2 * kt + krl
                                    pp = slice(krl * 64, (krl + 1) * 64)
                                    if rs(r) <= kr < rs(r) + 8:
                                        dr = kr - r
                                        in1 = TTv[pp, half, :, dr + 7, :]
                                        rd = [t_TT]
                                    else:
                                        in1 = NEGT[pp, 0:2, :]
                                        rd = [t_NEGT]
                                    A("vector", lambda e, pp=pp, rl=rl, in1=in1, pv=pv, half=half, Sb5=Sb5: e.tensor_tensor(out=Sb5[pp, half, :, rl * 64:(rl + 1) * 64], in0=pv[pp, :, rl * 64:(rl + 1) * 64], in1=in1, op=ALU.add),
                                      reads=[tps_] + rd, writes=[t_Sb])
                        else:
                            o = kt - j + 1
                            A("vector", lambda e, pv=pv, o=o, half=half, Sb5=Sb5, ALt=ALt: e.tensor_tensor(out=Sb5[:, half, :, :], in0=pv, in1=ALt[:, o, half * 2:half * 2 + 2, :], op=ALU.add),
                              reads=[tps_, t_AL], writes=[t_Sb])
                    Pt, t_Pt, _ = Ptr.next()
                    A("scalar", lambda e, Pt=Pt, Sb=Sb: e.activation(out=Pt, in_=Sb, func=AF.Exp), reads=[t_Sb], writes=[t_Pt])
                    for h in range(4):
                        vh = h if br == 0 else h // 2
                        hh = (h % 2) * 2 + h // 2 if br == 0 else h
                        mm(O[:, h * 128:(h + 1) * 128], Vt[:, kt, vh, :], Pt[:, hh, :], ki == 0 and h == 0, ki == nk - 1, [t_Vt, t_Pt], [tO],
                           skip_group_check=True)
                    if ki == nk - 1:
                        ys, t_ys, sl = finish_heads(O, tO, [(h, h * 128, 128) for h in range(4)], br, None, rc, t_rc, yst,
                                                    add_sink=(es, t_es) if br == 1 else None)
                        P.dma("gpsimd", YT[br, :, :, qs], ys, sl, reads=[t_ys], writes=[("yt", br, j // 4, j % 4)])

                DPAB = 2
                for s_ in range(len(stepsAB) + DPAB):
                    if s_ < len(stepsAB):
                        ab_qk(s_)
                    if s_ >= DPAB:
                        ab_rest(s_ - DPAB)
                AR.release(mB); P.next_slot = sl_mB
                P.barrier()
                if stop_after == "attn%d" % br:
                    break
            if stop_after in ("attn0", "attn1"):
                break

            for br in (2, 3):
                mC = AR.mark(); sl_mC = P.next_slot
                sl0 = P.slot()
                if br == 2:
                    KT, t_KT = AR.alloc([4, S], BF16)
                    Vt, t_Vt = AR.alloc([NT, 4, 128], BF16)
                    for h in range(4):
                        P.dma("sync", KT[0:96, h, :], KT_C[h], sl0, reads=[("kc", h, c_) for c_ in range(NCH)], writes=[t_KT])
                    for tq in range(4):
                        P.dma("sync", Vt[:, tq * 8:(tq + 1) * 8], V_C.rearrange("(t p) h c -> p t h c", p=128)[:, tq * 8:(tq + 1) * 8], sl0, reads=[("vc", t_) for t_ in range(NT)], writes=[t_Vt])
                    scl = 96.0 ** -0.5
                else:
                    KT, t_KT = AR.alloc([1, S], BF16)
                    Vt, t_Vt = AR.alloc([NT, 2, 128], BF16)
                    P.dma("sync", KT[:, 0, :], KT_D[0], sl0, reads=[("kd", 0, c_) for c_ in range(NCH)], writes=[t_KT])
                    for tq in range(4):
                        P.dma("sync", Vt[:, tq * 8:(tq + 1) * 8], V_ABD.rearrange("(t p) h c -> p t h c", p=128)[:, tq * 8:(tq + 1) * 8, 6:8, :], sl0, reads=[("vabd", t_) for t_ in range(NT)], writes=[t_Vt])
                    scl = 1.0
                Qr = Rot(P, AR, 3, [CH], BF16)
                Ptr = Rot(P, AR, 3, [2 * CH], BF16)
                rc, t_rc = AR.alloc([512], F32)
                yst = Rot(P, AR, 2, [CH], BF16)
                blocksCD = [(h, ch) for h in range(4) for ch in range(NCH)]
                qbuf = {}

                def cd_loadq(bi):
                    h, ch = blocksCD[bi]
                    cs = slice(ch * CH, (ch + 1) * CH)
                    Qc, t_Qc, sl = Qr.next()
                    if br == 2:
                        P.dma("sync", Qc[0:96, :], QT_C[h, :, cs], sl, reads=[("qc", h, ch)], writes=[t_Qc])
                    else:
                        po = (h // 2) * 64
                        P.dma("sync", Qc[po:po + 64, :], QT_D[h % 2, po:po + 64, cs], sl, reads=[("qd", h % 2, ch)], writes=[t_Qc])
                    qbuf[bi] = (Qc, t_Qc)

                NP2 = NT // 2
                npairs = len(blocksCD) * NP2
                pairrot = [0]
                pendCD = {}

                def cd_qk(p_):
                    bi, pp_ = divmod(p_, NP2)
                    h, ch = blocksCD[bi]
                    if pp_ == 0 and bi + 1 < len(blocksCD):
                        cd_loadq(bi + 1)
                    Qc, t_Qc = qbuf[bi]
                    k = 1 + pairrot[0] % 3
                    pairrot[0] += 1
                    for half in range(2):
                        kt = pp_ * 2 + half
                        ks = slice(kt * 128, (kt + 1) * 128)
                        ps, tps = PS[2 * k + half], PST[2 * k + half]
                        if br == 2:
                            mm(ps[:, :], KT[0:96, h, ks], Qc[0:96, :], True, True, [t_KT, t_Qc], [tps])
                        else:
                            po = (h // 2) * 64
                            mm(ps[:, :], KT[po:po + 64, 0, ks], Qc[po:po + 64, :], True, True, [t_KT, t_Qc], [tps])
                    pendCD[p_] = k

                def cd_rest(p_):
                    bi, pp_ = divmod(p_, NP2)
                    h, ch = blocksCD[bi]
                    cs = slice(ch * CH, (ch + 1) * CH)
                    k = pendCD.pop(p_)
                    O, tO = PS[bi % 2], PST[bi % 2]
                    Pt, t_Pt, _ = Ptr.next()
                    pbk = PB[k]
                    A("scalar", lambda e, Pt=Pt, pbk=pbk, scl=scl: e.activation(out=Pt, in_=pbk[:, :], func=AF.Exp, scale=scl),
                      reads=[PST[2 * k], PST[2 * k + 1]], writes=[t_Pt])
                    vh = h if br == 2 else h // 2
                    for half in range(2):
                        kt = pp_ * 2 + half
                        mm(O[:, :], Vt[:, kt, vh, :], Pt[:, half * CH:(half + 1) * CH], kt == 0, kt == NT - 1, [t_Vt, t_Pt], [tO])
                    if pp_ == NP2 - 1:
                        ys, t_ys, sl = finish_heads(O, tO, [(h, 0, CH)], br, None, rc, t_rc, yst)
                        po2 = (h % 2) * 64
                        P.dma("gpsimd", YT[br, po2:po2 + 64, h // 2, cs], ys[po2:po2 + 64, :], sl, reads=[t_ys], writes=[("yt", br, ch, h)])

                DPCD = 2
                cd_loadq(0)
                for p_ in range(npairs + DPCD):
                    if p_ < npairs:
                        cd_qk(p_)
                    if p_ >= DPCD:
                        cd_rest(p_ - DPCD)
                AR.release(mC); P.next_slot = sl_mC
                P.barrier()
                if stop_after == "attn%d" % br:
                    break
            if stop_after in ("attn", "attn2", "attn3"):
                break

            mM = AR.mark(); sl_mM = P.next_slot
            Wg, t_Wg = AR.alloc([8, 4096], BF16)
            Wb, t_Wb = AR.alloc([4, 2, D], BF16)
            Wo, t_Wo = AR.alloc([8, D], BF16)
            sw = P.slot()
            for n in range(8):
                P.dma("gpsimd", Wg[:, :, n * 512:(n + 1) * 512], win_v[:, :, 2272 + n * 512:2272 + (n + 1) * 512], sw, writes=[t_Wg])
            P.dma("gpsimd", Wb, I["w_branch"][l].rearrange("n (k p) d -> p n k d", p=128), sw, writes=[t_Wb])
            P.dma("gpsimd", Wo, I["w_out"][l].rearrange("(k p) n -> p k n", p=128), sw, writes=[t_Wo])
            hTr = Rot(P, AR, 1, [8, CH], BF16)
            xrot = Rot(P, AR, 4, [D], F32)
            nb = {"junk": AR.alloc([D], BF16), "ss": AR.alloc([4], F32), "hf": AR.alloc([D], F32), "hb": Rot(P, AR, 2, [D], BF16)}
            Yr = Rot(P, AR, 1, [4, 2, CH], BF16)
            gtr = Rot(P, AR, 2, [CH], BF16)
            macc, t_macc = AR.alloc([CH], F32)
            mtmp = Rot(P, AR, 2, [CH], F32)
            mT, t_mT = AR.alloc([8, CH], BF16)
            slxo = P.slot()
            for ch in range(NCH):
                cs = slice(ch * CH, (ch + 1) * CH)
                hT, t_hT, _ = hTr.next()
                xts = norm_chunk(xsrc, xtk, ch, G1, SH1, [t_G1, t_mod], hT, t_hT, xrot, nb)
                Y, t_Y, sly = Yr.next()
                for b4 in range(4):
                    P.dma("sync", Y[:, b4], YT[b4, :, :, cs], sly, reads=[("yt", b4, ch, k_) for k_ in range(4)], writes=[t_Y])
                for dc in range(8):
                    for n in range(4):
                        pg, tpg = ps_next(0, 4)
                        for kc in range(8):
                            mm(pg[:, :], Wg[:, kc, n * D + dc * 128:n * D + (dc + 1) * 128], hT[:, kc, :], kc == 0, kc == 7, [t_Wg, t_hT], [tpg])
                        gt, t_gt, _ = gtr.next()
                        A("scalar", lambda e, gt=gt, pg=pg: e.activation(out=gt, in_=pg[:, :], func=AF.Sigmoid), reads=[tpg], writes=[t_gt])
                        pb, tpb = ps_next(0, 4)
                        for k in range(2):
                            mm(pb[:, :], Wb[:, n, k, dc * 128:(dc + 1) * 128], Y[:, n, k, :], k == 0, k == 1, [t_Wb, t_Y], [tpb])
                        if n == 0:
                            A("vector", lambda e, pb=pb, gt=gt: e.tensor_tensor(out=macc, in0=pb[:, :], in1=gt, op=ALU.mult), reads=[tpb, t_gt], writes=[t_macc])
                        else:
                            mt, t_mt, _ = mtmp.next()
                            A("vector", lambda e, pb=pb, gt=gt, mt=mt: e.tensor_tensor(out=mt, in0=pb[:, :], in1=gt, op=ALU.mult), reads=[tpb, t_gt], writes=[t_mt])
                            if n < 3:
                                A("gpsimd", lambda e, mt=mt: e.tensor_tensor(out=macc, in0=macc, in1=mt, op=ALU.add), reads=[t_macc, t_mt], writes=[t_macc])
                            else:
                                A("gpsimd", lambda e, mt=mt, dc=dc: e.tensor_tensor(out=mT[:, dc, :], in0=macc, in1=mt, op=ALU.add), reads=[t_macc, t_mt], writes=[t_mT])
                for t in range(4):
                    tile = ch * 4 + t
                    xt, t_xt, slx = xts[t]
                    xn, t_xn = xt, t_xt
                    for nh in range(2):
                        po_, tpo = ps_next(4, 8)
                        for dc in range(8):
                            mm(po_[:, :], mT[:, dc, t * 128:(t + 1) * 128], Wo[:, dc, nh * 512:(nh + 1) * 512], dc == 0, dc == 7, [t_mT, t_Wo], [tpo])
                        mt, t_mt, _ = mtmp.next()
                        A("vector", lambda e, po_=po_, mt=mt, nh=nh: e.tensor_tensor(out=mt, in0=po_[:, :], in1=GT1[:, nh * 512:(nh + 1) * 512], op=ALU.mult),
                          reads=[tpo, t_mod], writes=[t_mt])
                        A("gpsimd", lambda e, xn=xn, xt=xt, mt=mt, nh=nh: e.tensor_tensor(out=xn[:, nh * 512:(nh + 1) * 512], in0=xt[:, nh * 512:(nh + 1) * 512], in1=mt, op=ALU.add),
                          reads=[t_xt, t_mt], writes=[t_xn])
                    P.dma("gpsimd", XR[tile * 128:(tile + 1) * 128, :], xn, slx, reads=[t_xn, ("xr", tile)], writes=[("xr", tile), ("xm", tile)])
            AR.release(mM); P.next_slot = sl_mM
            P.barrier()
            if stop_after == "merge":
                break

            mF = AR.mark(); sl_mF = P.next_slot
            SC = 1024
            h2, t_h2 = AR.alloc([8, SC], BF16)
            acc, t_acc = AR.alloc([8, D], F32)
            comb, t_comb = AR.alloc([8, 32], F32)
            Wr, t_Wr = AR.alloc([8, 36], BF16)
            brt, t_brt = AR.alloc([36], F32)
            sw = P.slot()
            P.dma("gpsimd", Wr[:, :, 0:4], I["w_group"][l].rearrange("(k p) n -> p k n", p=128), sw, writes=[t_Wr])
            P.dma("gpsimd", Wr[:, :, 4:36], I["w_router"][l].rearrange("(k p) n -> p k n", p=128), sw, writes=[t_Wr])
            swb = P.slot()
            P.dma("sync", brt[:, 0:4], I["b_group"][l:l + 1, :].partition_broadcast(128), swb, writes=[t_brt])
            P.dma("sync", brt[:, 4:36], I["b_router"][l:l + 1, :].partition_broadcast(128), swb, writes=[t_brt])
            hTr = Rot(P, AR, 1, [8, CH], BF16)
            xrot = Rot(P, AR, 2, [D], F32)
            nb = {"junk": AR.alloc([D], BF16), "ss": AR.alloc([4], F32), "hf": AR.alloc([D], F32), "hb": Rot(P, AR, 2, [D], BF16)}
            W1r = Rot(P, AR, 2, [8, 256], BF16)
            W3r = Rot(P, AR, 2, [8, 256], BF16)
            W2r = Rot(P, AR, 2, [2, D], BF16)
            slr = Rot(P, AR, 2, [CH], F32)
            hidr = Rot(P, AR, 3, [2, CH], BF16)
            rt, t_rt = AR.alloc([160], F32)
            xo = Rot(P, AR, 2, [D], F32)
            gfin, t_gfin = AR.alloc([D], F32)
            if l == depth - 1:
                P.dma("sync", gfin, I["final_norm_g"].rearrange("(o d) -> o d", o=1).partition_broadcast(128), swb, writes=[t_gfin])
            for sc in range(4):
                for c4 in range(2):
                    ch = sc * 2 + c4
                    hT, t_hT, _ = hTr.next()
                    norm_chunk(XR, "xm", ch, G2, SH2, [t_G2, t_mod], hT, t_hT, xrot, nb)
                    A("gpsimd", lambda e, c4=c4, hT=hT: e.tensor_copy(out=h2[:, :, c4 * CH:(c4 + 1) * CH], in_=hT), reads=[t_hT], writes=[t_h2])
                    for t in range(4):
                        lt = c4 * 4 + t
                        ps, tps = ps_next(0, 4)
                        for kc in range(8):
                            mm(ps[:, 0:36], hT[:, kc, t * 128:(t + 1) * 128], Wr[:, kc, :], kc == 0, kc == 7, [t_hT, t_Wr], [tps])
                        lg = rt[:, 0:36]
                        V_ = "vector"
                        R = [t_rt]
                        A(V_, lambda e, ps=ps: e.tensor_tensor(out=lg, in0=ps[:, 0:36], in1=brt, op=ALU.add), reads=[tps, t_brt], writes=R)
                        gmax, ngm, gsum, gw = rt[:, 36:37], rt[:, 37:38], rt[:, 38:39], rt[:, 39:40]
                        goh, pen, ge = rt[:, 40:44], rt[:, 44:48], rt[:, 48:52]
                        el2 = rt[:, 52:84]
                        oh1, el3, oh2 = rt[:, 84:116], rt[:, 116:148], rt[:, 52:84]
                        m1, m2, dd, ee, w1, w2 = (rt[:, 148 + i:149 + i] for i in range(6))
                        A(V_, lambda e: e.reduce_max(out=gmax, in_=lg[:, 0:4], axis=AX.X), reads=R, writes=R)
                        A(V_, lambda e: e.tensor_scalar(out=ngm, in0=gmax, scalar1=-1.0, scalar2=None, op0=ALU.mult), reads=R, writes=R)
                        A(V_, lambda e: e.tensor_scalar(out=goh, in0=lg[:, 0:4], scalar1=gmax, scalar2=None, op0=ALU.is_ge), reads=R, writes=R)
                        A("scalar", lambda e: e.activation(out=ge, in_=lg[:, 0:4], func=AF.Exp, bias=ngm, scale=1.0, accum_out=gsum), reads=R, writes=R)
                        A(V_, lambda e: e.reciprocal(out=gw, in_=gsum), reads=R, writes=R)
                        A(V_, lambda e: e.tensor_scalar(out=pen, in0=goh, scalar1=-1.0, scalar2=1.0e9, op0=ALU.add, op1=ALU.mult), reads=R, writes=R)
                        A(V_, lambda e: e.tensor_tensor(out=el2.rearrange("p (g x) -> p g x", g=4), in0=lg[:, 4:36].rearrange("p (g x) -> p g x", g=4),
                                                        in1=pen.unsqueeze(2).to_broadcast([128, 4, 8]), op=ALU.add), reads=R, writes=R)
                        A(V_, lambda e: e.reduce_max(out=m1, in_=el2, axis=AX.X), reads=R, writes=R)
                        A(V_, lambda e: e.tensor_scalar(out=oh1, in0=el2, scalar1=m1, scalar2=None, op0=ALU.is_ge), reads=R, writes=R)
                        A(V_, lambda e: e.scalar_tensor_tensor(out=el3, in0=oh1, scalar=-1.0e9, in1=el2, op0=ALU.mult, op1=ALU.add), reads=R, writes=R)
                        A(V_, lambda e: e.reduce_max(out=m2, in_=el3, axis=AX.X), reads=R, writes=R)
                        A(V_, lambda e: e.tensor_scalar(out=oh2, in0=el3, scalar1=m2, scalar2=None, op0=ALU.is_ge), reads=R, writes=R)
                        A(V_, lambda e: e.tensor_tensor(out=dd, in0=m2, in1=m1, op=ALU.subtract), reads=R, writes=R)
                        A("scalar", lambda e: e.activation(out=ee, in_=dd, func=AF.Exp), reads=R, writes=R)
                        A(V_, lambda e: e.tensor_scalar(out=w1, in0=ee, scalar1=1.0, scalar2=None, op0=ALU.add), reads=R, writes=R)
                        A(V_, lambda e: e.reciprocal(out=w1, in_=w1), reads=R, writes=R)
                        A(V_, lambda e: e.tensor_tensor(out=w2, in0=ee, in1=w1, op=ALU.mult), reads=R, writes=R)
                        A(V_, lambda e: e.tensor_tensor(out=w1, in0=w1, in1=gw, op=ALU.mult), reads=R, writes=R)
                        A(V_, lambda e: e.tensor_tensor(out=w2, in0=w2, in1=gw, op=ALU.mult), reads=R, writes=R)
                        A(V_, lambda e, lt=lt: e.tensor_scalar(out=comb[:, lt, :], in0=oh1, scalar1=w1, scalar2=None, op0=ALU.mult), reads=R, writes=[t_comb])
                        A(V_, lambda e, lt=lt: e.scalar_tensor_tensor(out=comb[:, lt, :], in0=oh2, scalar=w2, in1=comb[:, lt, :], op0=ALU.mult, op1=ALU.add), reads=R + [t_comb], writes=[t_comb])
                stepsM = [(ex, c4) for ex in range(32) for c4 in range(2)]
                wbuf = {}
                hbuf = {}

                def moe_s1(i_):
                    ex, c4 = stepsM[i_]
                    if c4 == 0:
                        W1, t_W1, s1_ = W1r.next()
                        W3, t_W3, s3_ = W3r.next()
                        W2, t_W2, s2_ = W2r.next()
                        P.dma("gpsimd", W1, I["w_exp1"][l, ex].rearrange("(k p) n -> p k n", p=128), s1_, writes=[t_W1])
                        P.dma("gpsimd", W3, I["w_exp3"][l, ex].rearrange("(k p) n -> p k n", p=128), s3_, writes=[t_W3])
                        P.dma("gpsimd", W2, I["w_exp2"][l, ex].rearrange("(k p) n -> p k n", p=128), s2_, writes=[t_W2])
                        wbuf[ex] = (W1, t_W1, W3, t_W3, W2, t_W2)
                    W1, t_W1, W3, t_W3, W2, t_W2 = wbuf[ex]
                    hid, t_hid, _ = hidr.next()
                    for fh in range(2):
                        p1, tp1 = ps_next(0, 4)
                        for kc in range(8):
                            mm(p1[:, :], W1[:, kc, fh * 128:(fh + 1) * 128], h2[:, kc, c4 * CH:(c4 + 1) * CH], kc == 0, kc == 7, [t_W1, t_h2], [tp1])
                        p3, tp3 = ps_next(0, 4)
                        for kc in range(8):
                            mm(p3[:, :], W3[:, kc, fh * 128:(fh + 1) * 128], h2[:, kc, c4 * CH:(c4 + 1) * CH], kc == 0, kc == 7, [t_W3, t_h2], [tp3])
                        sl_, t_sl, _ = slr.next()
                        A("scalar", lambda e, sl_=sl_, p1=p1: e.activation(out=sl_, in_=p1[:, :], func=AF.Silu), reads=[tp1], writes=[t_sl])
                        A("vector", lambda e, hid=hid, fh=fh, p3=p3, sl_=sl_: e.tensor_tensor(out=hid[:, fh, :], in0=p3[:, :], in1=sl_, op=ALU.mult), reads=[tp3, t_sl], writes=[t_hid])
                    hbuf[i_] = (hid, t_hid)

                def moe_s2(i_):
                    ex, c4 = stepsM[i_]
                    W1, t_W1, W3, t_W3, W2, t_W2 = wbuf[ex]
                    hid, t_hid = hbuf.pop(i_)
                    for t in range(4):
                        lt = c4 * 4 + t
                        for nh in range(2):
                            po_, tpo = ps_next(4, 8)
                            for fh in range(2):
                                mm(po_[:, :], hid[:, fh, t * 128:(t + 1) * 128], W2[:, fh, nh * 512:(nh + 1) * 512], fh == 0, fh == 1, [t_hid, t_W2], [tpo])
                            asl = acc[:, lt, nh * 512:(nh + 1) * 512]
                            if ex == 0:
                                A("vector", lambda e, po_=po_, asl=asl, lt=lt: e.tensor_scalar(out=asl, in0=po_[:, :], scalar1=comb[:, lt, 0:1], scalar2=None, op0=ALU.mult),
                                  reads=[tpo, t_comb], writes=[(t_acc, lt)])
                            else:
                                A("vector", lambda e, po_=po_, asl=asl, lt=lt, ex=ex: e.scalar_tensor_tensor(out=asl, in0=po_[:, :], scalar=comb[:, lt, ex:ex + 1], in1=asl, op0=ALU.mult, op1=ALU.add),
                                  reads=[tpo, t_comb, (t_acc, lt)], writes=[(t_acc, lt)])

                for i_ in range(len(stepsM) + 1):
                    if i_ < len(stepsM):
                        moe_s1(i_)
                    if i_ >= 1:
                        moe_s2(i_ - 1)
                for lt in range(8):
                    tile = sc * 8 + lt
                    xt, t_xt, slx = xrot.next()
                    P.dma("sync", xt, XR[tile * 128:(tile + 1) * 128, :], slx, reads=[("xm", tile)], writes=[t_xt])
                    xn, t_xn, slo = xo.next()
                    A("gpsimd", lambda e, lt=lt, xn=xn: e.tensor_tensor(out=xn, in0=acc[:, lt, :], in1=GT2, op=ALU.mult), reads=[(t_acc, lt), t_mod], writes=[t_xn])
                    A("gpsimd", lambda e, xn=xn, xt=xt: e.tensor_tensor(out=xn, in0=xn, in1=xt, op=ALU.add), reads=[t_xn, t_xt], writes=[t_xn])
                    if l < depth - 1:
                        P.dma("sync", XR[tile * 128:(tile + 1) * 128, :], xn, slo, reads=[t_xn, ("xm", tile)], writes=[("xr", tile)])
                    else:
                        junk, t_junk = nb["junk"]
                        ssq, t_ssq = nb["ss"]
                        A("scalar", lambda e, xn=xn: e.activation(out=junk, in_=xn, func=AF.Square, accum_out=ssq[:, 0:1]), reads=[t_xn], writes=[t_junk, t_ssq])
                        rstd_from(ssq[:, 0:1], ssq[:, 1:2], float(D), [t_ssq], [t_ssq], ssq[:, 2:3])
                        A("vector", lambda e, xn=xn: e.scalar_tensor_tensor(out=xn, in0=xn, scalar=ssq[:, 1:2], in1=gfin, op0=ALU.mult, op1=ALU.mult),
                          reads=[t_xn, t_ssq, t_gfin], writes=[t_xn])
                        out_dmas.append(P.dma("sync", y_out[tile * 128:(tile + 1) * 128, :], xn, slo, reads=[t_xn], writes=[("y", tile)]))
            AR.release(mF); P.next_slot = sl_mF
            P.barrier()

        if not out_dmas:
            d0, t_d0 = AR.alloc([8], F32)
            A("vector", lambda e: e.memset(d0, 0.0), writes=[t_d0])
            out_dmas.append(P.dma("sync", y_out[0:128, 0:8], d0, P.slot(), reads=[t_d0]))
        finals = list(P.dma_last.values())
        P.emit(final_waits=finals)
    return nc


_CACHE = {}


def kernel(**inputs):
    if "nc" not in _CACHE:
        _CACHE["nc"] = build_program()
        _CACHE["consts"] = host_consts()
    nc = _CACHE["nc"]
    consts = _CACHE["consts"]
    x = np.ascontiguousarray(np.asarray(inputs["x"], dtype=np.float32))
    c = np.ascontiguousarray(np.asarray(inputs["c"], dtype=np.float32))
    shared = {name: np.ascontiguousarray(np.asarray(inputs[name], dtype=np.float32)) for name, _ in WEIGHT_SPECS}
    shared.update(consts)
    in_maps = []
    for b in range(8):
        m = dict(shared)
        m["x"] = x[b]
        m["c"] = c[b]
        in_maps.append(m)
    res = run_bass_kernel_spmd(nc, in_maps, core_ids=list(range(8)))
    out = np.stack([np.asarray(res.results[b]["y"], dtype=np.float32) for b in range(8)], axis=0)
    return out
```
